# Optimizing a Trainium2 kernel written in Bass

```python
import jax, jax.numpy as jnp
from jax import lax
import numpy as np

D_MODEL = 2048
BATCH = 4
SEQ = 2048
DEPTH = 2

CONV_WIDTH = D_MODEL // 2
CONV_KERNEL = 31
POOL_WIDTH = D_MODEL // 2
POOL_WINDOWS = (2, 4, 8, 16)
N_POOL_GROUPS = len(POOL_WINDOWS)
POOL_GROUP_IN = POOL_WIDTH // N_POOL_GROUPS
POOL_GROUP_OUT = D_MODEL // N_POOL_GROUPS
IN_COLS = 2 * CONV_WIDTH + POOL_WIDTH + 2 * D_MODEL
N_GROUPS = 4
EXPERTS_PER_GROUP = 8
TOP_K_IN_GROUP = 2
D_EXPERT = D_MODEL // 8
PLE_DIM = 256
EPS = 1e-6

kernel_name = "hybrid_conv_pool_hmoe_ple"


def rms_norm(x, g):
    xf = x.astype(jnp.float32)
    y = xf * lax.rsqrt(jnp.mean(xf * xf, axis=-1, keepdims=True) + EPS)
    return (y * g.astype(jnp.float32)).astype(x.dtype)


def layer_norm(x, g, b):
    xf = x.astype(jnp.float32)
    mu = jnp.mean(xf, axis=-1, keepdims=True)
    xc = xf - mu
    var = jnp.mean(xc * xc, axis=-1, keepdims=True)
    y = xc * lax.rsqrt(var + EPS)
    return (y * g.astype(jnp.float32) + b.astype(jnp.float32)).astype(x.dtype)


def conformer_conv_branch(a_in, b_glu, conv_w, conv_b, ln_g, ln_b, w_conv_out):
    a = a_in + b_glu
    a = a[..., :CONV_WIDTH] * jax.nn.sigmoid(a[..., CONV_WIDTH:])
    a = lax.conv_general_dilated(
        a, conv_w[:, None, :].astype(a.dtype), window_strides=(1,),
        padding=[(CONV_KERNEL - 1, 0)], dimension_numbers=("NWC", "WIO", "NWC"),
        feature_group_count=CONV_WIDTH) + conv_b
    a = jax.nn.silu(layer_norm(a, ln_g, ln_b))
    return a @ w_conv_out


def multiscale_pool_branch(v, pool_w, pool_scale):
    S = v.shape[1]
    vf = v.astype(jnp.float32)
    csum = jnp.cumsum(vf, axis=1)
    pos = jnp.arange(S) + 1
    outs = []
    for g, w in enumerate(POOL_WINDOWS):
        sl = slice(g * POOL_GROUP_IN, (g + 1) * POOL_GROUP_IN)
        c = csum[..., sl]
        prev = jnp.pad(c, ((0, 0), (w, 0), (0, 0)))[:, :S]
        count = jnp.minimum(pos, w).astype(jnp.float32)[None, :, None]
        outs.append((c - prev) / count - vf[..., sl])
    pooled = jnp.stack(outs, axis=2).astype(v.dtype)
    y = jnp.einsum("bsgc,gcd->bsgd", pooled, pool_w)
    return y.reshape(v.shape[0], S, D_MODEL) * pool_scale


def hierarchical_moe(v, rg_w, rg_b, re_w, re_b, w_gate, w_up, w_down):
    B, S, D = v.shape
    vt = v.reshape(B * S, D)
    g_logits = (vt @ rg_w + rg_b).astype(jnp.float32)
    g_probs = jax.nn.softmax(g_logits, axis=-1)
    g_val, g_idx = lax.top_k(g_probs, 1)
    e_logits = (vt @ re_w + re_b).astype(jnp.float32).reshape(-1, N_GROUPS, EXPERTS_PER_GROUP)
    e_sel = jnp.take_along_axis(e_logits, g_idx[:, :, None], axis=1)[:, 0]
    e_probs = jax.nn.softmax(e_sel, axis=-1)
    e_val, e_idx = lax.top_k(e_probs, TOP_K_IN_GROUP)
    e_val = e_val / jnp.sum(e_val, axis=-1, keepdims=True)
    weights = g_val * e_val
    combine = jnp.einsum("ng,nke,nk->nge",
                         jax.nn.one_hot(g_idx[:, 0], N_GROUPS, dtype=jnp.float32),
                         jax.nn.one_hot(e_idx, EXPERTS_PER_GROUP, dtype=jnp.float32),
                         weights).astype(v.dtype)
    y = jnp.zeros_like(vt)
    for g in range(N_GROUPS):
        h = jax.nn.silu(jnp.einsum("nd,edf->nef", vt, w_gate[g])) * jnp.einsum("nd,edf->nef", vt, w_up[g])
        h = h * combine[:, g, :, None]
        y = y + jnp.einsum("nef,efd->nd", h, w_down[g])
    return y.reshape(B, S, D)


def setup_inputs(seed: int = 0) -> dict:
    key = jax.random.key(seed)
    ks = jax.random.split(key, 24)
    f32 = jnp.float32
    L, D, Cc, E, G, F = DEPTH, D_MODEL, CONV_WIDTH, EXPERTS_PER_GROUP, N_GROUPS, D_EXPERT

    def nrm(k, shape, scale):
        return jax.random.normal(k, shape, f32) * scale

    def gain(k, shape):
        return 1.0 + 0.02 * jax.random.normal(k, shape, f32)

    return {
        "x": nrm(ks[0], (BATCH, SEQ, D), 1.0),
        "p": nrm(ks[1], (DEPTH, BATCH, SEQ, PLE_DIM), 1.0),
        "norm_mix_g": gain(ks[2], (L, D)),
        "w_in": nrm(ks[3], (L, D, IN_COLS), D ** -0.5),
        "b_glu": nrm(ks[4], (L, 2 * Cc), 0.02),
        "conv_w": nrm(ks[5], (L, CONV_KERNEL, Cc), CONV_KERNEL ** -0.5),
        "conv_b": nrm(ks[6], (L, Cc), 0.02),
        "conv_ln_g": gain(ks[7], (L, Cc)),
        "conv_ln_b": nrm(ks[8], (L, Cc), 0.02),
        "w_conv_out": nrm(ks[9], (L, Cc, D), Cc ** -0.5),
        "pool_w": nrm(ks[10], (L, N_POOL_GROUPS, POOL_GROUP_IN, POOL_GROUP_OUT), POOL_GROUP_IN ** -0.5),
        "pool_scale": gain(ks[11], (L, D)),
        "w_out": nrm(ks[12], (L, D, D), D ** -0.5),
        "norm_ffn_g": gain(ks[13], (L, D)),
        "router_group_w": nrm(ks[14], (L, D, G), D ** -0.5),
        "router_group_b": nrm(ks[15], (L, G), 0.01),
        "router_expert_w": nrm(ks[16], (L, D, G * E), D ** -0.5),
        "router_expert_b": nrm(ks[17], (L, G * E), 0.01),
        "expert_w_gate": nrm(ks[18], (L, G, E, D, F), D ** -0.5),
        "expert_w_up": nrm(ks[19], (L, G, E, D, F), D ** -0.5),
        "expert_w_down": nrm(ks[20], (L, G, E, F, D), F ** -0.5),
        "norm_ple_g": gain(ks[21], (L, D)),
        "ple_gate_w": nrm(ks[22], (L, D, D), D ** -0.5),
        "ple_proj_w": nrm(ks[23], (L, PLE_DIM, D), PLE_DIM ** -0.5),
        "final_norm_g": gain(jax.random.fold_in(key, 99), (D,)),
    }


def reference(x, p, norm_mix_g, w_in, b_glu, conv_w, conv_b, conv_ln_g, conv_ln_b, w_conv_out,
              pool_w, pool_scale, w_out, norm_ffn_g, router_group_w, router_group_b,
              router_expert_w, router_expert_b, expert_w_gate, expert_w_up, expert_w_down,
              norm_ple_g, ple_gate_w, ple_proj_w, final_norm_g):
    s0 = 2 * CONV_WIDTH
    s1 = s0 + POOL_WIDTH
    s2 = s1 + D_MODEL
    for i in range(DEPTH):
        u = rms_norm(x, norm_mix_g[i])
        z = u @ w_in[i]
        a_in, pool_in, gate_a, gate_b = z[..., :s0], z[..., s0:s1], z[..., s1:s2], z[..., s2:]
        branch_a = conformer_conv_branch(a_in, b_glu[i], conv_w[i], conv_b[i],
                                         conv_ln_g[i], conv_ln_b[i], w_conv_out[i])
        branch_b = multiscale_pool_branch(pool_in, pool_w[i], pool_scale[i])
        merged = jax.nn.sigmoid(gate_a) * branch_a + jax.nn.sigmoid(gate_b) * branch_b
        x = x + merged @ w_out[i]
        v = rms_norm(x, norm_ffn_g[i])
        x = x + hierarchical_moe(v, router_group_w[i], router_group_b[i], router_expert_w[i],
                                 router_expert_b[i], expert_w_gate[i], expert_w_up[i], expert_w_down[i])
        ple = p[i] @ ple_proj_w[i]
        gate = jax.nn.sigmoid(rms_norm(x, norm_ple_g[i]) @ ple_gate_w[i])
        x = x + gate * ple
    return rms_norm(x, final_norm_g)
```

```python
import numpy as np
import concourse.bass as bass
import concourse.mybir as mybir
from concourse.bass_utils import run_bass_kernel_spmd

F32 = mybir.dt.float32
BF16 = mybir.dt.bfloat16
AF = mybir.ActivationFunctionType
ALU = mybir.AluOpType
AX = mybir.AxisListType

NL = 2
D = 2048
T = 1024
H = 62
W = T + H
NPC = 3
PW = W // NPC
KCONV = 31
EPS = 1e-6
NTT = 9
POOL_WIN = (2, 4, 8, 16)

O_GMIX, O_BGLU, O_CONVW, O_CONVB, O_LNG, O_LNB, O_PSC, O_GFFN, O_GPLE = 0, 16, 32, 280, 288, 296, 304, 320, 336
LS = 352
O_GFIN = NL * LS
O_HM = O_GFIN + 16
O_INVC = O_HM + 1
O_RB = O_INVC + 64
NS = O_RB + NL * 36

A_ACTA = 0
A_ACTB = A_ACTA + 8 * (30 + W)
A_MRG = A_ACTB + 8 * (16 + W)
A_DG = A_MRG + 4 * W
A_PTMP = A_DG + 8 * 128
ARENA = A_PTMP + 2 * (16 + W)
RING = 18432
M_VTOK = 7168
M_XJT = M_VTOK + 4096
M_H = M_XJT + 4096
M_Y = M_H + 1536
NTILE = 48
NSLOT = NTILE * 128
ENG = ["pe", "act", "dve", "pool", "sp"]
REGS = {}


class Prog:
    def __init__(self, nc, dry):
        self.nc = nc
        self.dry = dry
        self.q = {e: [] for e in ENG}
        self.sem = {}
        self.cnt = {}
        self.nsem = 0
        self.dma_sems = {}
        self.regs = {}
        if not dry:
            for e in ENG:
                self._new_sem(e)

    def _new_sem(self, e):
        self.nsem += 1
        self.sem[e] = self.nc.alloc_semaphore(f"s_{e}_{self.nsem}")
        self.cnt[e] = 0

    def op(self, e, fn, waits=(), sig=True):
        if self.dry:
            return None
        ev = None
        if sig:
            if self.cnt[e] >= 20000:
                self._new_sem(e)
            self.cnt[e] += 1
            ev = (self.sem[e], self.cnt[e])
        self.q[e].append((fn, tuple(w for w in waits if w is not None), ev, 1))
        return ev

    def dma(self, e, fn, sem_name, waits=()):
        if self.dry:
            return None
        if sem_name not in self.dma_sems:
            self.dma_sems[sem_name] = [self.nc.alloc_semaphore(f"d_{sem_name}"), 0]
        s = self.dma_sems[sem_name]
        s[1] += 16
        ev = (s[0], s[1])
        self.q[e].append((fn, tuple(w for w in waits if w is not None), ev, 16))
        return ev

    def run(self):
        nc = self.nc
        with nc.Block() as block:
            def mk(e):
                def body(eng):
                    seen = {}
                    if e == "pool":
                        r = eng.alloc_register("bc4095")
                        eng.reg_mov(r, 4095)
                        REGS["bc"] = r
                    for fn, waits, ev, amt in self.q[e]:
                        for (s, v) in waits:
                            k = id(s)
                            if seen.get(k, 0) >= v:
                                continue
                            seen[k] = v
                            eng.wait_ge(s, v)
                        ins = fn(eng)
                        if ev is not None:
                            ins.then_inc(ev[0], amt)
                return body
            block.tensor(mk("pe"))
            block.scalar(mk("act"))
            block.vector(mk("dve"))
            block.gpsimd(mk("pool"))
            block.sync(mk("sp"))


class Banks:
    def __init__(self, tensors):
        self.t = tensors
        self.n = len(tensors)
        self.rel = [[] for _ in tensors]
        self.busy = [False] * self.n
        self.i = 0

    def _next(self):
        for _ in range(self.n):
            b = self.i
            self.i = (self.i + 1) % self.n
            if not self.busy[b]:
                self.busy[b] = True
                return b
        raise RuntimeError("no free slot")

    def get(self):
        b = self._next()
        return b, list(self.rel[b])

    def free(self, b, evs):
        self.rel[b] = [e for e in evs if e is not None]
        self.busy[b] = False


class Temps(Banks):
    def get(self):
        b = self._next()
        return b, self.t[b], list(self.rel[b])


class Unit:
    def __init__(self, off, ev, rec):
        self.off = off
        self.ev = ev
        self.rec = rec


class WMgr:
    NSEM = 24
    LOOK = 12

    def __init__(self, P, order):
        self.P = P
        self.order = order
        self.req = []
        self.cur = 0
        self.next_load = 0
        self.loaded = {}
        self.live = []
        self.off = 0
        self.gates = {}

    def set_gate(self, tag, ev):
        self.gates[tag] = ev

    def _alloc(self, n):
        off = self.off
        travelled = 0
        while True:
            if off + n > RING:
                travelled += RING - off
                off = 0
            s, e = off, off + n
            blockers = [a for a in self.live if a["s"] < e and s < a["e"] and a["rel"] is None]
            if not blockers:
                break
            nxt = max(a["e"] for a in blockers)
            travelled += nxt - off
            off = nxt
            if travelled >= RING:
                return None
        waits = []
        keep = []
        for a in self.live:
            if a["s"] < e and s < a["e"]:
                waits += a["rel"]
            else:
                keep.append(a)
        rec = {"s": s, "e": e, "rel": None}
        keep.append(rec)
        assert len(keep) < self.NSEM - 2
        self.live = keep
        self.off = e
        return rec, waits

    def _pump(self, upto):
        while self.next_load <= min(upto, len(self.order) - 1):
            key, n, loader, gate = self.order[self.next_load]
            if gate is not None and gate not in self.gates:
                break
            r = self._alloc(n)
            if r is None:
                break
            rec, waits = r
            if gate is not None:
                waits = waits + [self.gates[gate]]
            i = self.next_load
            ev = self.P.dma("pool", loader(rec["s"]), f"w{i % self.NSEM}", waits=waits)
            self.loaded[i] = Unit(rec["s"], ev, rec)
            self.next_load += 1

    def get(self, key, n, loader, gate=None):
        if self.P.dry:
            self.req.append((key, n, loader, gate))
            return Unit(0, None, None)
        i = self.cur
        self.cur += 1
        assert self.order[i][0] == key, (self.order[i][0], key)
        self._pump(i + self.LOOK)
        assert i in self.loaded, f"ring too small for unit {key}"
        return self.loaded.pop(i)

    def release(self, unit, evs):
        if self.P.dry:
            return
        unit.rec["rel"] = [e for e in evs if e is not None]


def emit(nc, P, Wm, tn, n_layers):
    dr = tn["dram"]
    xT, uT, arena, ring, sm = tn["xT"], tn["uT"], tn["arena"], tn["ring"], tn["sm"]
    identf, identb, onesb, rstdW = tn["identf"], tn["identb"], tn["onesb"], tn["rstdW"]
    PS = Banks(tn["psum"])
    SQ = Temps(tn["sqp"])
    T32 = Temps(tn["t32"])
    TB = Temps(tn["tb16"])
    trib = tn["trib"]

    def pc(p):
        return slice(p * PW, (p + 1) * PW)

    def av(off, n, b):
        return arena[:, off:off + n].rearrange("p (a b) -> p a b", b=b)

    def avf(off_bf, n_f32, b):
        return arena[:, off_bf:off_bf + 2 * n_f32].bitcast(F32).rearrange("p (a b) -> p a b", b=b)

    actA = av(A_ACTA, 8 * (30 + W), 30 + W)
    actB = av(A_ACTB, 8 * (16 + W), 16 + W)
    mrg = av(A_MRG, 4 * W, W)
    dg = av(A_DG, 8 * 128, 128)
    ptmp = av(A_PTMP, 2 * (16 + W), 16 + W)
    I32 = mybir.dt.int32
    o = 0
    rwf = avf(o, 16 * 36, 36); o += 2 * 16 * 36
    LG = avf(o, NTT * 36, 36); o += 2 * NTT * 36
    RS = avf(o, NTT * 64, 64); o += 2 * NTT * 64
    CT = av(o, NTT * 32, 32); o += NTT * 32
    NB = arena[:, o:o + 320].bitcast(F32); o += 320
    S2 = avf(o, NTT * 48, 48); o += 2 * NTT * 48
    SLf = avf(o, NTT * 2, 2); o += 2 * NTT * 2
    SL = arena[:, o:o + 2 * NTT * 2].bitcast(I32).rearrange("p (a b) -> p a b", b=2); o += 2 * NTT * 2
    IWf = arena[:, o:o + 96].bitcast(F32); o += 96
    IW = arena[:, o:o + 96].bitcast(I32); o += 96
    DE = arena[:, o:o + 48]; o += 48
    CMP = arena[:, o:o + 68].bitcast(F32); o += 68
    pTl = av(o, 2 * W, W); o += 2 * W
    assert o <= M_VTOK, o
    vtok = [arena[:, M_VTOK + i * 2048:M_VTOK + (i + 1) * 2048] for i in range(2)]
    xjT2 = [arena[:, M_XJT + i * 2048:M_XJT + (i + 1) * 2048] for i in range(2)]
    xjT3 = [t.rearrange("p (a b) -> p a b", b=128) for t in xjT2]
    hbuf = [arena[:, M_H + i * 768:M_H + (i + 1) * 768] for i in range(2)]
    ytile = [arena[:, M_Y + i * 2048:M_Y + (i + 1) * 2048] for i in range(2)]
    gbA = [arena[:, M_Y + 4096 + i * 2048:M_Y + 4096 + (i + 1) * 2048] for i in range(2)]
    gbB = [arena[:, M_VTOK + i * 2048:M_VTOK + (i + 1) * 2048] for i in range(2)]
    DG2 = [arena[:, M_XJT + i * 512:M_XJT + (i + 1) * 512].rearrange("p (a b) -> p a b", b=128) for i in range(3)]
    assert M_Y + 8192 <= ARENA
    outtmp = avf(A_ACTA, 2 * W, W)

    def rv(off, a, b):
        return ring[:, off:off + a * b].rearrange("p (a b) -> p a b", b=b)

    def smc(col, n=1):
        return sm[:, col:col + n]

    st = {}

    e_sm = P.dma("sp", lambda g: g.dma_start(out=sm[:], in_=dr["sm"]), "sm")
    e_idf = P.dma("sp", lambda g: g.dma_start(out=identf[:], in_=dr["ident"]), "idf")
    e_idb = P.dma("pool", lambda g: g.dma_start(out=identb[:], in_=dr["ident"]), "idb")
    xv = dr["xT"].rearrange("(c p) w -> p c w", p=128)
    x_ev = [[None] * NPC for _ in range(16)]
    for c in range(16):
        q = "sp" if c % 2 == 0 else "act"
        e = P.dma(q, lambda g, c=c: g.dma_start(out=xT[:, c, :], in_=xv[:, c, :]), f"x{c}")
        for p in range(NPC):
            x_ev[c][p] = e
    e_ones = P.op("dve", lambda g: g.memset(onesb[:], 1.0))
    e_tri = P.dma("pool", lambda g: g.dma_start(out=trib[:], in_=dr["tri"]), "tri")
    st_zi = []
    st["u_rd"] = []
    st["arena_rd"] = []

    def mm_group(out_ap, pairs, waits):
        n = len(pairs)
        ev = None
        for i, (l, r) in enumerate(pairs):
            ev = P.op("pe", lambda g, l=l, r=r, i=i: g.matmul(out_ap, lhsT=l, rhs=r, start=(i == 0), stop=(i == n - 1)),
                      waits=(waits if i == 0 else ()), sig=(i == n - 1))
        return ev

    def rmsnorm(gcol, dst_waits):
        u_ev = [[None] * NPC for _ in range(16)]
        for p in range(NPC):
            b, bw = PS.get()
            bank = PS.t[b]
            last = None
            for c in range(16):
                si, sq, sw = SQ.get()
                e_sq = P.op("act", lambda g, c=c, p=p, sq=sq: g.activation(out=sq[:], in_=xT[:, c, pc(p)], func=AF.Square),
                            waits=[x_ev[c][p]] + sw)
                last = P.op("pe", lambda g, c=c, sq=sq, bank=bank: g.matmul(bank[:, 0:PW], lhsT=onesb[:], rhs=sq[:], start=(c == 0), stop=(c == 15)),
                            waits=[e_sq, e_ones] + (bw if c == 0 else []), sig=True)
                SQ.free(si, [last])
            ti, tt, tw = T32.get()
            e_rt = P.op("act", lambda g, tt=tt, bank=bank: g.activation(out=tt[:], in_=bank[:, 0:PW], func=AF.Sqrt, bias=smc(O_EPS), scale=1.0 / D),
                        waits=[last, e_sm] + tw + st.get("rstd_rd", []))
            PS.free(b, [e_rt])
            e_rs = P.op("dve", lambda g, tt=tt, p=p: g.reciprocal(out=rstdW[:, pc(p)], in_=tt[:]), waits=[e_rt] + st.get("rstd_rd", []))
            T32.free(ti, [e_rs])
            for c in range(16):
                u_ev[c][p] = P.op("dve", lambda g, c=c, p=p: g.scalar_tensor_tensor(
                    out=uT[:, c, pc(p)], in0=xT[:, c, pc(p)], scalar=smc(gcol + c), in1=rstdW[:, pc(p)],
                    op0=ALU.mult, op1=ALU.mult), waits=[e_rs, x_ev[c][p]] + dst_waits)
        st["rstd_rd"] = [u_ev[15][NPC - 1]]
        return u_ev

    def layer_body(ly):
        sb = ly * LS
        u_ev = rmsnorm(sb + O_GMIX, st["u_rd"])
        st["u_rd"] = []
        w_in_v = dr["w_in"][ly].rearrange("(c p) n -> p c n", p=128)

        def win_unit(col0, ly=ly, w_in_v=w_in_v):
            def loader(off):
                return lambda g: g.dma_start(out=rv(off, 16, 128), in_=w_in_v[:, :, col0:col0 + 128])
            return Wm.get(("w_in", ly, col0), 16 * 128, loader)

        a_ev = [[None] * NPC for _ in range(8)]
        aw = list(st["arena_rd"])
        e_padA = P.op("dve", lambda g: g.memset(actA[:, :, 0:30], 0.0), waits=aw)
        e_padB = P.op("dve", lambda g: g.memset(actB[:, :, 0:16], 0.0), waits=aw)
        e_padT = P.op("dve", lambda g: g.memset(ptmp[:, :, 0:16], 0.0), waits=aw)
        st["pads"] = [e_padA, e_padB, e_padT]
        first_arena_waits = aw + st["pads"]
        for j in range(8):
            u1 = win_unit(j * 128)
            u2 = win_unit(1024 + j * 128)
            w1 = rv(u1.off, 16, 128)
            w2 = rv(u2.off, 16, 128)
            evl = None
            for p in range(NPC):
                bA, wA = PS.get()
                bB, wB = PS.get()
                eA = mm_group(PS.t[bA][:, 0:PW], [(w1[:, k, :], uT[:, k, pc(p)]) for k in range(16)],
                              [u1.ev] + [u_ev[k][p] for k in range(16)] + wA)
                eB = mm_group(PS.t[bB][:, 0:PW], [(w2[:, k, :], uT[:, k, pc(p)]) for k in range(16)],
                              [u2.ev] + wB)
                evl = eB
                si, sg, sw = TB.get()
                e_sg = P.op("act", lambda g, sg=sg, bB=bB, j=j: g.activation(out=sg[:], in_=PS.t[bB][:, 0:PW], func=AF.Sigmoid, bias=smc(sb + O_BGLU + 8 + j)),
                            waits=[eB, e_sm] + sw)
                PS.free(bB, [e_sg])
                e_a = P.op("dve", lambda g, sg=sg, bA=bA, j=j, p=p: g.scalar_tensor_tensor(
                    out=actA[:, j, 30 + p * PW:30 + (p + 1) * PW], in0=PS.t[bA][:, 0:PW], scalar=smc(sb + O_BGLU + j),
                    in1=sg[:], op0=ALU.add, op1=ALU.mult), waits=[eA, e_sg] + first_arena_waits)
                PS.free(bA, [e_a])
                TB.free(si, [e_a])
                if p == 0:
                    e_a = P.op("dve", lambda g, j=j: g.tensor_scalar(out=actA[:, j, 30:30 + H], in0=actA[:, j, 30:30 + H],
                                                                    scalar1=smc(O_HM), scalar2=None, op0=ALU.mult), waits=[e_a])
                a_ev[j][p] = e_a
            Wm.release(u1, [evl])
            Wm.release(u2, [evl])
        if ly == 0:
            zi = []
            zt = arena[:, A_MRG:A_MRG + 2048]
            e_zt = P.op("dve", lambda g: g.memset(zt, 0.0), waits=first_arena_waits)
            for r_ in range((NSLOT + 256) // 128):
                zi.append(P.dma("sp", lambda g, r_=r_: g.dma_start(out=dr["XS"][r_ * 128:(r_ + 1) * 128, :], in_=zt), "zi",
                                waits=[a_ev[7][NPC - 1], e_zt]))
            for r_ in range(NSLOT // 128, (NSLOT + 256) // 128):
                zi.append(P.dma("sp", lambda g, r_=r_: g.dma_start(out=dr["YS"][r_ * 128:(r_ + 1) * 128, :], in_=zt), "zi", waits=[e_zt]))
            st_zi.append(zi[-1])
        p_ev = [[None] * NPC for _ in range(8)]
        for j in range(8):
            u1 = win_unit(2048 + j * 128)
            w1 = rv(u1.off, 16, 128)
            evl = None
            for p in range(NPC):
                bA, wA = PS.get()
                eA = mm_group(PS.t[bA][:, 0:PW], [(w1[:, k, :], uT[:, k, pc(p)]) for k in range(16)], [u1.ev] + wA)
                evl = eA
                e_p = P.op("act", lambda g, bA=bA, j=j, p=p: g.activation(out=actB[:, j, 16 + p * PW:16 + (p + 1) * PW], in_=PS.t[bA][:, 0:PW], func=AF.Copy),
                           waits=[eA] + first_arena_waits)
                PS.free(bA, [e_p])
                if p == 0:
                    e_p = P.op("dve", lambda g, j=j: g.tensor_scalar(out=actB[:, j, 16:16 + H], in0=actB[:, j, 16:16 + H],
                                                                    scalar1=smc(O_HM), scalar2=None, op0=ALU.mult), waits=[e_p])
                p_ev[j][p] = e_p
            Wm.release(u1, [evl])

        dg_rel = [[] for _ in range(8)]
        dgi = 0
        cv_ev = [[None] * NPC for _ in range(8)]
        for c in range(8):
            banks = []
            for p in (2, 1, 0):
                banks.append((p,) + PS.get())
            last_mm = {}
            for k in range(KCONV):
                slot = dgi % 8
                dgi += 1
                e_dg = P.op("dve", lambda g, slot=slot, k=k, c=c: g.tensor_scalar(
                    out=dg[:, slot, :], in0=identb[:], scalar1=smc(sb + O_CONVW + k * 8 + c), scalar2=None, op0=ALU.mult),
                    waits=[e_idb, e_sm] + dg_rel[slot] + (first_arena_waits if c == 0 else []))
                ev = None
                for (p, b, bw) in banks:
                    w = [e_dg]
                    if k == 0:
                        w += bw + [a_ev[c][q] for q in range(NPC)]
                    ev = P.op("pe", lambda g, slot=slot, k=k, c=c, p=p, b=b: g.matmul(
                        PS.t[b][:, 0:PW], lhsT=dg[:, slot, :], rhs=actA[:, c, p * PW + k:p * PW + k + PW],
                        start=(k == 0), stop=(k == KCONV - 1)), waits=w, sig=True)
                    last_mm[p] = ev
                dg_rel[slot] = [ev]
            for (p, b, bw) in banks:
                e_cv = P.op("act", lambda g, c=c, p=p, b=b: g.activation(out=actA[:, c, 30 + p * PW:30 + (p + 1) * PW], in_=PS.t[b][:, 0:PW],
                                                                         func=AF.Identity, bias=smc(sb + O_CONVB + c)),
                            waits=[last_mm[p], last_mm[0]])
                PS.free(b, [e_cv])
                cv_ev[c][p] = e_cv

        pl_ev = [None] * 8
        pt_rel = []
        for c in range(8):
            gidx = c // 2
            nst = gidx + 1
            src = actB[:, c, :]
            cur = src
            evs = [p_ev[c][q] for q in range(NPC)]
            e_prev = None
            for s in range(nst):
                sh = 1 << s
                dst = ptmp[:, s % 2, :]
                e_prev = P.op("dve", lambda g, cur=cur, dst=dst, sh=sh: g.tensor_tensor(
                    out=dst[:, 16:16 + W], in0=cur[:, 16:16 + W], in1=cur[:, 16 - sh:16 - sh + W], op=ALU.add),
                    waits=evs + pt_rel + ([e_prev] if e_prev is not None else []) + st["pads"])
                cur = dst
                evs = []
            wsz = POOL_WIN[gidx]
            fi, fx, fw = T32.get()
            e_f1 = P.op("dve", lambda g, cur=cur, fx=fx, gidx=gidx: g.tensor_tensor(
                out=fx[:, 0:16], in0=cur[:, 16 + H:16 + H + 16], in1=sm[:, O_INVC + gidx * 16:O_INVC + gidx * 16 + 16], op=ALU.mult),
                waits=[e_prev, e_sm] + fw)
            e_f2 = P.op("dve", lambda g, fx=fx, src=src: g.tensor_tensor(
                out=fx[:, 0:16], in0=fx[:, 0:16], in1=src[:, 16 + H:16 + H + 16], op=ALU.subtract), waits=[e_f1])
            e_pl = P.op("dve", lambda g, cur=cur, src=src, wsz=wsz: g.scalar_tensor_tensor(
                out=src[:, 16:16 + W], in0=cur[:, 16:16 + W], scalar=1.0 / wsz, in1=src[:, 16:16 + W],
                op0=ALU.mult, op1=ALU.subtract), waits=[e_f2])
            e_pl = P.op("dve", lambda g, fx=fx, src=src: g.tensor_copy(out=src[:, 16 + H:16 + H + 16], in_=fx[:, 0:16]), waits=[e_pl])
            T32.free(fi, [e_pl])
            pt_rel = [e_pl]
            pl_ev[c] = e_pl

        c_ev = [[None] * NPC for _ in range(8)]
        ln_rel = []
        for p in range(NPC):
            bS, wS = PS.get()
            bQ, wQ = PS.get()
            eS = eQ = None
            for c in range(8):
                si, sq, sw = SQ.get()
                e_sq = P.op("act", lambda g, c=c, p=p, sq=sq: g.activation(out=sq[:], in_=actA[:, c, 30 + p * PW:30 + (p + 1) * PW], func=AF.Square),
                            waits=[cv_ev[c][p]] + sw)
                eS = P.op("pe", lambda g, c=c, p=p, bS=bS: g.matmul(PS.t[bS][:, 0:PW], lhsT=onesb[:], rhs=actA[:, c, 30 + p * PW:30 + (p + 1) * PW],
                                                                    start=(c == 0), stop=(c == 7)), waits=[cv_ev[c][p]] + (wS if c == 0 else []), sig=True)
                eQ = P.op("pe", lambda g, c=c, sq=sq, bQ=bQ: g.matmul(PS.t[bQ][:, 0:PW], lhsT=onesb[:], rhs=sq[:], start=(c == 0), stop=(c == 7)),
                          waits=[e_sq] + (wQ if c == 0 else []), sig=True)
                SQ.free(si, [eQ])
            mean = rstdW[:, 0:PW]
            rstd = rstdW[:, PW:2 * PW]
            lw = ln_rel + st["rstd_rd"]
            e_mean = P.op("dve", lambda g, bS=bS: g.tensor_scalar(out=mean, in0=PS.t[bS][:, 0:PW], scalar1=1.0 / 1024, scalar2=None, op0=ALU.mult),
                          waits=[eS] + lw)
            PS.free(bS, [e_mean])
            e_msq = P.op("dve", lambda g: g.tensor_tensor(out=rstd, in0=mean, in1=mean, op=ALU.mult), waits=[e_mean] + lw)
            e_var = P.op("dve", lambda g, bQ=bQ: g.scalar_tensor_tensor(out=rstd, in0=PS.t[bQ][:, 0:PW], scalar=1.0 / 1024, in1=rstd,
                                                                        op0=ALU.mult, op1=ALU.subtract), waits=[eQ, e_msq])
            PS.free(bQ, [e_var])
            e_sd = P.op("act", lambda g: g.activation(out=rstd, in_=rstd, func=AF.Sqrt, bias=smc(O_EPS), scale=1.0), waits=[e_var])
            e_rstd = P.op("dve", lambda g: g.reciprocal(out=rstd, in_=rstd), waits=[e_sd])
            e2 = None
            for c in range(8):
                ti, tt, tw = T32.get()
                sl = actA[:, c, 30 + p * PW:30 + (p + 1) * PW]
                e1 = P.op("dve", lambda g, sl=sl, tt=tt: g.tensor_tensor(out=tt[:], in0=sl, in1=mean, op=ALU.subtract),
                          waits=[e_rstd, cv_ev[c][p]] + tw)
                e2 = P.op("dve", lambda g, tt=tt: g.tensor_tensor(out=tt[:], in0=tt[:], in1=rstd, op=ALU.mult), waits=[e1])
                e3 = P.op("act", lambda g, sl=sl, tt=tt, c=c: g.activation(out=sl, in_=tt[:], func=AF.Silu, bias=smc(sb + O_LNB + c), scale=smc(sb + O_LNG + c)),
                          waits=[e2])
                T32.free(ti, [e3])
                c_ev[c][p] = e3
            ln_rel = [e2]
        st["rstd_rd"] = st["rstd_rd"] + ln_rel

        cov = dr["w_conv_out"][ly].rearrange("(c p) n -> p c n", p=128)
        wov = dr["w_out"][ly].rearrange("(c p) n -> p c n", p=128)
        mrg_rel = list(st_zi) if ly == 0 else []
        for kg in range(4):
            pu = Wm.get(("pool_w", ly, kg), 2 * 512,
                        lambda off, kg=kg, ly=ly: (lambda g: g.dma_start(out=rv(off, 2, 512), in_=dr["pool_w"][ly, kg].rearrange("(c p) n -> p c n", p=128))))
            pw_ = rv(pu.off, 2, 512)
            m_ev = [[None] * NPC for _ in range(4)]
            last_pe = None
            for jl in range(4):
                j = kg * 4 + jl
                uga = win_unit(3072 + j * 128)
                ugb = win_unit(5120 + j * 128)
                if jl % 2 == 0:
                    cu = Wm.get(("conv_out", ly, j), 8 * 256,
                                lambda off, j=j, cov=cov: (lambda g: g.dma_start(out=rv(off, 8, 256), in_=cov[:, :, j * 128:j * 128 + 256])))
                    cw = rv(cu.off, 8, 256)
                wga = rv(uga.off, 16, 128)
                wgb = rv(ugb.off, 16, 128)
                for p in range(NPC):
                    bGA, w1 = PS.get()
                    bGB, w2 = PS.get()
                    bA, w3 = PS.get()
                    bB, w4 = PS.get()
                    eGA = mm_group(PS.t[bGA][:, 0:PW], [(wga[:, k, :], uT[:, k, pc(p)]) for k in range(16)], [uga.ev] + w1)
                    eGB = mm_group(PS.t[bGB][:, 0:PW], [(wgb[:, k, :], uT[:, k, pc(p)]) for k in range(16)], [ugb.ev] + w2)
                    eA = mm_group(PS.t[bA][:, 0:PW], [(cw[:, k, (jl % 2) * 128:(jl % 2) * 128 + 128], actA[:, k, 30 + p * PW:30 + (p + 1) * PW]) for k in range(8)],
                                  [cu.ev] + [c_ev[k][p] for k in range(8)] + w3)
                    eB = mm_group(PS.t[bB][:, 0:PW], [(pw_[:, k, jl * 128:jl * 128 + 128], actB[:, 2 * kg + k, 16 + p * PW:16 + (p + 1) * PW]) for k in range(2)],
                                  [pu.ev, pl_ev[2 * kg], pl_ev[2 * kg + 1]] + w4)
                    last_pe = eB
                    s1i, sga, sw1 = TB.get()
                    s2i, sgb, sw2 = TB.get()
                    e_sa = P.op("act", lambda g, sga=sga, bGA=bGA: g.activation(out=sga[:], in_=PS.t[bGA][:, 0:PW], func=AF.Sigmoid), waits=[eGA] + sw1)
                    PS.free(bGA, [e_sa])
                    e_sb = P.op("act", lambda g, sgb=sgb, bGB=bGB: g.activation(out=sgb[:], in_=PS.t[bGB][:, 0:PW], func=AF.Sigmoid), waits=[eGB] + sw2)
                    PS.free(bGB, [e_sb])
                    m1i, m1, mw1 = T32.get()
                    e_m1 = P.op("dve", lambda g, m1=m1, bA=bA, sga=sga: g.tensor_tensor(out=m1[:], in0=PS.t[bA][:, 0:PW], in1=sga[:], op=ALU.mult),
                                waits=[eA, e_sa] + mw1)
                    PS.free(bA, [e_m1])
                    TB.free(s1i, [e_m1])
                    e_m2 = P.op("dve", lambda g, sgb=sgb, bB=bB, j=j: g.scalar_tensor_tensor(out=sgb[:], in0=PS.t[bB][:, 0:PW], scalar=smc(sb + O_PSC + j),
                                                                                             in1=sgb[:], op0=ALU.mult, op1=ALU.mult), waits=[eB, e_sb])
                    PS.free(bB, [e_m2])
                    e_m = P.op("dve", lambda g, m1=m1, sgb=sgb, jl=jl, p=p: g.tensor_tensor(out=mrg[:, jl, pc(p)], in0=m1[:], in1=sgb[:], op=ALU.add),
                               waits=[e_m2, e_m1] + mrg_rel)
                    T32.free(m1i, [e_m])
                    TB.free(s2i, [e_m])
                    m_ev[jl][p] = e_m
                Wm.release(uga, [last_pe])
                Wm.release(ugb, [last_pe])
                if jl % 2 == 1:
                    Wm.release(cu, [last_pe])
            Wm.release(pu, [last_pe])
            if kg == 3:
                st["u_rd"] = [last_pe]
            for qd in range(4):
                wu_ = Wm.get(("w_out", ly, kg, qd), 4 * 512,
                             lambda off, kg=kg, qd=qd, wov=wov: (lambda g: g.dma_start(out=rv(off, 4, 512), in_=wov[:, kg * 4:kg * 4 + 4, qd * 512:qd * 512 + 512])))
                ww = rv(wu_.off, 4, 512)
                evl = None
                for jo4 in range(4):
                    jo = qd * 4 + jo4
                    for p in range(NPC):
                        b, bw = PS.get()
                        e_mm = mm_group(PS.t[b][:, 0:PW], [(ww[:, k, jo4 * 128:jo4 * 128 + 128], mrg[:, k, pc(p)]) for k in range(4)],
                                        [wu_.ev] + [m_ev[k][p] for k in range(4)] + bw)
                        evl = e_mm
                        e_x = P.op("dve", lambda g, jo=jo, p=p, b=b: g.tensor_tensor(out=xT[:, jo, pc(p)], in0=PS.t[b][:, 0:PW], in1=xT[:, jo, pc(p)], op=ALU.add),
                                   waits=[e_mm, x_ev[jo][p]])
                        PS.free(b, [e_x])
                        x_ev[jo][p] = e_x
                Wm.release(wu_, [evl])
                mrg_rel = [evl]
        st["arena_rd"] = list(mrg_rel)

        v_ev = rmsnorm(sb + O_GFFN, st["u_rd"])
        st["u_rd"] = []
        moe_w = list(st["arena_rd"])
        e_rw = P.dma("sp", lambda g, ly=ly: g.dma_start(out=rwf[:], in_=dr["rw"][ly].rearrange("p (a b) -> p a b", b=36)), "rw", waits=moe_w)
        e_pl_ld = P.dma("pool", lambda g, ly=ly: g.dma_start(out=pTl[:], in_=dr["pT"][ly].rearrange("(c p) w -> p c w", p=128)), "pTl", waits=moe_w)
        e_rws = None
        for c in range(16):
            e_rws = P.op("dve", lambda g, c=c: g.tensor_scalar(out=rwf[:, c, :], in0=rwf[:, c, :], scalar1=smc(sb + O_GFFN + c), scalar2=None, op0=ALU.mult),
                         waits=[e_rw, e_sm])
        e_z1 = P.op("dve", lambda g: g.memset(LG[:], 0.0), waits=moe_w)
        e_z2 = P.op("dve", lambda g: g.memset(CT[:], 0.0), waits=moe_w)
        e_zsl0 = P.op("dve", lambda g: g.memset(SLf[:, :, 0:1], float(NSLOT)), waits=moe_w)
        e_zsl = P.op("dve", lambda g: g.memset(SLf[:, :, 1:2], float(NSLOT + 128)), waits=moe_w + [e_zsl0])
        e_zsl = P.op("dve", lambda g: g.tensor_scalar(out=SLf[:], in0=SLf[:], scalar1=smc(O_IOTA), scalar2=None, op0=ALU.add), waits=[e_zsl, e_sm])
        e_on = P.op("dve", lambda g: g.memset(NB[:, 128:160], 1.0), waits=moe_w)
        e_z3 = P.op("dve", lambda g: g.memset(RS[:], 0.0), waits=moe_w)
        ct_ev = []
        for tt in range(NTT):
            t0 = tt * 128
            ts_ = min(128, W - t0)
            pidx = [q for q in range(NPC) if q * PW < t0 + ts_ and (q + 1) * PW > t0]
            b, bw = PS.get()
            bank = PS.t[b]
            e_lg = mm_group(bank[0:ts_, 0:36], [(xT[:, c, t0:t0 + ts_], rwf[:, c, :]) for c in range(16)],
                            [e_rws] + [x_ev[c][q] for c in range(16) for q in pidx] + bw)
            e_rt = P.op("pe", lambda g, bank=bank, t0=t0, ts_=ts_: g.matmul(bank[0:ts_, 64:66], lhsT=rstdW[:, t0:t0 + ts_], rhs=identf[:, 0:2], start=True, stop=True),
                        waits=st["rstd_rd"] + [e_idf], sig=True)
            R = RS[0:ts_, tt, :]
            lg = LG[0:ts_, tt, :]

            def rcol(i, n=1, R=R):
                return R[:, i:i + n]
            e0 = P.op("dve", lambda g, bank=bank, ts_=ts_, R=R: g.tensor_copy(out=R[:, 0:1], in_=bank[0:ts_, 64:65]), waits=[e_rt, e_z3])
            e1 = P.op("dve", lambda g, bank=bank, ts_=ts_, lg=lg, R=R, ly=ly: g.scalar_tensor_tensor(
                out=lg, in0=bank[0:ts_, 0:36], scalar=R[:, 0:1], in1=sm[0:ts_, O_RB + ly * 36:O_RB + ly * 36 + 36], op0=ALU.mult, op1=ALU.add),
                waits=[e_lg, e0, e_z1, e_sm])
            PS.free(b, [e1])
            e2 = P.op("dve", lambda g, lg=lg, R=R: g.tensor_reduce(out=R[:, 1:2], in_=lg[:, 0:4], axis=AX.X, op=ALU.max, negate=True), waits=[e1])
            e3 = P.op("act", lambda g, lg=lg, R=R: g.activation(out=R[:, 4:8], in_=lg[:, 0:4], func=AF.Exp, bias=R[:, 1:2], accum_out=R[:, 2:3]), waits=[e2])
            e4 = P.op("dve", lambda g, lg=lg, R=R: g.tensor_scalar(out=R[:, 8:12], in0=lg[:, 0:4], scalar1=R[:, 1:2], scalar2=0.0, op0=ALU.add, op1=ALU.is_equal), waits=[e2])
            e5 = P.op("dve", lambda g, R=R: g.reciprocal(out=R[:, 3:4], in_=R[:, 2:3]), waits=[e3])
            e6 = P.op("dve", lambda g, lg=lg, R=R: g.tensor_scalar(out=R[:, 16:24], in0=lg[:, 4:12], scalar1=R[:, 8:9], scalar2=None, op0=ALU.mult), waits=[e4])
            for gi in range(1, 4):
                e6 = P.op("dve", lambda g, lg=lg, R=R, gi=gi: g.scalar_tensor_tensor(out=R[:, 16:24], in0=lg[:, 4 + 8 * gi:12 + 8 * gi], scalar=R[:, 8 + gi:9 + gi],
                                                                                  in1=R[:, 16:24], op0=ALU.mult, op1=ALU.add), waits=[e6])
            e7 = P.op("dve", lambda g, R=R: g.tensor_reduce(out=R[:, 12:13], in_=R[:, 16:24], axis=AX.X, op=ALU.max), waits=[e6])
            e8 = P.op("dve", lambda g, R=R: g.tensor_scalar(out=R[:, 24:32], in0=R[:, 16:24], scalar1=R[:, 12:13], scalar2=None, op0=ALU.is_equal), waits=[e7])
            e9 = P.op("dve", lambda g, R=R: g.scalar_tensor_tensor(out=R[:, 32:40], in0=R[:, 24:32], scalar=-1e30, in1=R[:, 16:24], op0=ALU.mult, op1=ALU.add), waits=[e8])
            e10 = P.op("dve", lambda g, R=R: g.tensor_reduce(out=R[:, 13:14], in_=R[:, 32:40], axis=AX.X, op=ALU.max), waits=[e9])
            e11 = P.op("dve", lambda g, R=R: g.tensor_scalar(out=R[:, 40:48], in0=R[:, 32:40], scalar1=R[:, 13:14], scalar2=None, op0=ALU.is_equal), waits=[e10])
            e12 = P.op("dve", lambda g, R=R: g.tensor_tensor(out=R[:, 14:15], in0=R[:, 13:14], in1=R[:, 12:13], op=ALU.subtract), waits=[e10])
            e13 = P.op("act", lambda g, R=R: g.activation(out=R[:, 15:16], in_=R[:, 14:15], func=AF.Exp), waits=[e12])
            e14 = P.op("dve", lambda g, R=R: g.tensor_scalar(out=R[:, 48:49], in0=R[:, 15:16], scalar1=1.0, scalar2=None, op0=ALU.add), waits=[e13])
            e15 = P.op("dve", lambda g, R=R: g.reciprocal(out=R[:, 49:50], in_=R[:, 48:49]), waits=[e14])
            e16 = P.op("dve", lambda g, R=R: g.tensor_tensor(out=R[:, 50:51], in0=R[:, 49:50], in1=R[:, 3:4], op=ALU.mult), waits=[e15, e5])
            e17 = P.op("dve", lambda g, R=R: g.tensor_tensor(out=R[:, 51:52], in0=R[:, 50:51], in1=R[:, 15:16], op=ALU.mult), waits=[e16])
            e18 = P.op("dve", lambda g, R=R: g.tensor_tensor(out=R[:, 52:60], in0=R[:, 24:32], in1=R[:, 40:48], op=ALU.add), waits=[e8, e11, e17])
            e20 = None
            for gi in range(4):
                e20 = P.op("dve", lambda g, R=R, gi=gi, ts_=ts_, tt=tt: g.tensor_scalar(out=CT[0:ts_, tt, gi * 8:gi * 8 + 8], in0=R[:, 52:60], scalar1=R[:, 8 + gi:9 + gi],
                                                                                   scalar2=None, op0=ALU.mult), waits=[e18, e4, e_z2])
            ct_ev.append(e20)
            st["rstd_rd"] = st["rstd_rd"] + [e_rt]

        Nrep, TL, INC, BASE, ONF = NB[:, 0:32], NB[:, 32:64], NB[:, 64:96], NB[:, 96:128], NB[:, 128:160]
        bN, wN = PS.get()
        eN = None
        for tt in range(NTT):
            eN = P.op("pe", lambda g, tt=tt, bN=bN: g.matmul(PS.t[bN][:, 0:32], lhsT=onesb[:], rhs=CT[:, tt, :], start=(tt == 0), stop=(tt == NTT - 1)),
                      waits=[ct_ev[tt], e_ones] + (wN if tt == 0 else []), sig=(tt == NTT - 1))
        e_n = P.op("dve", lambda g, bN=bN: g.tensor_copy(out=Nrep, in_=PS.t[bN][:, 0:32]), waits=[eN] + moe_w)
        PS.free(bN, [e_n])
        e_tl = P.op("dve", lambda g: g.tensor_scalar(out=TL, in0=Nrep, scalar1=0.0, scalar2=None, op0=ALU.is_gt), waits=[e_n])
        for jj in range(1, 9):
            e_tl = P.op("dve", lambda g, jj=jj: g.scalar_tensor_tensor(out=TL, in0=Nrep, scalar=128.0 * jj, in1=TL, op0=ALU.is_gt, op1=ALU.add), waits=[e_tl])
        e_inc = P.op("dve", lambda g: g.tensor_tensor_scan(out=INC, data0=ONF, data1=TL, initial=0.0, op0=ALU.mult, op1=ALU.add), waits=[e_tl, e_on])
        e_b1 = P.op("dve", lambda g: g.tensor_tensor(out=BASE, in0=INC, in1=TL, op=ALU.subtract), waits=[e_inc])
        e_base = P.op("dve", lambda g: g.tensor_scalar(out=BASE, in0=BASE, scalar1=128.0, scalar2=None, op0=ALU.mult), waits=[e_b1])
        e_c1 = P.op("dve", lambda g: g.tensor_scalar(out=CMP[:, 0:32], in0=INC, scalar1=smc(O_IOTA), scalar2=None, op0=ALU.is_le), waits=[e_inc, e_sm] + moe_w)
        e_et = P.op("dve", lambda g: g.tensor_reduce(out=CMP[:, 32:33], in_=CMP[:, 0:32], axis=AX.X, op=ALU.add), waits=[e_c1])
        e_et2 = P.op("dve", lambda g: g.tensor_scalar(out=CMP[:, 32:33], in0=CMP[:, 32:33], scalar1=32.0, scalar2=None, op0=ALU.min), waits=[e_et])
        e_de = P.op("dve", lambda g: g.tensor_scalar(out=DE, in0=identf[:, 0:48], scalar1=CMP[:, 32:33], scalar2=None, op0=ALU.mult), waits=[e_et2, e_idf])
        bE, wE = PS.get()
        eE = P.op("pe", lambda g, bE=bE: g.matmul(PS.t[bE][:, 0:48], lhsT=onesb[:], rhs=DE, start=True, stop=True), waits=[e_de] + wE, sig=True)
        e_iwf = P.op("dve", lambda g, bE=bE: g.tensor_scalar(out=IWf, in0=PS.t[bE][:, 0:48], scalar1=128.0, scalar2=smc(O_IOTA), op0=ALU.mult, op1=ALU.add), waits=[eE])
        PS.free(bE, [e_iwf])
        e_iw = P.op("dve", lambda g: g.tensor_copy(out=IW, in_=IWf), waits=[e_iwf])
        Wm.set_gate(("iw", ly), e_iw)

        sl_ev = []
        for tt in range(NTT):
            t0 = tt * 128
            ts_ = min(128, W - t0)
            R = RS[0:ts_, tt, :]
            bP, wP = PS.get()
            eP = None
            for t2 in range(tt + 1):
                lhs = onesb if t2 < tt else trib
                eP = P.op("pe", lambda g, t2=t2, tt=tt, bP=bP, lhs=lhs: g.matmul(PS.t[bP][:, 0:32], lhsT=lhs[:], rhs=CT[:, t2, :], start=(t2 == 0), stop=(t2 == tt)),
                          waits=[e_tri] + (wP if t2 == 0 else []), sig=(t2 == tt))
            S = S2[0:ts_, tt, 0:32]
            Sg = S2[0:ts_, tt, 32:40]
            tm = S2[0:ts_, tt, 40:48]
            e_s = P.op("dve", lambda g, S=S, bP=bP, ts_=ts_: g.tensor_tensor(out=S, in0=PS.t[bP][0:ts_, 0:32], in1=BASE[0:ts_, :], op=ALU.add), waits=[eP, e_base] + moe_w)
            PS.free(bP, [e_s])
            e_g = P.op("dve", lambda g, S=S, Sg=Sg, R=R: g.tensor_scalar(out=Sg, in0=S[:, 0:8], scalar1=R[:, 8:9], scalar2=None, op0=ALU.mult), waits=[e_s])
            for gi in range(1, 4):
                e_g = P.op("dve", lambda g, S=S, Sg=Sg, R=R, gi=gi: g.scalar_tensor_tensor(out=Sg, in0=S[:, 8 * gi:8 * gi + 8], scalar=R[:, 8 + gi:9 + gi], in1=Sg,
                                                                                         op0=ALU.mult, op1=ALU.add), waits=[e_g])
            e_m1 = P.op("dve", lambda g, Sg=Sg, tm=tm, R=R: g.tensor_tensor(out=tm, in0=Sg, in1=R[:, 24:32], op=ALU.mult), waits=[e_g])
            e_r1 = P.op("dve", lambda g, tm=tm, ts_=ts_, tt=tt: g.tensor_reduce(out=SLf[0:ts_, tt, 0:1], in_=tm, axis=AX.X, op=ALU.add), waits=[e_m1])
            e_m2 = P.op("dve", lambda g, Sg=Sg, tm=tm, R=R: g.tensor_tensor(out=tm, in0=Sg, in1=R[:, 40:48], op=ALU.mult), waits=[e_r1])
            e_r2 = P.op("dve", lambda g, tm=tm, ts_=ts_, tt=tt: g.tensor_reduce(out=SLf[0:ts_, tt, 1:2], in_=tm, axis=AX.X, op=ALU.add), waits=[e_m2])
            e_sl = P.op("dve", lambda g, tt=tt: g.tensor_copy(out=SL[:, tt, :], in_=SLf[:, tt, :]), waits=[e_r2, e_zsl])
            sl_ev.append(e_sl)

        XS = dr["XS"]
        YS = dr["YS"]
        sc_ev = [None, None]
        vt_rel = [list(moe_w) + st_zi, list(moe_w) + st_zi]
        last_vtr = None
        for tt in range(NTT):
            t0 = tt * 128
            ts_ = min(128, W - t0)
            pidx = [q for q in range(NPC) if q * PW < t0 + ts_ and (q + 1) * PW > t0]
            s = tt % 2
            vt = vtok[s]
            bA, wA = PS.get()
            bB, wB = PS.get()
            bfA = PS.t[bA][:, :].bitcast(BF16)
            bfB = PS.t[bB][:, :].bitcast(BF16)
            evA = evB = None
            for c in range(16):
                bk = bfA if c < 8 else bfB
                w = [e_idb] + [v_ev[c][q] for q in pidx]
                if c == 0:
                    w += wA
                if c == 8:
                    w += wB
                ev = P.op("pe", lambda g, bk=bk, c=c, t0=t0, ts_=ts_: g.transpose(bk[0:ts_, (c % 8) * 128:(c % 8) * 128 + 128], uT[:, c, t0:t0 + ts_], identb[:, :]),
                          waits=w, sig=(c in (7, 15)))
                if c == 7:
                    evA = ev
                if c == 15:
                    evB = ev
            last_vtr = evB
            e_c1 = P.op("act", lambda g, vt=vt, bfA=bfA, ts_=ts_: g.activation(out=vt[0:ts_, 0:1024], in_=bfA[0:ts_, :], func=AF.Copy), waits=[evA] + vt_rel[s])
            e_c2 = P.op("dve", lambda g, vt=vt, bfB=bfB, ts_=ts_: g.tensor_copy(out=vt[0:ts_, 1024:2048], in_=bfB[0:ts_, :]), waits=[evB] + vt_rel[s])
            PS.free(bA, [e_c1])
            PS.free(bB, [e_c2])
            e_sc = None
            for k in range(2):
                e_sc = P.dma("pool", lambda g, vt=vt, tt=tt, k=k: g.indirect_dma_start(
                    out=XS[:, :], out_offset=bass.IndirectOffsetOnAxis(ap=SL[:, tt, k:k + 1], axis=0), in_=vt[:, :], in_offset=None), f"sc{s}", waits=[e_c1, e_c2, sl_ev[tt]])
            vt_rel[s] = [e_sc]
            sc_ev[s] = e_sc
        st["u_rd"] = [last_vtr]

        wgh = dr[f"wg{ly}"][:, :]
        wuh = dr[f"wu{ly}"][:, :]
        wdh = dr[f"wd{ly}"][:, :]
        sc_all = [e for e in sc_ev if e is not None]
        xs_rel = [[], []]
        xj_rel = [list(moe_w), list(moe_w)]
        hb_rel = [list(moe_w), list(moe_w)]
        y_rel = [list(moe_w), list(moe_w)]
        ys_ev = [None, None]
        gate = ("iw", ly)
        tile_order = []
        for i_ in range(NTILE // 2):
            tile_order += [i_, NTILE - 1 - i_]
        for idx_, j in enumerate(tile_order):
            s = idx_ % 2
            xs = vtok[s]
            e_xs = P.dma("sp", lambda g, j=j, xs=xs: g.dma_start(out=xs[:, :], in_=XS[j * 128:(j + 1) * 128, :]), f"xs{s}",
                         waits=sc_all + xs_rel[s])
            bA, wA = PS.get()
            bB, wB = PS.get()
            bfA = PS.t[bA][:, :].bitcast(BF16)
            bfB = PS.t[bB][:, :].bitcast(BF16)
            evA = evB = None
            for c in range(16):
                bk = bfA if c < 8 else bfB
                w = [e_xs, e_idb]
                if c == 0:
                    w += wA
                if c == 8:
                    w += wB
                ev = P.op("pe", lambda g, bk=bk, c=c, xs=xs: g.transpose(bk[:, (c % 8) * 128:(c % 8) * 128 + 128], xs[:, c * 128:(c + 1) * 128], identb[:, :]),
                          waits=w, sig=(c in (7, 15)))
                if c == 7:
                    evA = ev
                if c == 15:
                    evB = ev
            xs_rel[s] = [evB]
            xj2 = xjT2[s]
            xj3 = xjT3[s]
            e_x1 = P.op("act", lambda g, xj2=xj2, bfA=bfA: g.activation(out=xj2[:, 0:1024], in_=bfA[:, :], func=AF.Copy), waits=[evA] + xj_rel[s])
            e_x2 = P.op("dve", lambda g, xj2=xj2, bfB=bfB: g.tensor_copy(out=xj2[:, 1024:2048], in_=bfB[:, :]), waits=[evB] + xj_rel[s])
            PS.free(bA, [e_x1])
            PS.free(bB, [e_x2])
            ug = Wm.get(("mg", ly, j), 4096, lambda off, j=j, wgh=wgh: (lambda g: g.indirect_dma_start(
                out=ring[:, off:off + 4096], out_offset=None, in_=wgh, in_offset=bass.IndirectOffsetOnAxis(ap=IW[:, j:j + 1], axis=0), bounds_check=REGS["bc"], oob_is_err=False)), gate)
            uu = Wm.get(("mu", ly, j), 4096, lambda off, j=j, wuh=wuh: (lambda g: g.indirect_dma_start(
                out=ring[:, off:off + 4096], out_offset=None, in_=wuh, in_offset=bass.IndirectOffsetOnAxis(ap=IW[:, j:j + 1], axis=0), bounds_check=REGS["bc"], oob_is_err=False)), gate)
            wg3 = rv(ug.off, 16, 256)
            wu3 = rv(uu.off, 16, 256)
            bG, wG = PS.get()
            bU, wU = PS.get()
            eG = mm_group(PS.t[bG][:, 0:256], [(xj3[:, c, :], wg3[:, c, :]) for c in range(16)], [ug.ev, e_x1, e_x2] + wG)
            eU = mm_group(PS.t[bU][:, 0:256], [(xj3[:, c, :], wu3[:, c, :]) for c in range(16)], [uu.ev] + wU)
            xj_rel[s] = [eU]
            Wm.release(ug, [eG])
            Wm.release(uu, [eU])
            hb = hbuf[s]
            sgt, ht, hTt = hb[:, 0:256], hb[:, 256:512], hb[:, 512:768]
            hT3 = hTt.rearrange("p (a b) -> p a b", b=128)
            e_sg = P.op("act", lambda g, sgt=sgt, bG=bG: g.activation(out=sgt, in_=PS.t[bG][:, 0:256], func=AF.Silu), waits=[eG] + hb_rel[s])
            PS.free(bG, [e_sg])
            e_h = P.op("dve", lambda g, sgt=sgt, ht=ht, bU=bU: g.tensor_tensor(out=ht, in0=PS.t[bU][:, 0:256], in1=sgt, op=ALU.mult), waits=[eU, e_sg] + hb_rel[s])
            PS.free(bU, [e_h])
            bH, wH = PS.get()
            bfH = PS.t[bH][:, :].bitcast(BF16)
            evH = None
            for fc in range(2):
                evH = P.op("pe", lambda g, bfH=bfH, ht=ht, fc=fc: g.transpose(bfH[:, fc * 128:fc * 128 + 128], ht[:, fc * 128:fc * 128 + 128], identb[:, :]),
                           waits=[e_h] + (wH if fc == 0 else []), sig=(fc == 1))
            e_ht = P.op("act", lambda g, hTt=hTt, bfH=bfH: g.activation(out=hTt, in_=bfH[:, 0:256], func=AF.Copy), waits=[evH] + hb_rel[s])
            PS.free(bH, [e_ht])
            ud = Wm.get(("md", ly, j), 4096, lambda off, j=j, wdh=wdh: (lambda g: g.indirect_dma_start(
                out=ring[:, off:off + 4096], out_offset=None, in_=wdh, in_offset=bass.IndirectOffsetOnAxis(ap=IW[:, j:j + 1], axis=0), bounds_check=REGS["bc"], oob_is_err=False)), gate)
            wd3 = rv(ud.off, 2, 2048)
            yt = ytile[s]
            evD = None
            evl = []
            for n in range(4):
                bD, wD = PS.get()
                evD = mm_group(PS.t[bD][:, 0:512], [(hT3[:, fc, :], wd3[:, fc, n * 512:(n + 1) * 512]) for fc in range(2)], [ud.ev, e_ht] + wD)
                if n < 2:
                    e_y = P.op("act", lambda g, yt=yt, bD=bD, n=n: g.activation(out=yt[:, n * 512:(n + 1) * 512], in_=PS.t[bD][:, 0:512], func=AF.Copy), waits=[evD] + y_rel[s])
                else:
                    e_y = P.op("dve", lambda g, yt=yt, bD=bD, n=n: g.tensor_copy(out=yt[:, n * 512:(n + 1) * 512], in_=PS.t[bD][:, 0:512]), waits=[evD] + y_rel[s])
                PS.free(bD, [e_y])
                evl.append(e_y)
            hb_rel[s] = [evD]
            Wm.release(ud, [evD])
            e_ys = P.dma("act", lambda g, j=j, yt=yt: g.dma_start(out=YS[j * 128:(j + 1) * 128, :], in_=yt[:, :]), f"ys{s}", waits=evl)
            y_rel[s] = [e_ys]
            ys_ev[s] = e_ys

        ys_all = [e for e in ys_ev if e is not None]
        pairs = [(gbA[0], gbA[1]), (gbB[0], gbB[1]), (ytile[0], ytile[1])]
        gb_rel = [[], [], []]
        tail_w = xs_rel[0] + xs_rel[1] + xj_rel[0] + xj_rel[1] + hb_rel[0] + hb_rel[1]
        last_add = None
        for tt in range(NTT):
            t0 = tt * 128
            ts_ = min(128, W - t0)
            R = RS[0:ts_, tt, :]
            pi = tt % 3
            Y1, Y2 = pairs[pi]
            Dg = DG2[pi]
            extra = tail_w if pi == 1 else []
            e_g1 = P.dma("pool", lambda g, Y1=Y1, tt=tt: g.indirect_dma_start(
                out=Y1[:, :], out_offset=None, in_=YS[:, :], in_offset=bass.IndirectOffsetOnAxis(ap=SL[:, tt, 0:1], axis=0)),
                f"gb{pi}a", waits=ys_all + gb_rel[pi] + extra)
            e_g2 = P.dma("pool", lambda g, Y2=Y2, tt=tt: g.indirect_dma_start(
                out=Y2[:, :], out_offset=None, in_=YS[:, :], in_offset=bass.IndirectOffsetOnAxis(ap=SL[:, tt, 1:2], axis=0)),
                f"gb{pi}b", waits=ys_all + gb_rel[pi] + extra)
            hb_ = DE[0:ts_, 2 * tt:2 * tt + 2]
            e_h1 = P.op("dve", lambda g, hb_=hb_, R=R: g.tensor_copy(out=hb_, in_=R[:, 50:52]), waits=[e_iw])
            e_lo = P.op("dve", lambda g, hb_=hb_, R=R: g.tensor_tensor(out=R[:, 60:62], in0=R[:, 50:52], in1=hb_, op=ALU.subtract), waits=[e_h1])
            dws = []
            for k in range(2):
                dws.append(P.op("dve", lambda g, Dg=Dg, k=k, ts_=ts_, tt=tt: g.tensor_scalar(
                    out=Dg[0:ts_, 2 * k, 0:ts_], in0=identf[0:ts_, 0:ts_], scalar1=DE[0:ts_, 2 * tt + k:2 * tt + k + 1], scalar2=None, op0=ALU.mult),
                    waits=[e_h1, e_idf] + gb_rel[pi] + tail_w))
                dws.append(P.op("dve", lambda g, Dg=Dg, k=k, ts_=ts_, R=R: g.tensor_scalar(
                    out=Dg[0:ts_, 2 * k + 1, 0:ts_], in0=identf[0:ts_, 0:ts_], scalar1=R[:, 60 + k:61 + k], scalar2=None, op0=ALU.mult),
                    waits=[e_lo, e_idf] + gb_rel[pi] + tail_w))
            bq = [PS.get() for _ in range(4)]
            evq = [None] * 4
            for c in range(16):
                q = c // 4
                cc = c % 4
                b_, w_ = bq[q]
                evq[q] = mm_group(PS.t[b_][:, cc * 128:cc * 128 + ts_],
                                  [(Y1[0:ts_, c * 128:(c + 1) * 128], Dg[0:ts_, 0, 0:ts_]), (Y1[0:ts_, c * 128:(c + 1) * 128], Dg[0:ts_, 1, 0:ts_]),
                                   (Y2[0:ts_, c * 128:(c + 1) * 128], Dg[0:ts_, 2, 0:ts_]), (Y2[0:ts_, c * 128:(c + 1) * 128], Dg[0:ts_, 3, 0:ts_])],
                                  [e_g1, e_g2] + dws + (w_ if cc == 0 else []))
            gb_rel[pi] = [evq[3]]
            for q in range(4):
                b_, w_ = bq[q]
                last_add = P.op("dve", lambda g, b_=b_, q=q, t0=t0, ts_=ts_: g.tensor_tensor(
                    out=xT[:, 4 * q:4 * q + 4, t0:t0 + ts_], in0=PS.t[b_][:, :].rearrange("p (a b) -> p a b", b=128)[:, :, 0:ts_],
                    in1=xT[:, 4 * q:4 * q + 4, t0:t0 + ts_], op=ALU.add), waits=[evq[q]])
                PS.free(b_, [last_add])
        for c in range(16):
            for p in range(NPC):
                x_ev[c][p] = last_add

        q_ev = rmsnorm(sb + O_GPLE, st["u_rd"])
        st["u_rd"] = []
        pgv = dr["ple_gate_w"][ly].rearrange("(c p) n -> p c n", p=128)
        ppv = dr["ple_proj_w"][ly]
        pps = []
        for k in range(2):
            pps.append(Wm.get(("ple_proj", ly, k), 2048,
                              lambda off, k=k, ppv=ppv: (lambda g: g.dma_start(out=ring[:, off:off + 2048], in_=ppv[k * 128:k * 128 + 128, :]))))
        evl = None
        for j in range(16):
            ug = Wm.get(("ple_gate", ly, j), 16 * 128,
                        lambda off, j=j, pgv=pgv: (lambda g: g.dma_start(out=rv(off, 16, 128), in_=pgv[:, :, j * 128:j * 128 + 128])))
            wg_ = rv(ug.off, 16, 128)
            for p in range(NPC):
                bG, w1 = PS.get()
                bP, w2 = PS.get()
                eG = mm_group(PS.t[bG][:, 0:PW], [(wg_[:, k, :], uT[:, k, pc(p)]) for k in range(16)], [ug.ev] + [q_ev[k][p] for k in range(16)] + w1)
                eP = mm_group(PS.t[bP][:, 0:PW], [(ring[:, pps[k].off + j * 128:pps[k].off + j * 128 + 128], pTl[:, k, pc(p)]) for k in range(2)],
                              [pps[0].ev, pps[1].ev, e_pl_ld] + w2)
                evl = eP
                ti, tt_, tw = T32.get()
                e_sg = P.op("act", lambda g, tt_=tt_, bG=bG: g.activation(out=tt_[:], in_=PS.t[bG][:, 0:PW], func=AF.Sigmoid), waits=[eG] + tw)
                PS.free(bG, [e_sg])
                e_t = P.op("dve", lambda g, tt_=tt_, bP=bP: g.tensor_tensor(out=tt_[:], in0=PS.t[bP][:, 0:PW], in1=tt_[:], op=ALU.mult), waits=[eP, e_sg])
                PS.free(bP, [e_t])
                e_x = P.op("dve", lambda g, tt_=tt_, j=j, p=p: g.tensor_tensor(out=xT[:, j, pc(p)], in0=tt_[:], in1=xT[:, j, pc(p)], op=ALU.add),
                           waits=[e_t, x_ev[j][p]])
                T32.free(ti, [e_x])
                x_ev[j][p] = e_x
            Wm.release(ug, [evl])
        for k in range(2):
            Wm.release(pps[k], [evl])
        st["u_rd"] = [evl]
        st["arena_rd"] = [evl]

    for _ly in range(n_layers):
        layer_body(_ly)

    fin_w = list(st["arena_rd"])
    ov = dr["outT"].rearrange("(c p) t -> p c t", p=128)
    out_evs = []
    rs_ev = [None] * NPC
    for p in range(NPC):
        b, bw = PS.get()
        bank = PS.t[b]
        last = None
        for c in range(16):
            si, sq, sw = SQ.get()
            e_sq = P.op("act", lambda g, c=c, p=p, sq=sq: g.activation(out=sq[:], in_=xT[:, c, pc(p)], func=AF.Square), waits=[x_ev[c][p]] + sw)
            last = P.op("pe", lambda g, c=c, sq=sq, bank=bank: g.matmul(bank[:, 0:PW], lhsT=onesb[:], rhs=sq[:], start=(c == 0), stop=(c == 15)),
                        waits=[e_sq] + (bw if c == 0 else []), sig=True)
            SQ.free(si, [last])
        ti, tt, tw = T32.get()
        e_rt = P.op("act", lambda g, tt=tt, bank=bank: g.activation(out=tt[:], in_=bank[:, 0:PW], func=AF.Sqrt, bias=smc(O_EPS), scale=1.0 / D),
                    waits=[last] + tw)
        PS.free(b, [e_rt])
        rs_ev[p] = P.op("dve", lambda g, tt=tt, p=p: g.reciprocal(out=rstdW[:, pc(p)], in_=tt[:]), waits=[e_rt] + st["rstd_rd"])
        T32.free(ti, [rs_ev[p]])
    ot_rel = [[], []]
    for c in range(16):
        s = c % 2
        e_o = None
        for p in range(NPC):
            e_o = P.op("dve", lambda g, c=c, p=p, s=s: g.scalar_tensor_tensor(out=outtmp[:, s, pc(p)], in0=xT[:, c, pc(p)], scalar=smc(O_GFIN + c), in1=rstdW[:, pc(p)],
                                                                             op0=ALU.mult, op1=ALU.mult), waits=[rs_ev[p], x_ev[c][p]] + fin_w + ot_rel[s])
        q = "sp" if c % 2 == 0 else "act"
        e_d = P.dma(q, lambda g, c=c, s=s: g.dma_start(out=ov[:, c, :], in_=outtmp[:, s, H:W]), f"o{c}", waits=[e_o])
        ot_rel[s] = [e_d]
        out_evs.append(e_d)
    P.op("sp", lambda g: g.nop(), waits=out_evs, sig=False)


O_IOTA = None
O_EPS = None


def build(n_layers=NL):
    global O_EPS, O_IOTA, NS
    O_EPS = O_RB + NL * 36
    O_IOTA = O_EPS + 1
    nst = O_IOTA + 1
    nc = bass.Bass("TRN2", target_bir_lowering=False)
    dr = {}

    def din(name, shape):
        dr[name] = nc.dram_tensor(name, list(shape), F32, kind="ExternalInput").ap()

    din("xT", (D, W))
    din("pT", (NL, 256, W))
    din("sm", (128, nst))
    din("rw", (NL, 128, 16 * 36))
    din("ident", (128, 128))
    din("tri", (128, 128))
    din("w_in", (NL, D, 7168))
    din("w_conv_out", (NL, 1024, D))
    din("pool_w", (NL, 4, 256, 512))
    din("w_out", (NL, D, D))
    for l_ in range(NL):
        for nm_ in ("wg", "wu", "wd"):
            dr[f"{nm_}{l_}"] = nc.dram_tensor(f"{nm_}{l_}", [4096, 4096], F32, kind="ExternalInput")
    din("ple_gate_w", (NL, D, D))
    din("ple_proj_w", (NL, 256, D))
    dr["outT"] = nc.dram_tensor("outT", [D, T], F32, kind="ExternalOutput").ap()
    dr["XS"] = nc.dram_tensor("XS", [NSLOT + 256, D], BF16, kind="Internal")
    dr["YS"] = nc.dram_tensor("YS", [NSLOT + 256, D], BF16, kind="Internal")

    tn = {"dram": dr}
    tn["xT"] = nc.alloc_sbuf_tensor("xT_sb", [128, 16, W], F32)
    tn["uT"] = nc.alloc_sbuf_tensor("uT_sb", [128, 16, W], BF16)
    tn["arena"] = nc.alloc_sbuf_tensor("arena", [128, ARENA], BF16)
    tn["ring"] = nc.alloc_sbuf_tensor("ring", [128, RING], BF16)
    tn["sm"] = nc.alloc_sbuf_tensor("sm_sb", [128, nst], F32)
    tn["identf"] = nc.alloc_sbuf_tensor("identf", [128, 128], F32)
    tn["identb"] = nc.alloc_sbuf_tensor("identb", [128, 128], BF16)
    tn["onesb"] = nc.alloc_sbuf_tensor("onesb", [128, 128], BF16)
    tn["rstdW"] = nc.alloc_sbuf_tensor("rstdW", [128, W], F32)
    tn["sqp"] = [nc.alloc_sbuf_tensor(f"sqp{i}", [128, PW], BF16) for i in range(3)]
    tn["t32"] = [nc.alloc_sbuf_tensor(f"t32_{i}", [128, PW], F32) for i in range(3)]
    tn["tb16"] = [nc.alloc_sbuf_tensor(f"tb16_{i}", [128, PW], BF16) for i in range(6)]
    tn["trib"] = nc.alloc_sbuf_tensor("trib", [128, 128], BF16)
    tn["psum"] = [nc.alloc_psum_tensor(f"ps{i}", [128, 512], F32) for i in range(8)]

    Pd = Prog(nc, dry=True)
    Wd = WMgr(Pd, None)
    emit(nc, Pd, Wd, tn, n_layers)
    P = Prog(nc, dry=False)
    Wm = WMgr(P, Wd.req)
    emit(nc, P, Wm, tn, n_layers)
    assert Wm.cur == len(Wd.req)
    P.run()
    return nc


def _prep_inputs(inp):
    f = lambda a: np.ascontiguousarray(np.asarray(a, dtype=np.float32))
    x = f(inp["x"])
    p = f(inp["p"])
    nst = O_RB + NL * 36 + 2

    def pm(v, nch):
        return v.reshape(nch, 128).T

    sm = np.zeros((128, nst), np.float32)
    for l in range(NL):
        b = l * LS
        sm[:, b + O_GMIX:b + O_GMIX + 16] = pm(f(inp["norm_mix_g"])[l], 16)
        sm[:, b + O_BGLU:b + O_BGLU + 16] = pm(f(inp["b_glu"])[l], 16)
        cw = f(inp["conv_w"])[l]
        sm[:, b + O_CONVW:b + O_CONVW + 248] = cw.reshape(31, 8, 128).transpose(2, 0, 1).reshape(128, 248)
        sm[:, b + O_CONVB:b + O_CONVB + 8] = pm(f(inp["conv_b"])[l], 8)
        sm[:, b + O_LNG:b + O_LNG + 8] = pm(f(inp["conv_ln_g"])[l], 8)
        sm[:, b + O_LNB:b + O_LNB + 8] = pm(f(inp["conv_ln_b"])[l], 8)
        sm[:, b + O_PSC:b + O_PSC + 16] = pm(f(inp["pool_scale"])[l], 16)
        sm[:, b + O_GFFN:b + O_GFFN + 16] = pm(f(inp["norm_ffn_g"])[l], 16)
        sm[:, b + O_GPLE:b + O_GPLE + 16] = pm(f(inp["norm_ple_g"])[l], 16)
        rb = np.concatenate([f(inp["router_group_b"])[l], f(inp["router_expert_b"])[l]])
        sm[:, O_RB + l * 36:O_RB + l * 36 + 36] = rb[None, :]
    sm[:, O_GFIN:O_GFIN + 16] = pm(f(inp["final_norm_g"]), 16)
    sm[:, nst - 2] = EPS
    sm[:, nst - 1] = np.arange(128, dtype=np.float32)
    rw = np.stack([np.concatenate([f(inp["router_group_w"])[l], f(inp["router_expert_w"])[l]], axis=1)
                   .reshape(16, 128, 36).transpose(1, 0, 2).reshape(128, 16 * 36) for l in range(NL)])
    shared = {
        "rw": np.ascontiguousarray(rw),
        "ident": np.eye(128, dtype=np.float32),
        "w_in": f(inp["w_in"]),
        "w_conv_out": f(inp["w_conv_out"]),
        "pool_w": f(inp["pool_w"]),
        "w_out": f(inp["w_out"]),
        "tri": np.triu(np.ones((128, 128), np.float32), 1),
        "ple_gate_w": f(inp["ple_gate_w"]),
        "ple_proj_w": f(inp["ple_proj_w"]),
    }
    wgh = f(inp["expert_w_gate"]).reshape(NL, 32, 16, 128, 256).transpose(0, 1, 3, 2, 4).reshape(NL, 4096, 4096)
    wuh = f(inp["expert_w_up"]).reshape(NL, 32, 16, 128, 256).transpose(0, 1, 3, 2, 4).reshape(NL, 4096, 4096)
    wdh = f(inp["expert_w_down"]).reshape(NL, 32, 2, 128, D).transpose(0, 1, 3, 2, 4).reshape(NL, 4096, 4096)
    for l in range(NL):
        shared[f"wg{l}"] = np.ascontiguousarray(wgh[l])
        shared[f"wu{l}"] = np.ascontiguousarray(wuh[l])
        shared[f"wd{l}"] = np.ascontiguousarray(wdh[l])
    in_maps = []
    for core in range(8):
        b, half = core // 2, core % 2
        t0 = half * T - H
        xs = np.zeros((W, D), np.float32)
        ps = np.zeros((NL, W, 256), np.float32)
        lo = max(t0, 0)
        xs[lo - t0:] = x[b, lo:t0 + W]
        ps[:, lo - t0:] = p[:, b, lo:t0 + W]
        smc = sm.copy()
        smc[:, O_HM] = float(half)
        for gi, wz in enumerate(POOL_WIN):
            pos = half * T + np.arange(16)
            smc[:, O_INVC + gi * 16:O_INVC + gi * 16 + 16] = (1.0 / np.minimum(pos + 1, wz)).astype(np.float32)[None, :]
        m = dict(shared)
        m["xT"] = np.ascontiguousarray(xs.T)
        m["pT"] = np.ascontiguousarray(ps.transpose(0, 2, 1))
        m["sm"] = smc
        in_maps.append(m)
    return in_maps


_NC_CACHE = {}


def kernel(**inputs):
    if "nc" not in _NC_CACHE:
        _NC_CACHE["nc"] = build(NL)
    nc = _NC_CACHE["nc"]
    in_maps = _prep_inputs(inputs)
    res = run_bass_kernel_spmd(nc, in_maps, core_ids=list(range(8)))
    out = np.zeros((4, 2 * T, D), np.float32)
    for core in range(8):
        b, half = core // 2, core % 2
        out[b, half * T:(half + 1) * T, :] = res.results[core]["outT"].T
    return out
```

```python
import numpy as np
import ml_dtypes
import concourse.bass as bass
import concourse.mybir as mybir
from concourse.bass_utils import run_bass_kernel_spmd

F32 = mybir.dt.float32
BF16 = mybir.dt.bfloat16
AF = mybir.ActivationFunctionType
ALU = mybir.AluOpType
AX = mybir.AxisListType

NL = 2
D = 2048
T = 1024
H = 62
W = T + H
NPC = 3
PW = W // NPC
KCONV = 31
EPS = 1e-6
NTT = 9
POOL_WIN = (2, 4, 8, 16)

O_GMIX, O_BGLU, O_CONVW, O_CONVB, O_LNG, O_LNB, O_PSC, O_GFFN, O_GPLE = 0, 16, 32, 280, 288, 296, 304, 320, 336
LS = 352
O_GFIN = NL * LS
O_HM = O_GFIN + 16
O_INVC = O_HM + 1
O_RB = O_INVC + 64
NS = O_RB + NL * 36

A_ACTA = 0
A_ACTB = A_ACTA + 8 * (30 + W)
A_MRG = A_ACTB + 8 * (16 + W)
A_DG = A_MRG + 4 * W
A_PTMP = A_DG + 8 * 128
ARENA = A_PTMP + 2 * (16 + W)
RING = 18432
M_VTOK = 7168
M_XJT = M_VTOK + 4096
M_H = M_XJT + 4096
M_Y = M_H + 1536
NTILE = 48
NSLOT = NTILE * 128
ENG = ["pe", "act", "dve", "pool", "sp"]
REGS = {}


class Prog:
    def __init__(self, nc, dry):
        self.nc = nc
        self.dry = dry
        self.q = {e: [] for e in ENG}
        self.sem = {}
        self.cnt = {}
        self.nsem = 0
        self.dma_sems = {}
        self.regs = {}
        if not dry:
            for e in ENG:
                self._new_sem(e)

    def _new_sem(self, e):
        self.nsem += 1
        self.sem[e] = self.nc.alloc_semaphore(f"s_{e}_{self.nsem}")
        self.cnt[e] = 0

    def op(self, e, fn, waits=(), sig=True):
        if self.dry:
            return None
        ev = None
        if sig:
            if self.cnt[e] >= 20000:
                self._new_sem(e)
            self.cnt[e] += 1
            ev = (self.sem[e], self.cnt[e])
        self.q[e].append((fn, tuple(w for w in waits if w is not None), ev, 1))
        return ev

    def dma(self, e, fn, sem_name, waits=()):
        if self.dry:
            return None
        if sem_name not in self.dma_sems:
            self.dma_sems[sem_name] = [self.nc.alloc_semaphore(f"d_{sem_name}"), 0]
        s = self.dma_sems[sem_name]
        s[1] += 16
        ev = (s[0], s[1])
        self.q[e].append((fn, tuple(w for w in waits if w is not None), ev, 16))
        return ev

    def run(self):
        nc = self.nc
        with nc.Block() as block:
            def mk(e):
                def body(eng):
                    seen = {}
                    if e == "pool":
                        r = eng.alloc_register("bc4095")
                        eng.reg_mov(r, 4095)
                        REGS["bc"] = r
                    for fn, waits, ev, amt in self.q[e]:
                        for (s, v) in waits:
                            k = id(s)
                            if seen.get(k, 0) >= v:
                                continue
                            seen[k] = v
                            eng.wait_ge(s, v)
                        ins = fn(eng)
                        if ev is not None:
                            ins.then_inc(ev[0], amt)
                return body
            block.tensor(mk("pe"))
            block.scalar(mk("act"))
            block.vector(mk("dve"))
            block.gpsimd(mk("pool"))
            block.sync(mk("sp"))


class Banks:
    def __init__(self, tensors):
        self.t = tensors
        self.n = len(tensors)
        self.rel = [[] for _ in tensors]
        self.busy = [False] * self.n
        self.i = 0

    def _next(self):
        for _ in range(self.n):
            b = self.i
            self.i = (self.i + 1) % self.n
            if not self.busy[b]:
                self.busy[b] = True
                return b
        raise RuntimeError("no free slot")

    def get(self):
        b = self._next()
        return b, list(self.rel[b])

    def free(self, b, evs):
        self.rel[b] = [e for e in evs if e is not None]
        self.busy[b] = False


class Temps(Banks):
    def get(self):
        b = self._next()
        return b, self.t[b], list(self.rel[b])


class Unit:
    def __init__(self, off, ev, rec):
        self.off = off
        self.ev = ev
        self.rec = rec


class WMgr:
    NSEM = 24
    LOOK = 12

    def __init__(self, P, order):
        self.P = P
        self.order = order
        self.req = []
        self.cur = 0
        self.next_load = 0
        self.loaded = {}
        self.live = []
        self.off = 0
        self.gates = {}

    def set_gate(self, tag, ev):
        self.gates[tag] = ev

    def _alloc(self, n):
        off = self.off
        travelled = 0
        while True:
            if off + n > RING:
                travelled += RING - off
                off = 0
            s, e = off, off + n
            blockers = [a for a in self.live if a["s"] < e and s < a["e"] and a["rel"] is None]
            if not blockers:
                break
            nxt = max(a["e"] for a in blockers)
            travelled += nxt - off
            off = nxt
            if travelled >= RING:
                return None
        waits = []
        keep = []
        for a in self.live:
            if a["s"] < e and s < a["e"]:
                waits += a["rel"]
            else:
                keep.append(a)
        rec = {"s": s, "e": e, "rel": None}
        keep.append(rec)
        assert len(keep) < self.NSEM - 2
        self.live = keep
        self.off = e
        return rec, waits

    def _pump(self, upto):
        while self.next_load <= min(upto, len(self.order) - 1):
            key, n, loader, gate = self.order[self.next_load]
            if gate is not None and gate not in self.gates:
                break
            r = self._alloc(n)
            if r is None:
                break
            rec, waits = r
            if gate is not None:
                waits = waits + [self.gates[gate]]
            i = self.next_load
            ev = self.P.dma("pool", loader(rec["s"]), f"w{i % self.NSEM}", waits=waits)
            self.loaded[i] = Unit(rec["s"], ev, rec)
            self.next_load += 1

    def get(self, key, n, loader, gate=None):
        if self.P.dry:
            self.req.append((key, n, loader, gate))
            return Unit(0, None, None)
        i = self.cur
        self.cur += 1
        assert self.order[i][0] == key, (self.order[i][0], key)
        self._pump(i + self.LOOK)
        assert i in self.loaded, f"ring too small for unit {key}"
        return self.loaded.pop(i)

    def release(self, unit, evs):
        if self.P.dry:
            return
        unit.rec["rel"] = [e for e in evs if e is not None]


def emit(nc, P, Wm, tn, n_layers):
    dr = tn["dram"]
    xT, uT, arena, ring, sm = tn["xT"], tn["uT"], tn["arena"], tn["ring"], tn["sm"]
    identf, identb, onesb, rstdW = tn["identf"], tn["identb"], tn["onesb"], tn["rstdW"]
    PS = Banks(tn["psum"])
    SQ = Temps(tn["sqp"])
    T32 = Temps(tn["t32"])
    TB = Temps(tn["tb16"])
    trib = tn["trib"]

    def pc(p):
        return slice(p * PW, (p + 1) * PW)

    def av(off, n, b):
        return arena[:, off:off + n].rearrange("p (a b) -> p a b", b=b)

    def avf(off_bf, n_f32, b):
        return arena[:, off_bf:off_bf + 2 * n_f32].bitcast(F32).rearrange("p (a b) -> p a b", b=b)

    actA = av(A_ACTA, 8 * (30 + W), 30 + W)
    actB = av(A_ACTB, 8 * (16 + W), 16 + W)
    mrg = av(A_MRG, 4 * W, W)
    dg = av(A_DG, 8 * 128, 128)
    ptmp = av(A_PTMP, 2 * (16 + W), 16 + W)
    I32 = mybir.dt.int32
    o = 0
    rwf = avf(o, 16 * 36, 36); o += 2 * 16 * 36
    LG = avf(o, NTT * 36, 36); o += 2 * NTT * 36
    RS = avf(o, NTT * 64, 64); o += 2 * NTT * 64
    CT = av(o, NTT * 32, 32); o += NTT * 32
    NB = arena[:, o:o + 320].bitcast(F32); o += 320
    S2 = avf(o, NTT * 48, 48); o += 2 * NTT * 48
    SLf = avf(o, NTT * 2, 2); o += 2 * NTT * 2
    SL = arena[:, o:o + 2 * NTT * 2].bitcast(I32).rearrange("p (a b) -> p a b", b=2); o += 2 * NTT * 2
    IWf = arena[:, o:o + 96].bitcast(F32); o += 96
    IW = arena[:, o:o + 96].bitcast(I32); o += 96
    DE = arena[:, o:o + 48]; o += 48
    CMP = arena[:, o:o + 68].bitcast(F32); o += 68
    pTl = av(o, 2 * W, W); o += 2 * W
    assert o <= M_VTOK, o
    vtok = [arena[:, M_VTOK + i * 2048:M_VTOK + (i + 1) * 2048] for i in range(2)]
    xjT2 = [arena[:, M_XJT + i * 2048:M_XJT + (i + 1) * 2048] for i in range(2)]
    xjT3 = [t.rearrange("p (a b) -> p a b", b=128) for t in xjT2]
    hbuf = [arena[:, M_H + i * 768:M_H + (i + 1) * 768] for i in range(2)]
    ytile = [arena[:, M_Y + i * 2048:M_Y + (i + 1) * 2048] for i in range(2)]
    gbA = [arena[:, M_Y + 4096 + i * 2048:M_Y + 4096 + (i + 1) * 2048] for i in range(2)]
    gbB = [arena[:, M_VTOK + i * 2048:M_VTOK + (i + 1) * 2048] for i in range(2)]
    DG2 = [arena[:, M_XJT + i * 512:M_XJT + (i + 1) * 512].rearrange("p (a b) -> p a b", b=128) for i in range(3)]
    assert M_Y + 8192 <= ARENA
    outtmp = avf(A_ACTA, 2 * W, W)

    def rv(off, a, b):
        return ring[:, off:off + a * b].rearrange("p (a b) -> p a b", b=b)

    def smc(col, n=1):
        return sm[:, col:col + n]

    st = {}

    e_sm = P.dma("sp", lambda g: g.dma_start(out=sm[:], in_=dr["sm"]), "sm")
    e_idf = P.dma("sp", lambda g: g.dma_start(out=identf[:], in_=dr["ident"]), "idf")
    e_idb = P.dma("pool", lambda g: g.dma_start(out=identb[:], in_=dr["ident"]), "idb")
    xv = dr["xT"].rearrange("(c p) w -> p c w", p=128)
    x_ev = [[None] * NPC for _ in range(16)]
    for c in range(16):
        q = "sp" if c % 2 == 0 else "act"
        e = P.dma(q, lambda g, c=c: g.dma_start(out=xT[:, c, :], in_=xv[:, c, :]), f"x{c}")
        for p in range(NPC):
            x_ev[c][p] = e
    e_ones = P.op("dve", lambda g: g.memset(onesb[:], 1.0))
    e_tri = P.dma("pool", lambda g: g.dma_start(out=trib[:], in_=dr["tri"]), "tri")
    st_zi = []
    st["u_rd"] = []
    st["arena_rd"] = []

    def mm_group(out_ap, pairs, waits):
        n = len(pairs)
        ev = None
        for i, (l, r) in enumerate(pairs):
            ev = P.op("pe", lambda g, l=l, r=r, i=i: g.matmul(out_ap, lhsT=l, rhs=r, start=(i == 0), stop=(i == n - 1)),
                      waits=(waits if i == 0 else ()), sig=(i == n - 1))
        return ev

    def rmsnorm(gcol, dst_waits):
        u_ev = [[None] * NPC for _ in range(16)]
        for p in range(NPC):
            b, bw = PS.get()
            bank = PS.t[b]
            last = None
            for c in range(16):
                si, sq, sw = SQ.get()
                e_sq = P.op("act", lambda g, c=c, p=p, sq=sq: g.activation(out=sq[:], in_=xT[:, c, pc(p)], func=AF.Square),
                            waits=[x_ev[c][p]] + sw)
                last = P.op("pe", lambda g, c=c, sq=sq, bank=bank: g.matmul(bank[:, 0:PW], lhsT=onesb[:], rhs=sq[:], start=(c == 0), stop=(c == 15)),
                            waits=[e_sq, e_ones] + (bw if c == 0 else []), sig=True)
                SQ.free(si, [last])
            ti, tt, tw = T32.get()
            e_rt = P.op("act", lambda g, tt=tt, bank=bank: g.activation(out=tt[:], in_=bank[:, 0:PW], func=AF.Sqrt, bias=smc(O_EPS), scale=1.0 / D),
                        waits=[last, e_sm] + tw + st.get("rstd_rd", []))
            PS.free(b, [e_rt])
            e_rs = P.op("dve", lambda g, tt=tt, p=p: g.reciprocal(out=rstdW[:, pc(p)], in_=tt[:]), waits=[e_rt] + st.get("rstd_rd", []))
            T32.free(ti, [e_rs])
            for c in range(16):
                u_ev[c][p] = P.op("dve", lambda g, c=c, p=p: g.scalar_tensor_tensor(
                    out=uT[:, c, pc(p)], in0=xT[:, c, pc(p)], scalar=smc(gcol + c), in1=rstdW[:, pc(p)],
                    op0=ALU.mult, op1=ALU.mult), waits=[e_rs, x_ev[c][p]] + dst_waits)
        st["rstd_rd"] = [u_ev[15][NPC - 1]]
        return u_ev

    def layer_body(ly):
        sb = ly * LS
        u_ev = rmsnorm(sb + O_GMIX, st["u_rd"])
        st["u_rd"] = []
        w_in_v = dr["w_in"][ly].rearrange("(c p) n -> p c n", p=128)

        def win_unit(col0, ly=ly, w_in_v=w_in_v):
            def loader(off):
                return lambda g: g.dma_start(out=rv(off, 16, 128), in_=w_in_v[:, :, col0:col0 + 128])
            return Wm.get(("w_in", ly, col0), 16 * 128, loader)

        a_ev = [[None] * NPC for _ in range(8)]
        aw = list(st["arena_rd"])
        e_padA = P.op("dve", lambda g: g.memset(actA[:, :, 0:30], 0.0), waits=aw)
        e_padB = P.op("dve", lambda g: g.memset(actB[:, :, 0:16], 0.0), waits=aw)
        e_padT = P.op("dve", lambda g: g.memset(ptmp[:, :, 0:16], 0.0), waits=aw)
        st["pads"] = [e_padA, e_padB, e_padT]
        first_arena_waits = aw + st["pads"]
        for j in range(8):
            u1 = win_unit(j * 128)
            u2 = win_unit(1024 + j * 128)
            w1 = rv(u1.off, 16, 128)
            w2 = rv(u2.off, 16, 128)
            evl = None
            for p in range(NPC):
                bA, wA = PS.get()
                bB, wB = PS.get()
                eA = mm_group(PS.t[bA][:, 0:PW], [(w1[:, k, :], uT[:, k, pc(p)]) for k in range(16)],
                              [u1.ev] + [u_ev[k][p] for k in range(16)] + wA)
                eB = mm_group(PS.t[bB][:, 0:PW], [(w2[:, k, :], uT[:, k, pc(p)]) for k in range(16)],
                              [u2.ev] + wB)
                evl = eB
                si, sg, sw = TB.get()
                e_sg = P.op("act", lambda g, sg=sg, bB=bB, j=j: g.activation(out=sg[:], in_=PS.t[bB][:, 0:PW], func=AF.Sigmoid, bias=smc(sb + O_BGLU + 8 + j)),
                            waits=[eB, e_sm] + sw)
                PS.free(bB, [e_sg])
                e_a = P.op("dve", lambda g, sg=sg, bA=bA, j=j, p=p: g.scalar_tensor_tensor(
                    out=actA[:, j, 30 + p * PW:30 + (p + 1) * PW], in0=PS.t[bA][:, 0:PW], scalar=smc(sb + O_BGLU + j),
                    in1=sg[:], op0=ALU.add, op1=ALU.mult), waits=[eA, e_sg] + first_arena_waits)
                PS.free(bA, [e_a])
                TB.free(si, [e_a])
                if p == 0:
                    e_a = P.op("dve", lambda g, j=j: g.tensor_scalar(out=actA[:, j, 30:30 + H], in0=actA[:, j, 30:30 + H],
                                                                    scalar1=smc(O_HM), scalar2=None, op0=ALU.mult), waits=[e_a])
                a_ev[j][p] = e_a
            Wm.release(u1, [evl])
            Wm.release(u2, [evl])
        if ly == 0:
            zi = []
            for r_ in range((NSLOT + 256) // 128):
                zi.append(P.dma("sp", lambda g, r_=r_: g.dma_start(out=dr["XS"][r_ * 128:(r_ + 1) * 128, :], in_=dr["zsrc"][:, :]), "zi",
                                waits=[a_ev[7][NPC - 1]]))
            for r_ in range(NSLOT // 128, (NSLOT + 256) // 128):
                zi.append(P.dma("sp", lambda g, r_=r_: g.dma_start(out=dr["YS"][r_ * 128:(r_ + 1) * 128, :], in_=dr["zsrc"][:, :]), "zi"))
            st_zi.append(zi[-1])
        p_ev = [[None] * NPC for _ in range(8)]
        for j in range(8):
            u1 = win_unit(2048 + j * 128)
            w1 = rv(u1.off, 16, 128)
            evl = None
            for p in range(NPC):
                bA, wA = PS.get()
                eA = mm_group(PS.t[bA][:, 0:PW], [(w1[:, k, :], uT[:, k, pc(p)]) for k in range(16)], [u1.ev] + wA)
                evl = eA
                e_p = P.op("act", lambda g, bA=bA, j=j, p=p: g.activation(out=actB[:, j, 16 + p * PW:16 + (p + 1) * PW], in_=PS.t[bA][:, 0:PW], func=AF.Copy),
                           waits=[eA] + first_arena_waits)
                PS.free(bA, [e_p])
                if p == 0:
                    e_p = P.op("dve", lambda g, j=j: g.tensor_scalar(out=actB[:, j, 16:16 + H], in0=actB[:, j, 16:16 + H],
                                                                    scalar1=smc(O_HM), scalar2=None, op0=ALU.mult), waits=[e_p])
                p_ev[j][p] = e_p
            Wm.release(u1, [evl])

        dg_rel = [[] for _ in range(8)]
        dgi = 0
        cv_ev = [[None] * NPC for _ in range(8)]
        for c in range(8):
            banks = []
            for p in (2, 1, 0):
                banks.append((p,) + PS.get())
            last_mm = {}
            for k in range(KCONV):
                slot = dgi % 8
                dgi += 1
                e_dg = P.op("dve", lambda g, slot=slot, k=k, c=c: g.tensor_scalar(
                    out=dg[:, slot, :], in0=identb[:], scalar1=smc(sb + O_CONVW + k * 8 + c), scalar2=None, op0=ALU.mult),
                    waits=[e_idb, e_sm] + dg_rel[slot] + (first_arena_waits if c == 0 else []))
                ev = None
                for (p, b, bw) in banks:
                    w = [e_dg]
                    if k == 0:
                        w += bw + [a_ev[c][q] for q in range(NPC)]
                    ev = P.op("pe", lambda g, slot=slot, k=k, c=c, p=p, b=b: g.matmul(
                        PS.t[b][:, 0:PW], lhsT=dg[:, slot, :], rhs=actA[:, c, p * PW + k:p * PW + k + PW],
                        start=(k == 0), stop=(k == KCONV - 1)), waits=w, sig=True)
                    last_mm[p] = ev
                dg_rel[slot] = [ev]
            for (p, b, bw) in banks:
                e_cv = P.op("act", lambda g, c=c, p=p, b=b: g.activation(out=actA[:, c, 30 + p * PW:30 + (p + 1) * PW], in_=PS.t[b][:, 0:PW],
                                                                         func=AF.Identity, bias=smc(sb + O_CONVB + c)),
                            waits=[last_mm[p], last_mm[0]])
                PS.free(b, [e_cv])
                cv_ev[c][p] = e_cv

        pl_ev = [None] * 8
        pt_rel = []
        for c in range(8):
            gidx = c // 2
            nst = gidx + 1
            src = actB[:, c, :]
            cur = src
            evs = [p_ev[c][q] for q in range(NPC)]
            e_prev = None
            for s in range(nst):
                sh = 1 << s
                dst = ptmp[:, s % 2, :]
                e_prev = P.op("dve", lambda g, cur=cur, dst=dst, sh=sh: g.tensor_tensor(
                    out=dst[:, 16:16 + W], in0=cur[:, 16:16 + W], in1=cur[:, 16 - sh:16 - sh + W], op=ALU.add),
                    waits=evs + pt_rel + ([e_prev] if e_prev is not None else []) + st["pads"])
                cur = dst
                evs = []
            wsz = POOL_WIN[gidx]
            fi, fx, fw = T32.get()
            e_f1 = P.op("dve", lambda g, cur=cur, fx=fx, gidx=gidx: g.tensor_tensor(
                out=fx[:, 0:16], in0=cur[:, 16 + H:16 + H + 16], in1=sm[:, O_INVC + gidx * 16:O_INVC + gidx * 16 + 16], op=ALU.mult),
                waits=[e_prev, e_sm] + fw)
            e_f2 = P.op("dve", lambda g, fx=fx, src=src: g.tensor_tensor(
                out=fx[:, 0:16], in0=fx[:, 0:16], in1=src[:, 16 + H:16 + H + 16], op=ALU.subtract), waits=[e_f1])
            e_pl = P.op("dve", lambda g, cur=cur, src=src, wsz=wsz: g.scalar_tensor_tensor(
                out=src[:, 16:16 + W], in0=cur[:, 16:16 + W], scalar=1.0 / wsz, in1=src[:, 16:16 + W],
                op0=ALU.mult, op1=ALU.subtract), waits=[e_f2])
            e_pl = P.op("dve", lambda g, fx=fx, src=src: g.tensor_copy(out=src[:, 16 + H:16 + H + 16], in_=fx[:, 0:16]), waits=[e_pl])
            T32.free(fi, [e_pl])
            pt_rel = [e_pl]
            pl_ev[c] = e_pl

        c_ev = [[None] * NPC for _ in range(8)]
        ln_rel = []
        for p in range(NPC):
            bS, wS = PS.get()
            bQ, wQ = PS.get()
            eS = eQ = None
            for c in range(8):
                si, sq, sw = SQ.get()
                e_sq = P.op("act", lambda g, c=c, p=p, sq=sq: g.activation(out=sq[:], in_=actA[:, c, 30 + p * PW:30 + (p + 1) * PW], func=AF.Square),
                            waits=[cv_ev[c][p]] + sw)
                eS = P.op("pe", lambda g, c=c, p=p, bS=bS: g.matmul(PS.t[bS][:, 0:PW], lhsT=onesb[:], rhs=actA[:, c, 30 + p * PW:30 + (p + 1) * PW],
                                                                    start=(c == 0), stop=(c == 7)), waits=[cv_ev[c][p]] + (wS if c == 0 else []), sig=True)
                eQ = P.op("pe", lambda g, c=c, sq=sq, bQ=bQ: g.matmul(PS.t[bQ][:, 0:PW], lhsT=onesb[:], rhs=sq[:], start=(c == 0), stop=(c == 7)),
                          waits=[e_sq] + (wQ if c == 0 else []), sig=True)
                SQ.free(si, [eQ])
            mean = rstdW[:, 0:PW]
            rstd = rstdW[:, PW:2 * PW]
            lw = ln_rel + st["rstd_rd"]
            e_mean = P.op("dve", lambda g, bS=bS: g.tensor_scalar(out=mean, in0=PS.t[bS][:, 0:PW], scalar1=1.0 / 1024, scalar2=None, op0=ALU.mult),
                          waits=[eS] + lw)
            PS.free(bS, [e_mean])
            e_msq = P.op("dve", lambda g: g.tensor_tensor(out=rstd, in0=mean, in1=mean, op=ALU.mult), waits=[e_mean] + lw)
            e_var = P.op("dve", lambda g, bQ=bQ: g.scalar_tensor_tensor(out=rstd, in0=PS.t[bQ][:, 0:PW], scalar=1.0 / 1024, in1=rstd,
                                                                        op0=ALU.mult, op1=ALU.subtract), waits=[eQ, e_msq])
            PS.free(bQ, [e_var])
            e_sd = P.op("act", lambda g: g.activation(out=rstd, in_=rstd, func=AF.Sqrt, bias=smc(O_EPS), scale=1.0), waits=[e_var])
            e_rstd = P.op("dve", lambda g: g.reciprocal(out=rstd, in_=rstd), waits=[e_sd])
            e2 = None
            for c in range(8):
                ti, tt, tw = T32.get()
                sl = actA[:, c, 30 + p * PW:30 + (p + 1) * PW]
                e1 = P.op("dve", lambda g, sl=sl, tt=tt: g.tensor_tensor(out=tt[:], in0=sl, in1=mean, op=ALU.subtract),
                          waits=[e_rstd, cv_ev[c][p]] + tw)
                e2 = P.op("dve", lambda g, tt=tt: g.tensor_tensor(out=tt[:], in0=tt[:], in1=rstd, op=ALU.mult), waits=[e1])
                e3 = P.op("act", lambda g, sl=sl, tt=tt, c=c: g.activation(out=sl, in_=tt[:], func=AF.Silu, bias=smc(sb + O_LNB + c), scale=smc(sb + O_LNG + c)),
                          waits=[e2])
                T32.free(ti, [e3])
                c_ev[c][p] = e3
            ln_rel = [e2]
        st["rstd_rd"] = st["rstd_rd"] + ln_rel

        cov = dr["w_conv_out"][ly].rearrange("(c p) n -> p c n", p=128)
        wov = dr["w_out"][ly].rearrange("(c p) n -> p c n", p=128)
        mrg_rel = []
        for kg in range(4):
            pu = Wm.get(("pool_w", ly, kg), 2 * 512,
                        lambda off, kg=kg, ly=ly: (lambda g: g.dma_start(out=rv(off, 2, 512), in_=dr["pool_w"][ly, kg].rearrange("(c p) n -> p c n", p=128))))
            pw_ = rv(pu.off, 2, 512)
            m_ev = [[None] * NPC for _ in range(4)]
            last_pe = None
            for jl in range(4):
                j = kg * 4 + jl
                uga = win_unit(3072 + j * 128)
                ugb = win_unit(5120 + j * 128)
                if jl % 2 == 0:
                    cu = Wm.get(("conv_out", ly, j), 8 * 256,
                                lambda off, j=j, cov=cov: (lambda g: g.dma_start(out=rv(off, 8, 256), in_=cov[:, :, j * 128:j * 128 + 256])))
                    cw = rv(cu.off, 8, 256)
                wga = rv(uga.off, 16, 128)
                wgb = rv(ugb.off, 16, 128)
                for p in range(NPC):
                    bGA, w1 = PS.get()
                    bGB, w2 = PS.get()
                    bA, w3 = PS.get()
                    bB, w4 = PS.get()
                    eGA = mm_group(PS.t[bGA][:, 0:PW], [(wga[:, k, :], uT[:, k, pc(p)]) for k in range(16)], [uga.ev] + w1)
                    eGB = mm_group(PS.t[bGB][:, 0:PW], [(wgb[:, k, :], uT[:, k, pc(p)]) for k in range(16)], [ugb.ev] + w2)
                    eA = mm_group(PS.t[bA][:, 0:PW], [(cw[:, k, (jl % 2) * 128:(jl % 2) * 128 + 128], actA[:, k, 30 + p * PW:30 + (p + 1) * PW]) for k in range(8)],
                                  [cu.ev] + [c_ev[k][p] for k in range(8)] + w3)
                    eB = mm_group(PS.t[bB][:, 0:PW], [(pw_[:, k, jl * 128:jl * 128 + 128], actB[:, 2 * kg + k, 16 + p * PW:16 + (p + 1) * PW]) for k in range(2)],
                                  [pu.ev, pl_ev[2 * kg], pl_ev[2 * kg + 1]] + w4)
                    last_pe = eB
                    s1i, sga, sw1 = TB.get()
                    s2i, sgb, sw2 = TB.get()
                    e_sa = P.op("act", lambda g, sga=sga, bGA=bGA: g.activation(out=sga[:], in_=PS.t[bGA][:, 0:PW], func=AF.Sigmoid), waits=[eGA] + sw1)
                    PS.free(bGA, [e_sa])
                    e_sb = P.op("act", lambda g, sgb=sgb, bGB=bGB: g.activation(out=sgb[:], in_=PS.t[bGB][:, 0:PW], func=AF.Sigmoid), waits=[eGB] + sw2)
                    PS.free(bGB, [e_sb])
                    m1i, m1, mw1 = T32.get()
                    e_m1 = P.op("dve", lambda g, m1=m1, bA=bA, sga=sga: g.tensor_tensor(out=m1[:], in0=PS.t[bA][:, 0:PW], in1=sga[:], op=ALU.mult),
                                waits=[eA, e_sa] + mw1)
                    PS.free(bA, [e_m1])
                    TB.free(s1i, [e_m1])
                    e_m2 = P.op("dve", lambda g, sgb=sgb, bB=bB, j=j: g.scalar_tensor_tensor(out=sgb[:], in0=PS.t[bB][:, 0:PW], scalar=smc(sb + O_PSC + j),
                                                                                             in1=sgb[:], op0=ALU.mult, op1=ALU.mult), waits=[eB, e_sb])
                    PS.free(bB, [e_m2])
                    e_m = P.op("dve", lambda g, m1=m1, sgb=sgb, jl=jl, p=p: g.tensor_tensor(out=mrg[:, jl, pc(p)], in0=m1[:], in1=sgb[:], op=ALU.add),
                               waits=[e_m2, e_m1] + mrg_rel)
                    T32.free(m1i, [e_m])
                    TB.free(s2i, [e_m])
                    m_ev[jl][p] = e_m
                Wm.release(uga, [last_pe])
                Wm.release(ugb, [last_pe])
                if jl % 2 == 1:
                    Wm.release(cu, [last_pe])
            Wm.release(pu, [last_pe])
            if kg == 3:
                st["u_rd"] = [last_pe]
            for qd in range(4):
                wu_ = Wm.get(("w_out", ly, kg, qd), 4 * 512,
                             lambda off, kg=kg, qd=qd, wov=wov: (lambda g: g.dma_start(out=rv(off, 4, 512), in_=wov[:, kg * 4:kg * 4 + 4, qd * 512:qd * 512 + 512])))
                ww = rv(wu_.off, 4, 512)
                evl = None
                for jo4 in range(4):
                    jo = qd * 4 + jo4
                    for p in range(NPC):
                        b, bw = PS.get()
                        e_mm = mm_group(PS.t[b][:, 0:PW], [(ww[:, k, jo4 * 128:jo4 * 128 + 128], mrg[:, k, pc(p)]) for k in range(4)],
                                        [wu_.ev] + [m_ev[k][p] for k in range(4)] + bw)
                        evl = e_mm
                        e_x = P.op("dve", lambda g, jo=jo, p=p, b=b: g.tensor_tensor(out=xT[:, jo, pc(p)], in0=PS.t[b][:, 0:PW], in1=xT[:, jo, pc(p)], op=ALU.add),
                                   waits=[e_mm, x_ev[jo][p]])
                        PS.free(b, [e_x])
                        x_ev[jo][p] = e_x
                Wm.release(wu_, [evl])
                mrg_rel = [evl]
        st["arena_rd"] = list(mrg_rel)

        v_ev = rmsnorm(sb + O_GFFN, st["u_rd"])
        st["u_rd"] = []
        moe_w = list(st["arena_rd"])
        e_rw = P.dma("sp", lambda g, ly=ly: g.dma_start(out=rwf[:], in_=dr["rw"][ly].rearrange("p (a b) -> p a b", b=36)), "rw", waits=moe_w)
        e_pl_ld = P.dma("pool", lambda g, ly=ly: g.dma_start(out=pTl[:], in_=dr["pT"][ly].rearrange("(c p) w -> p c w", p=128)), "pTl", waits=moe_w)
        e_rws = None
        for c in range(16):
            e_rws = P.op("dve", lambda g, c=c: g.tensor_scalar(out=rwf[:, c, :], in0=rwf[:, c, :], scalar1=smc(sb + O_GFFN + c), scalar2=None, op0=ALU.mult),
                         waits=[e_rw, e_sm])
        e_z1 = P.op("dve", lambda g: g.memset(LG[:], 0.0), waits=moe_w)
        e_z2 = P.op("dve", lambda g: g.memset(CT[:], 0.0), waits=moe_w)
        e_zsl0 = P.op("dve", lambda g: g.memset(SLf[:, :, 0:1], float(NSLOT)), waits=moe_w)
        e_zsl = P.op("dve", lambda g: g.memset(SLf[:, :, 1:2], float(NSLOT + 128)), waits=moe_w + [e_zsl0])
        e_zsl = P.op("dve", lambda g: g.tensor_scalar(out=SLf[:], in0=SLf[:], scalar1=smc(O_IOTA), scalar2=None, op0=ALU.add), waits=[e_zsl, e_sm])
        e_on = P.op("dve", lambda g: g.memset(NB[:, 128:160], 1.0), waits=moe_w)
        e_z3 = P.op("dve", lambda g: g.memset(RS[:], 0.0), waits=moe_w)
        ct_ev = []
        for tt in range(NTT):
            t0 = tt * 128
            ts_ = min(128, W - t0)
            pidx = [q for q in range(NPC) if q * PW < t0 + ts_ and (q + 1) * PW > t0]
            b, bw = PS.get()
            bank = PS.t[b]
            e_lg = mm_group(bank[0:ts_, 0:36], [(xT[:, c, t0:t0 + ts_], rwf[:, c, :]) for c in range(16)],
                            [e_rws] + [x_ev[c][q] for c in range(16) for q in pidx] + bw)
            e_rt = P.op("pe", lambda g, bank=bank, t0=t0, ts_=ts_: g.matmul(bank[0:ts_, 64:66], lhsT=rstdW[:, t0:t0 + ts_], rhs=identf[:, 0:2], start=True, stop=True),
                        waits=st["rstd_rd"] + [e_idf], sig=True)
            R = RS[0:ts_, tt, :]
            lg = LG[0:ts_, tt, :]

            def rcol(i, n=1, R=R):
                return R[:, i:i + n]
            e0 = P.op("dve", lambda g, bank=bank, ts_=ts_, R=R: g.tensor_copy(out=R[:, 0:1], in_=bank[0:ts_, 64:65]), waits=[e_rt, e_z3])
            e1 = P.op("dve", lambda g, bank=bank, ts_=ts_, lg=lg, R=R, ly=ly: g.scalar_tensor_tensor(
                out=lg, in0=bank[0:ts_, 0:36], scalar=R[:, 0:1], in1=sm[0:ts_, O_RB + ly * 36:O_RB + ly * 36 + 36], op0=ALU.mult, op1=ALU.add),
                waits=[e_lg, e0, e_z1, e_sm])
            PS.free(b, [e1])
            e2 = P.op("dve", lambda g, lg=lg, R=R: g.tensor_reduce(out=R[:, 1:2], in_=lg[:, 0:4], axis=AX.X, op=ALU.max, negate=True), waits=[e1])
            e3 = P.op("act", lambda g, lg=lg, R=R: g.activation(out=R[:, 4:8], in_=lg[:, 0:4], func=AF.Exp, bias=R[:, 1:2], accum_out=R[:, 2:3]), waits=[e2])
            e4 = P.op("dve", lambda g, lg=lg, R=R: g.tensor_scalar(out=R[:, 8:12], in0=lg[:, 0:4], scalar1=R[:, 1:2], scalar2=0.0, op0=ALU.add, op1=ALU.is_equal), waits=[e2])
            e5 = P.op("dve", lambda g, R=R: g.reciprocal(out=R[:, 3:4], in_=R[:, 2:3]), waits=[e3])
            e6 = P.op("dve", lambda g, lg=lg, R=R: g.tensor_scalar(out=R[:, 16:24], in0=lg[:, 4:12], scalar1=R[:, 8:9], scalar2=None, op0=ALU.mult), waits=[e4])
            for gi in range(1, 4):
                e6 = P.op("dve", lambda g, lg=lg, R=R, gi=gi: g.scalar_tensor_tensor(out=R[:, 16:24], in0=lg[:, 4 + 8 * gi:12 + 8 * gi], scalar=R[:, 8 + gi:9 + gi],
                                                                                  in1=R[:, 16:24], op0=ALU.mult, op1=ALU.add), waits=[e6])
            e7 = P.op("dve", lambda g, R=R: g.tensor_reduce(out=R[:, 12:13], in_=R[:, 16:24], axis=AX.X, op=ALU.max), waits=[e6])
            e8 = P.op("dve", lambda g, R=R: g.tensor_scalar(out=R[:, 24:32], in0=R[:, 16:24], scalar1=R[:, 12:13], scalar2=None, op0=ALU.is_equal), waits=[e7])
            e9 = P.op("dve", lambda g, R=R: g.scalar_tensor_tensor(out=R[:, 32:40], in0=R[:, 24:32], scalar=-1e30, in1=R[:, 16:24], op0=ALU.mult, op1=ALU.add), waits=[e8])
            e10 = P.op("dve", lambda g, R=R: g.tensor_reduce(out=R[:, 13:14], in_=R[:, 32:40], axis=AX.X, op=ALU.max), waits=[e9])
            e11 = P.op("dve", lambda g, R=R: g.tensor_scalar(out=R[:, 40:48], in0=R[:, 32:40], scalar1=R[:, 13:14], scalar2=None, op0=ALU.is_equal), waits=[e10])
            e12 = P.op("dve", lambda g, R=R: g.tensor_tensor(out=R[:, 14:15], in0=R[:, 13:14], in1=R[:, 12:13], op=ALU.subtract), waits=[e10])
            e13 = P.op("act", lambda g, R=R: g.activation(out=R[:, 15:16], in_=R[:, 14:15], func=AF.Exp), waits=[e12])
            e14 = P.op("dve", lambda g, R=R: g.tensor_scalar(out=R[:, 48:49], in0=R[:, 15:16], scalar1=1.0, scalar2=None, op0=ALU.add), waits=[e13])
            e15 = P.op("dve", lambda g, R=R: g.reciprocal(out=R[:, 49:50], in_=R[:, 48:49]), waits=[e14])
            e16 = P.op("dve", lambda g, R=R: g.tensor_tensor(out=R[:, 50:51], in0=R[:, 49:50], in1=R[:, 3:4], op=ALU.mult), waits=[e15, e5])
            e17 = P.op("dve", lambda g, R=R: g.tensor_tensor(out=R[:, 51:52], in0=R[:, 50:51], in1=R[:, 15:16], op=ALU.mult), waits=[e16])
            e18 = P.op("dve", lambda g, R=R: g.tensor_tensor(out=R[:, 52:60], in0=R[:, 24:32], in1=R[:, 40:48], op=ALU.add), waits=[e8, e11, e17])
            e20 = None
            for gi in range(4):
                e20 = P.op("dve", lambda g, R=R, gi=gi, ts_=ts_, tt=tt: g.tensor_scalar(out=CT[0:ts_, tt, gi * 8:gi * 8 + 8], in0=R[:, 52:60], scalar1=R[:, 8 + gi:9 + gi],
                                                                                   scalar2=None, op0=ALU.mult), waits=[e18, e4, e_z2])
            ct_ev.append(e20)
            st["rstd_rd"] = st["rstd_rd"] + [e_rt]

        Nrep, TL, INC, BASE, ONF = NB[:, 0:32], NB[:, 32:64], NB[:, 64:96], NB[:, 96:128], NB[:, 128:160]
        bN, wN = PS.get()
        eN = None
        for tt in range(NTT):
            eN = P.op("pe", lambda g, tt=tt, bN=bN: g.matmul(PS.t[bN][:, 0:32], lhsT=onesb[:], rhs=CT[:, tt, :], start=(tt == 0), stop=(tt == NTT - 1)),
                      waits=[ct_ev[tt], e_ones] + (wN if tt == 0 else []), sig=(tt == NTT - 1))
        e_n = P.op("dve", lambda g, bN=bN: g.tensor_copy(out=Nrep, in_=PS.t[bN][:, 0:32]), waits=[eN] + moe_w)
        PS.free(bN, [e_n])
        e_tl = P.op("dve", lambda g: g.tensor_scalar(out=TL, in0=Nrep, scalar1=0.0, scalar2=None, op0=ALU.is_gt), waits=[e_n])
        for jj in range(1, 9):
            e_tl = P.op("dve", lambda g, jj=jj: g.scalar_tensor_tensor(out=TL, in0=Nrep, scalar=128.0 * jj, in1=TL, op0=ALU.is_gt, op1=ALU.add), waits=[e_tl])
        e_inc = P.op("dve", lambda g: g.tensor_tensor_scan(out=INC, data0=ONF, data1=TL, initial=0.0, op0=ALU.mult, op1=ALU.add), waits=[e_tl, e_on])
        e_b1 = P.op("dve", lambda g: g.tensor_tensor(out=BASE, in0=INC, in1=TL, op=ALU.subtract), waits=[e_inc])
        e_base = P.op("dve", lambda g: g.tensor_scalar(out=BASE, in0=BASE, scalar1=128.0, scalar2=None, op0=ALU.mult), waits=[e_b1])
        e_c1 = P.op("dve", lambda g: g.tensor_scalar(out=CMP[:, 0:32], in0=INC, scalar1=smc(O_IOTA), scalar2=None, op0=ALU.is_le), waits=[e_inc, e_sm] + moe_w)
        e_et = P.op("dve", lambda g: g.tensor_reduce(out=CMP[:, 32:33], in_=CMP[:, 0:32], axis=AX.X, op=ALU.add), waits=[e_c1])
        e_et2 = P.op("dve", lambda g: g.tensor_scalar(out=CMP[:, 32:33], in0=CMP[:, 32:33], scalar1=32.0, scalar2=None, op0=ALU.min), waits=[e_et])
        e_de = P.op("dve", lambda g: g.tensor_scalar(out=DE, in0=identf[:, 0:48], scalar1=CMP[:, 32:33], scalar2=None, op0=ALU.mult), waits=[e_et2, e_idf])
        bE, wE = PS.get()
        eE = P.op("pe", lambda g, bE=bE: g.matmul(PS.t[bE][:, 0:48], lhsT=onesb[:], rhs=DE, start=True, stop=True), waits=[e_de] + wE, sig=True)
        e_iwf = P.op("dve", lambda g, bE=bE: g.tensor_scalar(out=IWf, in0=PS.t[bE][:, 0:48], scalar1=128.0, scalar2=smc(O_IOTA), op0=ALU.mult, op1=ALU.add), waits=[eE])
        PS.free(bE, [e_iwf])
        e_iw = P.op("dve", lambda g: g.tensor_copy(out=IW, in_=IWf), waits=[e_iwf])
        Wm.set_gate(("iw", ly), e_iw)

        sl_ev = []
        for tt in range(NTT):
            t0 = tt * 128
            ts_ = min(128, W - t0)
            R = RS[0:ts_, tt, :]
            bP, wP = PS.get()
            eP = None
            for t2 in range(tt + 1):
                lhs = onesb if t2 < tt else trib
                eP = P.op("pe", lambda g, t2=t2, tt=tt, bP=bP, lhs=lhs: g.matmul(PS.t[bP][:, 0:32], lhsT=lhs[:], rhs=CT[:, t2, :], start=(t2 == 0), stop=(t2 == tt)),
                          waits=[e_tri] + (wP if t2 == 0 else []), sig=(t2 == tt))
            S = S2[0:ts_, tt, 0:32]
            Sg = S2[0:ts_, tt, 32:40]
            tm = S2[0:ts_, tt, 40:48]
            e_s = P.op("dve", lambda g, S=S, bP=bP, ts_=ts_: g.tensor_tensor(out=S, in0=PS.t[bP][0:ts_, 0:32], in1=BASE[0:ts_, :], op=ALU.add), waits=[eP, e_base] + moe_w)
            PS.free(bP, [e_s])
            e_g = P.op("dve", lambda g, S=S, Sg=Sg, R=R: g.tensor_scalar(out=Sg, in0=S[:, 0:8], scalar1=R[:, 8:9], scalar2=None, op0=ALU.mult), waits=[e_s])
            for gi in range(1, 4):
                e_g = P.op("dve", lambda g, S=S, Sg=Sg, R=R, gi=gi: g.scalar_tensor_tensor(out=Sg, in0=S[:, 8 * gi:8 * gi + 8], scalar=R[:, 8 + gi:9 + gi], in1=Sg,
                                                                                         op0=ALU.mult, op1=ALU.add), waits=[e_g])
            e_m1 = P.op("dve", lambda g, Sg=Sg, tm=tm, R=R: g.tensor_tensor(out=tm, in0=Sg, in1=R[:, 24:32], op=ALU.mult), waits=[e_g])
            e_r1 = P.op("dve", lambda g, tm=tm, ts_=ts_, tt=tt: g.tensor_reduce(out=SLf[0:ts_, tt, 0:1], in_=tm, axis=AX.X, op=ALU.add), waits=[e_m1])
            e_m2 = P.op("dve", lambda g, Sg=Sg, tm=tm, R=R: g.tensor_tensor(out=tm, in0=Sg, in1=R[:, 40:48], op=ALU.mult), waits=[e_r1])
            e_r2 = P.op("dve", lambda g, tm=tm, ts_=ts_, tt=tt: g.tensor_reduce(out=SLf[0:ts_, tt, 1:2], in_=tm, axis=AX.X, op=ALU.add), waits=[e_m2])
            e_sl = P.op("dve", lambda g, tt=tt: g.tensor_copy(out=SL[:, tt, :], in_=SLf[:, tt, :]), waits=[e_r2, e_zsl])
            sl_ev.append(e_sl)

        XS = dr["XS"]
        YS = dr["YS"]
        sc_ev = [None, None]
        vt_rel = [list(moe_w) + st_zi, list(moe_w) + st_zi]
        last_vtr = None
        for tt in range(NTT):
            t0 = tt * 128
            ts_ = min(128, W - t0)
            pidx = [q for q in range(NPC) if q * PW < t0 + ts_ and (q + 1) * PW > t0]
            s = tt % 2
            vt = vtok[s]
            bA, wA = PS.get()
            bB, wB = PS.get()
            bfA = PS.t[bA][:, :].bitcast(BF16)
            bfB = PS.t[bB][:, :].bitcast(BF16)
            evA = evB = None
            for c in range(16):
                bk = bfA if c < 8 else bfB
                w = [e_idb] + [v_ev[c][q] for q in pidx]
                if c == 0:
                    w += wA
                if c == 8:
                    w += wB
                ev = P.op("pe", lambda g, bk=bk, c=c, t0=t0, ts_=ts_: g.transpose(bk[0:ts_, (c % 8) * 128:(c % 8) * 128 + 128], uT[:, c, t0:t0 + ts_], identb[:, :]),
                          waits=w, sig=(c in (7, 15)))
                if c == 7:
                    evA = ev
                if c == 15:
                    evB = ev
            last_vtr = evB
            e_c1 = P.op("act", lambda g, vt=vt, bfA=bfA, ts_=ts_: g.activation(out=vt[0:ts_, 0:1024], in_=bfA[0:ts_, :], func=AF.Copy), waits=[evA] + vt_rel[s])
            e_c2 = P.op("dve", lambda g, vt=vt, bfB=bfB, ts_=ts_: g.tensor_copy(out=vt[0:ts_, 1024:2048], in_=bfB[0:ts_, :]), waits=[evB] + vt_rel[s])
            PS.free(bA, [e_c1])
            PS.free(bB, [e_c2])
            e_sc = None
            for k in range(2):
                e_sc = P.dma("pool", lambda g, vt=vt, tt=tt, k=k: g.indirect_dma_start(
                    out=XS[:, :], out_offset=bass.IndirectOffsetOnAxis(ap=SL[:, tt, k:k + 1], axis=0), in_=vt[:, :], in_offset=None), f"sc{s}", waits=[e_c1, e_c2, sl_ev[tt]])
            vt_rel[s] = [e_sc]
            sc_ev[s] = e_sc
        st["u_rd"] = [last_vtr]

        wgh = dr[f"wg{ly}"][:, :]
        wuh = dr[f"wu{ly}"][:, :]
        wdh = dr[f"wd{ly}"][:, :]
        sc_all = [e for e in sc_ev if e is not None]
        xs_rel = [[], []]
        xj_rel = [list(moe_w), list(moe_w)]
        hb_rel = [list(moe_w), list(moe_w)]
        y_rel = [list(moe_w), list(moe_w)]
        ys_ev = [None, None]
        gate = ("iw", ly)
        tile_order = []
        for i_ in range(NTILE // 2):
            tile_order += [i_, NTILE - 1 - i_]
        for idx_, j in enumerate(tile_order):
            s = idx_ % 2
            xs = vtok[s]
            e_xs = P.dma("sp", lambda g, j=j, xs=xs: g.dma_start(out=xs[:, :], in_=XS[j * 128:(j + 1) * 128, :]), f"xs{s}",
                         waits=sc_all + xs_rel[s])
            bA, wA = PS.get()
            bB, wB = PS.get()
            bfA = PS.t[bA][:, :].bitcast(BF16)
            bfB = PS.t[bB][:, :].bitcast(BF16)
            evA = evB = None
            for c in range(16):
                bk = bfA if c < 8 else bfB
                w = [e_xs, e_idb]
                if c == 0:
                    w += wA
                if c == 8:
                    w += wB
                ev = P.op("pe", lambda g, bk=bk, c=c, xs=xs: g.transpose(bk[:, (c % 8) * 128:(c % 8) * 128 + 128], xs[:, c * 128:(c + 1) * 128], identb[:, :]),
                          waits=w, sig=(c in (7, 15)))
                if c == 7:
                    evA = ev
                if c == 15:
                    evB = ev
            xs_rel[s] = [evB]
            xj2 = xjT2[s]
            xj3 = xjT3[s]
            e_x1 = P.op("act", lambda g, xj2=xj2, bfA=bfA: g.activation(out=xj2[:, 0:1024], in_=bfA[:, :], func=AF.Copy), waits=[evA] + xj_rel[s])
            e_x2 = P.op("dve", lambda g, xj2=xj2, bfB=bfB: g.tensor_copy(out=xj2[:, 1024:2048], in_=bfB[:, :]), waits=[evB] + xj_rel[s])
            PS.free(bA, [e_x1])
            PS.free(bB, [e_x2])
            ug = Wm.get(("mg", ly, j), 4096, lambda off, j=j, wgh=wgh: (lambda g: g.indirect_dma_start(
                out=ring[:, off:off + 4096], out_offset=None, in_=wgh, in_offset=bass.IndirectOffsetOnAxis(ap=IW[:, j:j + 1], axis=0), bounds_check=REGS["bc"], oob_is_err=False)), gate)
            uu = Wm.get(("mu", ly, j), 4096, lambda off, j=j, wuh=wuh: (lambda g: g.indirect_dma_start(
                out=ring[:, off:off + 4096], out_offset=None, in_=wuh, in_offset=bass.IndirectOffsetOnAxis(ap=IW[:, j:j + 1], axis=0), bounds_check=REGS["bc"], oob_is_err=False)), gate)
            wg3 = rv(ug.off, 16, 256)
            wu3 = rv(uu.off, 16, 256)
            bG, wG = PS.get()
            bU, wU = PS.get()
            eG = mm_group(PS.t[bG][:, 0:256], [(xj3[:, c, :], wg3[:, c, :]) for c in range(16)], [ug.ev, e_x1, e_x2] + wG)
            eU = mm_group(PS.t[bU][:, 0:256], [(xj3[:, c, :], wu3[:, c, :]) for c in range(16)], [uu.ev] + wU)
            xj_rel[s] = [eU]
            Wm.release(ug, [eG])
            Wm.release(uu, [eU])
            hb = hbuf[s]
            sgt, ht, hTt = hb[:, 0:256], hb[:, 256:512], hb[:, 512:768]
            hT3 = hTt.rearrange("p (a b) -> p a b", b=128)
            e_sg = P.op("act", lambda g, sgt=sgt, bG=bG: g.activation(out=sgt, in_=PS.t[bG][:, 0:256], func=AF.Silu), waits=[eG] + hb_rel[s])
            PS.free(bG, [e_sg])
            e_h = P.op("dve", lambda g, sgt=sgt, ht=ht, bU=bU: g.tensor_tensor(out=ht, in0=PS.t[bU][:, 0:256], in1=sgt, op=ALU.mult), waits=[eU, e_sg] + hb_rel[s])
            PS.free(bU, [e_h])
            bH, wH = PS.get()
            bfH = PS.t[bH][:, :].bitcast(BF16)
            evH = None
            for fc in range(2):
                evH = P.op("pe", lambda g, bfH=bfH, ht=ht, fc=fc: g.transpose(bfH[:, fc * 128:fc * 128 + 128], ht[:, fc * 128:fc * 128 + 128], identb[:, :]),
                           waits=[e_h] + (wH if fc == 0 else []), sig=(fc == 1))
            e_ht = P.op("act", lambda g, hTt=hTt, bfH=bfH: g.activation(out=hTt, in_=bfH[:, 0:256], func=AF.Copy), waits=[evH] + hb_rel[s])
            PS.free(bH, [e_ht])
            ud = Wm.get(("md", ly, j), 4096, lambda off, j=j, wdh=wdh: (lambda g: g.indirect_dma_start(
                out=ring[:, off:off + 4096], out_offset=None, in_=wdh, in_offset=bass.IndirectOffsetOnAxis(ap=IW[:, j:j + 1], axis=0), bounds_check=REGS["bc"], oob_is_err=False)), gate)
            wd3 = rv(ud.off, 2, 2048)
            yt = ytile[s]
            evD = None
            evl = []
            for n in range(4):
                bD, wD = PS.get()
                evD = mm_group(PS.t[bD][:, 0:512], [(hT3[:, fc, :], wd3[:, fc, n * 512:(n + 1) * 512]) for fc in range(2)], [ud.ev, e_ht] + wD)
                if n < 2:
                    e_y = P.op("act", lambda g, yt=yt, bD=bD, n=n: g.activation(out=yt[:, n * 512:(n + 1) * 512], in_=PS.t[bD][:, 0:512], func=AF.Copy), waits=[evD] + y_rel[s])
                else:
                    e_y = P.op("dve", lambda g, yt=yt, bD=bD, n=n: g.tensor_copy(out=yt[:, n * 512:(n + 1) * 512], in_=PS.t[bD][:, 0:512]), waits=[evD] + y_rel[s])
                PS.free(bD, [e_y])
                evl.append(e_y)
            hb_rel[s] = [evD]
            Wm.release(ud, [evD])
            e_ys = P.dma("act", lambda g, j=j, yt=yt: g.dma_start(out=YS[j * 128:(j + 1) * 128, :], in_=yt[:, :]), f"ys{s}", waits=evl)
            y_rel[s] = [e_ys]
            ys_ev[s] = e_ys

        ys_all = [e for e in ys_ev if e is not None]
        pairs = [(gbA[0], gbA[1]), (gbB[0], gbB[1]), (ytile[0], ytile[1])]
        gb_rel = [[], [], []]
        tail_w = xs_rel[0] + xs_rel[1] + xj_rel[0] + xj_rel[1] + hb_rel[0] + hb_rel[1]
        last_add = None
        for tt in range(NTT):
            t0 = tt * 128
            ts_ = min(128, W - t0)
            R = RS[0:ts_, tt, :]
            pi = tt % 3
            Y1, Y2 = pairs[pi]
            Dg = DG2[pi]
            extra = tail_w if pi == 1 else []
            e_g1 = P.dma("pool", lambda g, Y1=Y1, tt=tt: g.indirect_dma_start(
                out=Y1[:, :], out_offset=None, in_=YS[:, :], in_offset=bass.IndirectOffsetOnAxis(ap=SL[:, tt, 0:1], axis=0)),
                f"gb{pi}a", waits=ys_all + gb_rel[pi] + extra)
            e_g2 = P.dma("pool", lambda g, Y2=Y2, tt=tt: g.indirect_dma_start(
                out=Y2[:, :], out_offset=None, in_=YS[:, :], in_offset=bass.IndirectOffsetOnAxis(ap=SL[:, tt, 1:2], axis=0)),
                f"gb{pi}b", waits=ys_all + gb_rel[pi] + extra)
            hb_ = DE[0:ts_, 2 * tt:2 * tt + 2]
            e_h1 = P.op("dve", lambda g, hb_=hb_, R=R: g.tensor_copy(out=hb_, in_=R[:, 50:52]), waits=[e_iw])
            e_lo = P.op("dve", lambda g, hb_=hb_, R=R: g.tensor_tensor(out=R[:, 60:62], in0=R[:, 50:52], in1=hb_, op=ALU.subtract), waits=[e_h1])
            dws = []
            for k in range(2):
                dws.append(P.op("dve", lambda g, Dg=Dg, k=k, ts_=ts_, tt=tt: g.tensor_scalar(
                    out=Dg[0:ts_, 2 * k, 0:ts_], in0=identf[0:ts_, 0:ts_], scalar1=DE[0:ts_, 2 * tt + k:2 * tt + k + 1], scalar2=None, op0=ALU.mult),
                    waits=[e_h1, e_idf] + gb_rel[pi] + tail_w))
                dws.append(P.op("dve", lambda g, Dg=Dg, k=k, ts_=ts_, R=R: g.tensor_scalar(
                    out=Dg[0:ts_, 2 * k + 1, 0:ts_], in0=identf[0:ts_, 0:ts_], scalar1=R[:, 60 + k:61 + k], scalar2=None, op0=ALU.mult),
                    waits=[e_lo, e_idf] + gb_rel[pi] + tail_w))
            bq = [PS.get() for _ in range(4)]
            evq = [None] * 4
            for c in range(16):
                q = c // 4
                cc = c % 4
                b_, w_ = bq[q]
                evq[q] = mm_group(PS.t[b_][:, cc * 128:cc * 128 + ts_],
                                  [(Y1[0:ts_, c * 128:(c + 1) * 128], Dg[0:ts_, 0, 0:ts_]), (Y1[0:ts_, c * 128:(c + 1) * 128], Dg[0:ts_, 1, 0:ts_]),
                                   (Y2[0:ts_, c * 128:(c + 1) * 128], Dg[0:ts_, 2, 0:ts_]), (Y2[0:ts_, c * 128:(c + 1) * 128], Dg[0:ts_, 3, 0:ts_])],
                                  [e_g1, e_g2] + dws + (w_ if cc == 0 else []))
            gb_rel[pi] = [evq[3]]
            for q in range(4):
                b_, w_ = bq[q]
                last_add = P.op("dve", lambda g, b_=b_, q=q, t0=t0, ts_=ts_: g.tensor_tensor(
                    out=xT[:, 4 * q:4 * q + 4, t0:t0 + ts_], in0=PS.t[b_][:, :].rearrange("p (a b) -> p a b", b=128)[:, :, 0:ts_],
                    in1=xT[:, 4 * q:4 * q + 4, t0:t0 + ts_], op=ALU.add), waits=[evq[q]])
                PS.free(b_, [last_add])
        for c in range(16):
            for p in range(NPC):
                x_ev[c][p] = last_add

        q_ev = rmsnorm(sb + O_GPLE, st["u_rd"])
        st["u_rd"] = []
        pgv = dr["ple_gate_w"][ly].rearrange("(c p) n -> p c n", p=128)
        ppv = dr["ple_proj_w"][ly]
        pps = []
        for k in range(2):
            pps.append(Wm.get(("ple_proj", ly, k), 2048,
                              lambda off, k=k, ppv=ppv: (lambda g: g.dma_start(out=ring[:, off:off + 2048], in_=ppv[k * 128:k * 128 + 128, :]))))
        evl = None
        for j in range(16):
            ug = Wm.get(("ple_gate", ly, j), 16 * 128,
                        lambda off, j=j, pgv=pgv: (lambda g: g.dma_start(out=rv(off, 16, 128), in_=pgv[:, :, j * 128:j * 128 + 128])))
            wg_ = rv(ug.off, 16, 128)
            for p in range(NPC):
                bG, w1 = PS.get()
                bP, w2 = PS.get()
                eG = mm_group(PS.t[bG][:, 0:PW], [(wg_[:, k, :], uT[:, k, pc(p)]) for k in range(16)], [ug.ev] + [q_ev[k][p] for k in range(16)] + w1)
                eP = mm_group(PS.t[bP][:, 0:PW], [(ring[:, pps[k].off + j * 128:pps[k].off + j * 128 + 128], pTl[:, k, pc(p)]) for k in range(2)],
                              [pps[0].ev, pps[1].ev, e_pl_ld] + w2)
                evl = eP
                ti, tt_, tw = T32.get()
                e_sg = P.op("act", lambda g, tt_=tt_, bG=bG: g.activation(out=tt_[:], in_=PS.t[bG][:, 0:PW], func=AF.Sigmoid), waits=[eG] + tw)
                PS.free(bG, [e_sg])
                e_t = P.op("dve", lambda g, tt_=tt_, bP=bP: g.tensor_tensor(out=tt_[:], in0=PS.t[bP][:, 0:PW], in1=tt_[:], op=ALU.mult), waits=[eP, e_sg])
                PS.free(bP, [e_t])
                e_x = P.op("dve", lambda g, tt_=tt_, j=j, p=p: g.tensor_tensor(out=xT[:, j, pc(p)], in0=tt_[:], in1=xT[:, j, pc(p)], op=ALU.add),
                           waits=[e_t, x_ev[j][p]])
                T32.free(ti, [e_x])
                x_ev[j][p] = e_x
            Wm.release(ug, [evl])
        for k in range(2):
            Wm.release(pps[k], [evl])
        st["u_rd"] = [evl]
        st["arena_rd"] = [evl]

    for _ly in range(n_layers):
        layer_body(_ly)

    fin_w = list(st["arena_rd"])
    ov = dr["outT"].rearrange("(c p) t -> p c t", p=128)
    out_evs = []
    rs_ev = [None] * NPC
    for p in range(NPC):
        b, bw = PS.get()
        bank = PS.t[b]
        last = None
        for c in range(16):
            si, sq, sw = SQ.get()
            e_sq = P.op("act", lambda g, c=c, p=p, sq=sq: g.activation(out=sq[:], in_=xT[:, c, pc(p)], func=AF.Square), waits=[x_ev[c][p]] + sw)
            last = P.op("pe", lambda g, c=c, sq=sq, bank=bank: g.matmul(bank[:, 0:PW], lhsT=onesb[:], rhs=sq[:], start=(c == 0), stop=(c == 15)),
                        waits=[e_sq] + (bw if c == 0 else []), sig=True)
            SQ.free(si, [last])
        ti, tt, tw = T32.get()
        e_rt = P.op("act", lambda g, tt=tt, bank=bank: g.activation(out=tt[:], in_=bank[:, 0:PW], func=AF.Sqrt, bias=smc(O_EPS), scale=1.0 / D),
                    waits=[last] + tw)
        PS.free(b, [e_rt])
        rs_ev[p] = P.op("dve", lambda g, tt=tt, p=p: g.reciprocal(out=rstdW[:, pc(p)], in_=tt[:]), waits=[e_rt] + st["rstd_rd"])
        T32.free(ti, [rs_ev[p]])
    ot_rel = [[], []]
    for c in range(16):
        s = c % 2
        e_o = None
        for p in range(NPC):
            e_o = P.op("dve", lambda g, c=c, p=p, s=s: g.scalar_tensor_tensor(out=outtmp[:, s, pc(p)], in0=xT[:, c, pc(p)], scalar=smc(O_GFIN + c), in1=rstdW[:, pc(p)],
                                                                             op0=ALU.mult, op1=ALU.mult), waits=[rs_ev[p], x_ev[c][p]] + fin_w + ot_rel[s])
        q = "sp" if c % 2 == 0 else "act"
        e_d = P.dma(q, lambda g, c=c, s=s: g.dma_start(out=ov[:, c, :], in_=outtmp[:, s, H:W]), f"o{c}", waits=[e_o])
        ot_rel[s] = [e_d]
        out_evs.append(e_d)
    P.op("sp", lambda g: g.nop(), waits=out_evs, sig=False)


O_IOTA = None
O_EPS = None


def build(n_layers=NL):
    global O_EPS, O_IOTA, NS
    O_EPS = O_RB + NL * 36
    O_IOTA = O_EPS + 1
    nst = O_IOTA + 1
    nc = bass.Bass("TRN2", target_bir_lowering=False)
    dr = {}

    def din(name, shape):
        dr[name] = nc.dram_tensor(name, list(shape), F32, kind="ExternalInput").ap()

    din("xT", (D, W))
    din("pT", (NL, 256, W))
    din("sm", (128, nst))
    din("rw", (NL, 128, 16 * 36))
    din("ident", (128, 128))
    din("tri", (128, 128))
    dr["zsrc"] = nc.dram_tensor("zsrc", [128, 2048], BF16, kind="ExternalInput")
    din("w_in", (NL, D, 7168))
    din("w_conv_out", (NL, 1024, D))
    din("pool_w", (NL, 4, 256, 512))
    din("w_out", (NL, D, D))
    for l_ in range(NL):
        for nm_ in ("wg", "wu", "wd"):
            dr[f"{nm_}{l_}"] = nc.dram_tensor(f"{nm_}{l_}", [4096, 4096], F32, kind="ExternalInput")
    din("ple_gate_w", (NL, D, D))
    din("ple_proj_w", (NL, 256, D))
    dr["outT"] = nc.dram_tensor("outT", [D, T], F32, kind="ExternalOutput").ap()
    dr["XS"] = nc.dram_tensor("XS", [NSLOT + 256, D], BF16, kind="Internal")
    dr["YS"] = nc.dram_tensor("YS", [NSLOT + 256, D], BF16, kind="Internal")

    tn = {"dram": dr}
    tn["xT"] = nc.alloc_sbuf_tensor("xT_sb", [128, 16, W], F32)
    tn["uT"] = nc.alloc_sbuf_tensor("uT_sb", [128, 16, W], BF16)
    tn["arena"] = nc.alloc_sbuf_tensor("arena", [128, ARENA], BF16)
    tn["ring"] = nc.alloc_sbuf_tensor("ring", [128, RING], BF16)
    tn["sm"] = nc.alloc_sbuf_tensor("sm_sb", [128, nst], F32)
    tn["identf"] = nc.alloc_sbuf_tensor("identf", [128, 128], F32)
    tn["identb"] = nc.alloc_sbuf_tensor("identb", [128, 128], BF16)
    tn["onesb"] = nc.alloc_sbuf_tensor("onesb", [128, 128], BF16)
    tn["rstdW"] = nc.alloc_sbuf_tensor("rstdW", [128, W], F32)
    tn["sqp"] = [nc.alloc_sbuf_tensor(f"sqp{i}", [128, PW], BF16) for i in range(3)]
    tn["t32"] = [nc.alloc_sbuf_tensor(f"t32_{i}", [128, PW], F32) for i in range(3)]
    tn["tb16"] = [nc.alloc_sbuf_tensor(f"tb16_{i}", [128, PW], BF16) for i in range(6)]
    tn["trib"] = nc.alloc_sbuf_tensor("trib", [128, 128], BF16)
    tn["psum"] = [nc.alloc_psum_tensor(f"ps{i}", [128, 512], F32) for i in range(8)]

    Pd = Prog(nc, dry=True)
    Wd = WMgr(Pd, None)
    emit(nc, Pd, Wd, tn, n_layers)
    P = Prog(nc, dry=False)
    Wm = WMgr(P, Wd.req)
    emit(nc, P, Wm, tn, n_layers)
    assert Wm.cur == len(Wd.req)
    P.run()
    return nc


def _prep_inputs(inp):
    f = lambda a: np.ascontiguousarray(np.asarray(a, dtype=np.float32))
    x = f(inp["x"])
    p = f(inp["p"])
    nst = O_RB + NL * 36 + 2

    def pm(v, nch):
        return v.reshape(nch, 128).T

    sm = np.zeros((128, nst), np.float32)
    for l in range(NL):
        b = l * LS
        sm[:, b + O_GMIX:b + O_GMIX + 16] = pm(f(inp["norm_mix_g"])[l], 16)
        sm[:, b + O_BGLU:b + O_BGLU + 16] = pm(f(inp["b_glu"])[l], 16)
        cw = f(inp["conv_w"])[l]
        sm[:, b + O_CONVW:b + O_CONVW + 248] = cw.reshape(31, 8, 128).transpose(2, 0, 1).reshape(128, 248)
        sm[:, b + O_CONVB:b + O_CONVB + 8] = pm(f(inp["conv_b"])[l], 8)
        sm[:, b + O_LNG:b + O_LNG + 8] = pm(f(inp["conv_ln_g"])[l], 8)
        sm[:, b + O_LNB:b + O_LNB + 8] = pm(f(inp["conv_ln_b"])[l], 8)
        sm[:, b + O_PSC:b + O_PSC + 16] = pm(f(inp["pool_scale"])[l], 16)
        sm[:, b + O_GFFN:b + O_GFFN + 16] = pm(f(inp["norm_ffn_g"])[l], 16)
        sm[:, b + O_GPLE:b + O_GPLE + 16] = pm(f(inp["norm_ple_g"])[l], 16)
        rb = np.concatenate([f(inp["router_group_b"])[l], f(inp["router_expert_b"])[l]])
        sm[:, O_RB + l * 36:O_RB + l * 36 + 36] = rb[None, :]
    sm[:, O_GFIN:O_GFIN + 16] = pm(f(inp["final_norm_g"]), 16)
    sm[:, nst - 2] = EPS
    sm[:, nst - 1] = np.arange(128, dtype=np.float32)
    rw = np.stack([np.concatenate([f(inp["router_group_w"])[l], f(inp["router_expert_w"])[l]], axis=1)
                   .reshape(16, 128, 36).transpose(1, 0, 2).reshape(128, 16 * 36) for l in range(NL)])
    shared = {
        "rw": np.ascontiguousarray(rw),
        "ident": np.eye(128, dtype=np.float32),
        "w_in": f(inp["w_in"]),
        "w_conv_out": f(inp["w_conv_out"]),
        "pool_w": f(inp["pool_w"]),
        "w_out": f(inp["w_out"]),
        "tri": np.triu(np.ones((128, 128), np.float32), 1),
        "zsrc": np.zeros((128, 2048), dtype=ml_dtypes.bfloat16),
        "ple_gate_w": f(inp["ple_gate_w"]),
        "ple_proj_w": f(inp["ple_proj_w"]),
    }
    wgh = f(inp["expert_w_gate"]).reshape(NL, 32, 16, 128, 256).transpose(0, 1, 3, 2, 4).reshape(NL, 4096, 4096)
    wuh = f(inp["expert_w_up"]).reshape(NL, 32, 16, 128, 256).transpose(0, 1, 3, 2, 4).reshape(NL, 4096, 4096)
    wdh = f(inp["expert_w_down"]).reshape(NL, 32, 2, 128, D).transpose(0, 1, 3, 2, 4).reshape(NL, 4096, 4096)
    for l in range(NL):
        shared[f"wg{l}"] = np.ascontiguousarray(wgh[l])
        shared[f"wu{l}"] = np.ascontiguousarray(wuh[l])
        shared[f"wd{l}"] = np.ascontiguousarray(wdh[l])
    in_maps = []
    for core in range(8):
        b, half = core // 2, core % 2
        t0 = half * T - H
        xs = np.zeros((W, D), np.float32)
        ps = np.zeros((NL, W, 256), np.float32)
        lo = max(t0, 0)
        xs[lo - t0:] = x[b, lo:t0 + W]
        ps[:, lo - t0:] = p[:, b, lo:t0 + W]
        smc = sm.copy()
        smc[:, O_HM] = float(half)
        for gi, wz in enumerate(POOL_WIN):
            pos = half * T + np.arange(16)
            smc[:, O_INVC + gi * 16:O_INVC + gi * 16 + 16] = (1.0 / np.minimum(pos + 1, wz)).astype(np.float32)[None, :]
        m = dict(shared)
        m["xT"] = np.ascontiguousarray(xs.T)
        m["pT"] = np.ascontiguousarray(ps.transpose(0, 2, 1))
        m["sm"] = smc
        in_maps.append(m)
    return in_maps


_NC_CACHE = {}


def kernel(**inputs):
    if "nc" not in _NC_CACHE:
        _NC_CACHE["nc"] = build(NL)
    nc = _NC_CACHE["nc"]
    in_maps = _prep_inputs(inputs)
    res = run_bass_kernel_spmd(nc, in_maps, core_ids=list(range(8)))
    out = np.zeros((4, 2 * T, D), np.float32)
    for core in range(8):
        b, half = core // 2, core % 2
        out[b, half * T:(half + 1) * T, :] = res.results[core]["outT"].T
    return out
```

```python
import numpy as np
import concourse.bass as bass
import concourse.mybir as mybir
from concourse.bass_utils import run_bass_kernel_spmd

F32 = mybir.dt.float32
BF16 = mybir.dt.bfloat16
AF = mybir.ActivationFunctionType
ALU = mybir.AluOpType
AX = mybir.AxisListType

NL = 2
D = 2048
T = 1024
H = 62
W = T + H
NPC = 3
PW = W // NPC
KCONV = 31
EPS = 1e-6
NTT = 9
POOL_WIN = (2, 4, 8, 16)

O_GMIX, O_BGLU, O_CONVW, O_CONVB, O_LNG, O_LNB, O_PSC, O_GFFN, O_GPLE = 0, 16, 32, 280, 288, 296, 304, 320, 336
LS = 352
O_GFIN = NL * LS
O_HM = O_GFIN + 16
O_INVC = O_HM + 1
O_RB = O_INVC + 64
NS = O_RB + NL * 36

A_ACTA = 0
A_ACTB = A_ACTA + 8 * (30 + W)
A_MRG = A_ACTB + 8 * (16 + W)
A_DG = A_MRG + 4 * W
A_PTMP = A_DG + 8 * 128
ARENA = A_PTMP + 2 * (16 + W)
RING = 18432
M_VTOK = 7168
M_XJT = M_VTOK + 4096
M_H = M_XJT + 4096
M_Y = M_H + 1536
NTILE = 48
NSLOT = NTILE * 128
ENG = ["pe", "act", "dve", "pool", "sp"]
REGS = {}


class Prog:
    def __init__(self, nc, dry):
        self.nc = nc
        self.dry = dry
        self.q = {e: [] for e in ENG}
        self.sem = {}
        self.cnt = {}
        self.nsem = 0
        self.dma_sems = {}
        self.regs = {}
        if not dry:
            for e in ENG:
                self._new_sem(e)

    def _new_sem(self, e):
        self.nsem += 1
        self.sem[e] = self.nc.alloc_semaphore(f"s_{e}_{self.nsem}")
        self.cnt[e] = 0

    def op(self, e, fn, waits=(), sig=True):
        if self.dry:
            return None
        ev = None
        if sig:
            if self.cnt[e] >= 20000:
                self._new_sem(e)
            self.cnt[e] += 1
            ev = (self.sem[e], self.cnt[e])
        self.q[e].append((fn, tuple(w for w in waits if w is not None), ev, 1))
        return ev

    def dma(self, e, fn, sem_name, waits=()):
        if self.dry:
            return None
        if sem_name not in self.dma_sems:
            self.dma_sems[sem_name] = [self.nc.alloc_semaphore(f"d_{sem_name}"), 0]
        s = self.dma_sems[sem_name]
        s[1] += 16
        ev = (s[0], s[1])
        self.q[e].append((fn, tuple(w for w in waits if w is not None), ev, 16))
        return ev

    def run(self):
        nc = self.nc
        with nc.Block() as block:
            def mk(e):
                def body(eng):
                    seen = {}
                    if e == "pool":
                        r = eng.alloc_register("bc4095")
                        eng.reg_mov(r, 4095)
                        REGS["bc"] = r
                    for fn, waits, ev, amt in self.q[e]:
                        for (s, v) in waits:
                            k = id(s)
                            if seen.get(k, 0) >= v:
                                continue
                            seen[k] = v
                            eng.wait_ge(s, v)
                        ins = fn(eng)
                        if ev is not None:
                            ins.then_inc(ev[0], amt)
                return body
            block.tensor(mk("pe"))
            block.scalar(mk("act"))
            block.vector(mk("dve"))
            block.gpsimd(mk("pool"))
            block.sync(mk("sp"))


class Banks:
    def __init__(self, tensors):
        self.t = tensors
        self.n = len(tensors)
        self.rel = [[] for _ in tensors]
        self.busy = [False] * self.n
        self.i = 0

    def _next(self):
        for _ in range(self.n):
            b = self.i
            self.i = (self.i + 1) % self.n
            if not self.busy[b]:
                self.busy[b] = True
                return b
        raise RuntimeError("no free slot")

    def get(self):
        b = self._next()
        return b, list(self.rel[b])

    def free(self, b, evs):
        self.rel[b] = [e for e in evs if e is not None]
        self.busy[b] = False


class Temps(Banks):
    def get(self):
        b = self._next()
        return b, self.t[b], list(self.rel[b])


class Unit:
    def __init__(self, off, ev, rec):
        self.off = off
        self.ev = ev
        self.rec = rec


class WMgr:
    NSEM = 24
    LOOK = 12

    def __init__(self, P, order):
        self.P = P
        self.order = order
        self.req = []
        self.cur = 0
        self.next_load = 0
        self.loaded = {}
        self.live = []
        self.off = 0
        self.gates = {}

    def set_gate(self, tag, ev):
        self.gates[tag] = ev

    def _alloc(self, n):
        off = self.off
        travelled = 0
        while True:
            if off + n > RING:
                travelled += RING - off
                off = 0
            s, e = off, off + n
            blockers = [a for a in self.live if a["s"] < e and s < a["e"] and a["rel"] is None]
            if not blockers:
                break
            nxt = max(a["e"] for a in blockers)
            travelled += nxt - off
            off = nxt
            if travelled >= RING:
                return None
        waits = []
        keep = []
        for a in self.live:
            if a["s"] < e and s < a["e"]:
                waits += a["rel"]
            else:
                keep.append(a)
        rec = {"s": s, "e": e, "rel": None}
        keep.append(rec)
        assert len(keep) < self.NSEM - 2
        self.live = keep
        self.off = e
        return rec, waits

    def _pump(self, upto):
        while self.next_load <= min(upto, len(self.order) - 1):
            key, n, loader, gate = self.order[self.next_load]
            if gate is not None and gate not in self.gates:
                break
            r = self._alloc(n)
            if r is None:
                break
            rec, waits = r
            if gate is not None:
                waits = waits + [self.gates[gate]]
            i = self.next_load
            ev = self.P.dma("pool", loader(rec["s"]), f"w{i % self.NSEM}", waits=waits)
            self.loaded[i] = Unit(rec["s"], ev, rec)
            self.next_load += 1

    def get(self, key, n, loader, gate=None):
        if self.P.dry:
            self.req.append((key, n, loader, gate))
            return Unit(0, None, None)
        i = self.cur
        self.cur += 1
        assert self.order[i][0] == key, (self.order[i][0], key)
        self._pump(i + self.LOOK)
        assert i in self.loaded, f"ring too small for unit {key}"
        return self.loaded.pop(i)

    def release(self, unit, evs):
        if self.P.dry:
            return
        unit.rec["rel"] = [e for e in evs if e is not None]


def emit(nc, P, Wm, tn, n_layers):
    dr = tn["dram"]
    xT, uT, arena, ring, sm = tn["xT"], tn["uT"], tn["arena"], tn["ring"], tn["sm"]
    identf, identb, onesb, rstdW = tn["identf"], tn["identb"], tn["onesb"], tn["rstdW"]
    PS = Banks(tn["psum"])
    SQ = Temps(tn["sqp"])
    T32 = Temps(tn["t32"])
    TB = Temps(tn["tb16"])
    trib = tn["trib"]

    def pc(p):
        return slice(p * PW, (p + 1) * PW)

    def av(off, n, b):
        return arena[:, off:off + n].rearrange("p (a b) -> p a b", b=b)

    def avf(off_bf, n_f32, b):
        return arena[:, off_bf:off_bf + 2 * n_f32].bitcast(F32).rearrange("p (a b) -> p a b", b=b)

    actA = av(A_ACTA, 8 * (30 + W), 30 + W)
    actB = av(A_ACTB, 8 * (16 + W), 16 + W)
    mrg = av(A_MRG, 4 * W, W)
    dg = av(A_DG, 8 * 128, 128)
    ptmp = av(A_PTMP, 2 * (16 + W), 16 + W)
    I32 = mybir.dt.int32
    o = 0
    rwf = avf(o, 16 * 36, 36); o += 2 * 16 * 36
    LG = avf(o, NTT * 36, 36); o += 2 * NTT * 36
    RS = avf(o, NTT * 64, 64); o += 2 * NTT * 64
    CT = av(o, NTT * 32, 32); o += NTT * 32
    NB = arena[:, o:o + 320].bitcast(F32); o += 320
    S2 = avf(o, NTT * 48, 48); o += 2 * NTT * 48
    SLf = avf(o, NTT * 2, 2); o += 2 * NTT * 2
    SL = arena[:, o:o + 2 * NTT * 2].bitcast(I32).rearrange("p (a b) -> p a b", b=2); o += 2 * NTT * 2
    IWf = arena[:, o:o + 96].bitcast(F32); o += 96
    IW = arena[:, o:o + 96].bitcast(I32); o += 96
    DE = arena[:, o:o + 48]; o += 48
    CMP = arena[:, o:o + 68].bitcast(F32); o += 68
    pTl = av(o, 2 * W, W); o += 2 * W
    assert o <= M_VTOK, o
    vtok = [arena[:, M_VTOK + i * 2048:M_VTOK + (i + 1) * 2048] for i in range(2)]
    xjT2 = [arena[:, M_XJT + i * 2048:M_XJT + (i + 1) * 2048] for i in range(2)]
    xjT3 = [t.rearrange("p (a b) -> p a b", b=128) for t in xjT2]
    hbuf = [arena[:, M_H + i * 768:M_H + (i + 1) * 768] for i in range(2)]
    ytile = [arena[:, M_Y + i * 2048:M_Y + (i + 1) * 2048] for i in range(2)]
    gbA = [arena[:, M_Y + 4096 + i * 2048:M_Y + 4096 + (i + 1) * 2048] for i in range(2)]
    gbB = [arena[:, M_VTOK + i * 2048:M_VTOK + (i + 1) * 2048] for i in range(2)]
    DG2 = [arena[:, M_XJT + i * 512:M_XJT + (i + 1) * 512].rearrange("p (a b) -> p a b", b=128) for i in range(3)]
    assert M_Y + 8192 <= ARENA
    outtmp = avf(A_ACTA, 2 * W, W)

    def rv(off, a, b):
        return ring[:, off:off + a * b].rearrange("p (a b) -> p a b", b=b)

    def smc(col, n=1):
        return sm[:, col:col + n]

    st = {}

    e_sm = P.dma("sp", lambda g: g.dma_start(out=sm[:], in_=dr["sm"]), "sm")
    e_idf = P.dma("sp", lambda g: g.dma_start(out=identf[:], in_=dr["ident"]), "idf")
    e_idb = P.dma("pool", lambda g: g.dma_start(out=identb[:], in_=dr["ident"]), "idb")
    xv = dr["xT"].rearrange("(c p) w -> p c w", p=128)
    x_ev = [[None] * NPC for _ in range(16)]
    for c in range(16):
        q = "sp" if c % 2 == 0 else "act"
        e = P.dma(q, lambda g, c=c: g.dma_start(out=xT[:, c, :], in_=xv[:, c, :]), f"x{c}")
        for p in range(NPC):
            x_ev[c][p] = e
    e_ones = P.op("dve", lambda g: g.memset(onesb[:], 1.0))
    e_tri = P.dma("pool", lambda g: g.dma_start(out=trib[:], in_=dr["tri"]), "tri")
    st_zi = []
    st["u_rd"] = []
    st["arena_rd"] = []

    def mm_group(out_ap, pairs, waits):
        n = len(pairs)
        ev = None
        for i, (l, r) in enumerate(pairs):
            ev = P.op("pe", lambda g, l=l, r=r, i=i: g.matmul(out_ap, lhsT=l, rhs=r, start=(i == 0), stop=(i == n - 1)),
                      waits=(waits if i == 0 else ()), sig=(i == n - 1))
        return ev

    def rmsnorm(gcol, dst_waits):
        u_ev = [[None] * NPC for _ in range(16)]
        for p in range(NPC):
            b, bw = PS.get()
            bank = PS.t[b]
            last = None
            for c in range(16):
                si, sq, sw = SQ.get()
                e_sq = P.op("act", lambda g, c=c, p=p, sq=sq: g.activation(out=sq[:], in_=xT[:, c, pc(p)], func=AF.Square),
                            waits=[x_ev[c][p]] + sw)
                last = P.op("pe", lambda g, c=c, sq=sq, bank=bank: g.matmul(bank[:, 0:PW], lhsT=onesb[:], rhs=sq[:], start=(c == 0), stop=(c == 15)),
                            waits=[e_sq, e_ones] + (bw if c == 0 else []), sig=True)
                SQ.free(si, [last])
            ti, tt, tw = T32.get()
            e_rt = P.op("act", lambda g, tt=tt, bank=bank: g.activation(out=tt[:], in_=bank[:, 0:PW], func=AF.Sqrt, bias=smc(O_EPS), scale=1.0 / D),
                        waits=[last, e_sm] + tw + st.get("rstd_rd", []))
            PS.free(b, [e_rt])
            e_rs = P.op("dve", lambda g, tt=tt, p=p: g.reciprocal(out=rstdW[:, pc(p)], in_=tt[:]), waits=[e_rt] + st.get("rstd_rd", []))
            T32.free(ti, [e_rs])
            for c in range(16):
                u_ev[c][p] = P.op("dve", lambda g, c=c, p=p: g.scalar_tensor_tensor(
                    out=uT[:, c, pc(p)], in0=xT[:, c, pc(p)], scalar=smc(gcol + c), in1=rstdW[:, pc(p)],
                    op0=ALU.mult, op1=ALU.mult), waits=[e_rs, x_ev[c][p]] + dst_waits)
        st["rstd_rd"] = [u_ev[15][NPC - 1]]
        return u_ev

    def layer_body(ly):
        sb = ly * LS
        u_ev = rmsnorm(sb + O_GMIX, st["u_rd"])
        st["u_rd"] = []
        w_in_v = dr["w_in"][ly].rearrange("(c p) n -> p c n", p=128)

        def win_unit(col0, ly=ly, w_in_v=w_in_v):
            def loader(off):
                return lambda g: g.dma_start(out=rv(off, 16, 128), in_=w_in_v[:, :, col0:col0 + 128])
            return Wm.get(("w_in", ly, col0), 16 * 128, loader)

        a_ev = [[None] * NPC for _ in range(8)]
        aw = list(st["arena_rd"])
        e_padA = P.op("dve", lambda g: g.memset(actA[:, :, 0:30], 0.0), waits=aw)
        e_padB = P.op("dve", lambda g: g.memset(actB[:, :, 0:16], 0.0), waits=aw)
        e_padT = P.op("dve", lambda g: g.memset(ptmp[:, :, 0:16], 0.0), waits=aw)
        st["pads"] = [e_padA, e_padB, e_padT]
        first_arena_waits = aw + st["pads"]
        for j in range(8):
            u1 = win_unit(j * 128)
            u2 = win_unit(1024 + j * 128)
            w1 = rv(u1.off, 16, 128)
            w2 = rv(u2.off, 16, 128)
            evl = None
            for p in range(NPC):
                bA, wA = PS.get()
                bB, wB = PS.get()
                eA = mm_group(PS.t[bA][:, 0:PW], [(w1[:, k, :], uT[:, k, pc(p)]) for k in range(16)],
                              [u1.ev] + [u_ev[k][p] for k in range(16)] + wA)
                eB = mm_group(PS.t[bB][:, 0:PW], [(w2[:, k, :], uT[:, k, pc(p)]) for k in range(16)],
                              [u2.ev] + wB)
                evl = eB
                si, sg, sw = TB.get()
                e_sg = P.op("act", lambda g, sg=sg, bB=bB, j=j: g.activation(out=sg[:], in_=PS.t[bB][:, 0:PW], func=AF.Sigmoid, bias=smc(sb + O_BGLU + 8 + j)),
                            waits=[eB, e_sm] + sw)
                PS.free(bB, [e_sg])
                e_a = P.op("dve", lambda g, sg=sg, bA=bA, j=j, p=p: g.scalar_tensor_tensor(
                    out=actA[:, j, 30 + p * PW:30 + (p + 1) * PW], in0=PS.t[bA][:, 0:PW], scalar=smc(sb + O_BGLU + j),
                    in1=sg[:], op0=ALU.add, op1=ALU.mult), waits=[eA, e_sg] + first_arena_waits)
                PS.free(bA, [e_a])
                TB.free(si, [e_a])
                if p == 0:
                    e_a = P.op("dve", lambda g, j=j: g.tensor_scalar(out=actA[:, j, 30:30 + H], in0=actA[:, j, 30:30 + H],
                                                                    scalar1=smc(O_HM), scalar2=None, op0=ALU.mult), waits=[e_a])
                a_ev[j][p] = e_a
            Wm.release(u1, [evl])
            Wm.release(u2, [evl])
        if ly == 0:
            zi = []
            for r_ in range((NSLOT + 256) // 128):
                zi.append(P.dma("sp", lambda g, r_=r_: g.dma_start(out=dr["XS"][r_ * 128:(r_ + 1) * 128, :], in_=dr["zsrc"][:, :]), "zi",
                                waits=[a_ev[7][NPC - 1]]))
            for r_ in range(NSLOT // 128, (NSLOT + 256) // 128):
                zi.append(P.dma("sp", lambda g, r_=r_: g.dma_start(out=dr["YS"][r_ * 128:(r_ + 1) * 128, :], in_=dr["zsrc"][:, :]), "zi"))
            st_zi.append(zi[-1])
        p_ev = [[None] * NPC for _ in range(8)]
        for j in range(8):
            u1 = win_unit(2048 + j * 128)
            w1 = rv(u1.off, 16, 128)
            evl = None
            for p in range(NPC):
                bA, wA = PS.get()
                eA = mm_group(PS.t[bA][:, 0:PW], [(w1[:, k, :], uT[:, k, pc(p)]) for k in range(16)], [u1.ev] + wA)
                evl = eA
                e_p = P.op("act", lambda g, bA=bA, j=j, p=p: g.activation(out=actB[:, j, 16 + p * PW:16 + (p + 1) * PW], in_=PS.t[bA][:, 0:PW], func=AF.Copy),
                           waits=[eA] + first_arena_waits)
                PS.free(bA, [e_p])
                if p == 0:
                    e_p = P.op("dve", lambda g, j=j: g.tensor_scalar(out=actB[:, j, 16:16 + H], in0=actB[:, j, 16:16 + H],
                                                                    scalar1=smc(O_HM), scalar2=None, op0=ALU.mult), waits=[e_p])
                p_ev[j][p] = e_p
            Wm.release(u1, [evl])

        dg_rel = [[] for _ in range(8)]
        dgi = 0
        cv_ev = [[None] * NPC for _ in range(8)]
        for c in range(8):
            banks = []
            for p in (2, 1, 0):
                banks.append((p,) + PS.get())
            last_mm = {}
            for k in range(KCONV):
                slot = dgi % 8
                dgi += 1
                e_dg = P.op("dve", lambda g, slot=slot, k=k, c=c: g.tensor_scalar(
                    out=dg[:, slot, :], in0=identb[:], scalar1=smc(sb + O_CONVW + k * 8 + c), scalar2=None, op0=ALU.mult),
                    waits=[e_idb, e_sm] + dg_rel[slot] + (first_arena_waits if c == 0 else []))
                ev = None
                for (p, b, bw) in banks:
                    w = [e_dg]
                    if k == 0:
                        w += bw + [a_ev[c][q] for q in range(NPC)]
                    ev = P.op("pe", lambda g, slot=slot, k=k, c=c, p=p, b=b: g.matmul(
                        PS.t[b][:, 0:PW], lhsT=dg[:, slot, :], rhs=actA[:, c, p * PW + k:p * PW + k + PW],
                        start=(k == 0), stop=(k == KCONV - 1)), waits=w, sig=(p == 0 or k == KCONV - 1))
                    last_mm[p] = ev
                dg_rel[slot] = [ev]
            for (p, b, bw) in banks:
                e_cv = P.op("act", lambda g, c=c, p=p, b=b: g.activation(out=actA[:, c, 30 + p * PW:30 + (p + 1) * PW], in_=PS.t[b][:, 0:PW],
                                                                         func=AF.Identity, bias=smc(sb + O_CONVB + c)),
                            waits=[last_mm[p], last_mm[0]])
                PS.free(b, [e_cv])
                cv_ev[c][p] = e_cv

        pl_ev = [None] * 8
        pt_rel = []
        for c in range(8):
            gidx = c // 2
            nst = gidx + 1
            src = actB[:, c, :]
            cur = src
            evs = [p_ev[c][q] for q in range(NPC)]
            e_prev = None
            for s in range(nst):
                sh = 1 << s
                dst = ptmp[:, s % 2, :]
                e_prev = P.op("dve", lambda g, cur=cur, dst=dst, sh=sh: g.tensor_tensor(
                    out=dst[:, 16:16 + W], in0=cur[:, 16:16 + W], in1=cur[:, 16 - sh:16 - sh + W], op=ALU.add),
                    waits=evs + pt_rel + ([e_prev] if e_prev is not None else []) + st["pads"])
                cur = dst
                evs = []
            wsz = POOL_WIN[gidx]
            fi, fx, fw = T32.get()
            e_f1 = P.op("dve", lambda g, cur=cur, fx=fx, gidx=gidx: g.tensor_tensor(
                out=fx[:, 0:16], in0=cur[:, 16 + H:16 + H + 16], in1=sm[:, O_INVC + gidx * 16:O_INVC + gidx * 16 + 16], op=ALU.mult),
                waits=[e_prev, e_sm] + fw)
            e_f2 = P.op("dve", lambda g, fx=fx, src=src: g.tensor_tensor(
                out=fx[:, 0:16], in0=fx[:, 0:16], in1=src[:, 16 + H:16 + H + 16], op=ALU.subtract), waits=[e_f1])
            e_pl = P.op("dve", lambda g, cur=cur, src=src, wsz=wsz: g.scalar_tensor_tensor(
                out=src[:, 16:16 + W], in0=cur[:, 16:16 + W], scalar=1.0 / wsz, in1=src[:, 16:16 + W],
                op0=ALU.mult, op1=ALU.subtract), waits=[e_f2])
            e_pl = P.op("dve", lambda g, fx=fx, src=src: g.tensor_copy(out=src[:, 16 + H:16 + H + 16], in_=fx[:, 0:16]), waits=[e_pl])
            T32.free(fi, [e_pl])
            pt_rel = [e_pl]
            pl_ev[c] = e_pl

        c_ev = [[None] * NPC for _ in range(8)]
        ln_rel = []
        for p in range(NPC):
            bS, wS = PS.get()
            bQ, wQ = PS.get()
            eS = eQ = None
            for c in range(8):
                si, sq, sw = SQ.get()
                e_sq = P.op("act", lambda g, c=c, p=p, sq=sq: g.activation(out=sq[:], in_=actA[:, c, 30 + p * PW:30 + (p + 1) * PW], func=AF.Square),
                            waits=[cv_ev[c][p]] + sw)
                eS = P.op("pe", lambda g, c=c, p=p, bS=bS: g.matmul(PS.t[bS][:, 0:PW], lhsT=onesb[:], rhs=actA[:, c, 30 + p * PW:30 + (p + 1) * PW],
                                                                    start=(c == 0), stop=(c == 7)), waits=[cv_ev[c][p]] + (wS if c == 0 else []), sig=(c == 7))
                eQ = P.op("pe", lambda g, c=c, sq=sq, bQ=bQ: g.matmul(PS.t[bQ][:, 0:PW], lhsT=onesb[:], rhs=sq[:], start=(c == 0), stop=(c == 7)),
                          waits=[e_sq] + (wQ if c == 0 else []), sig=True)
                SQ.free(si, [eQ])
            mean = rstdW[:, 0:PW]
            rstd = rstdW[:, PW:2 * PW]
            lw = ln_rel + st["rstd_rd"]
            e_mean = P.op("dve", lambda g, bS=bS: g.tensor_scalar(out=mean, in0=PS.t[bS][:, 0:PW], scalar1=1.0 / 1024, scalar2=None, op0=ALU.mult),
                          waits=[eS] + lw)
            PS.free(bS, [e_mean])
            e_msq = P.op("dve", lambda g: g.tensor_tensor(out=rstd, in0=mean, in1=mean, op=ALU.mult), waits=[e_mean] + lw)
            e_var = P.op("dve", lambda g, bQ=bQ: g.scalar_tensor_tensor(out=rstd, in0=PS.t[bQ][:, 0:PW], scalar=1.0 / 1024, in1=rstd,
                                                                        op0=ALU.mult, op1=ALU.subtract), waits=[eQ, e_msq])
            PS.free(bQ, [e_var])
            e_sd = P.op("act", lambda g: g.activation(out=rstd, in_=rstd, func=AF.Sqrt, bias=smc(O_EPS), scale=1.0), waits=[e_var])
            e_rstd = P.op("dve", lambda g: g.reciprocal(out=rstd, in_=rstd), waits=[e_sd])
            e2 = None
            for c in range(8):
                ti, tt, tw = T32.get()
                sl = actA[:, c, 30 + p * PW:30 + (p + 1) * PW]
                e1 = P.op("dve", lambda g, sl=sl, tt=tt: g.tensor_tensor(out=tt[:], in0=sl, in1=mean, op=ALU.subtract),
                          waits=[e_rstd, cv_ev[c][p]] + tw)
                e2 = P.op("dve", lambda g, tt=tt: g.tensor_tensor(out=tt[:], in0=tt[:], in1=rstd, op=ALU.mult), waits=[e1])
                e3 = P.op("act", lambda g, sl=sl, tt=tt, c=c: g.activation(out=sl, in_=tt[:], func=AF.Silu, bias=smc(sb + O_LNB + c), scale=smc(sb + O_LNG + c)),
                          waits=[e2])
                T32.free(ti, [e3])
                c_ev[c][p] = e3
            ln_rel = [e2]
        st["rstd_rd"] = st["rstd_rd"] + ln_rel

        cov = dr["w_conv_out"][ly].rearrange("(c p) n -> p c n", p=128)
        wov = dr["w_out"][ly].rearrange("(c p) n -> p c n", p=128)
        mrg_rel = []
        for kg in range(4):
            pu = Wm.get(("pool_w", ly, kg), 2 * 512,
                        lambda off, kg=kg, ly=ly: (lambda g: g.dma_start(out=rv(off, 2, 512), in_=dr["pool_w"][ly, kg].rearrange("(c p) n -> p c n", p=128))))
            pw_ = rv(pu.off, 2, 512)
            m_ev = [[None] * NPC for _ in range(4)]
            last_pe = None
            for jl in range(4):
                j = kg * 4 + jl
                uga = win_unit(3072 + j * 128)
                ugb = win_unit(5120 + j * 128)
                if jl % 2 == 0:
                    cu = Wm.get(("conv_out", ly, j), 8 * 256,
                                lambda off, j=j, cov=cov: (lambda g: g.dma_start(out=rv(off, 8, 256), in_=cov[:, :, j * 128:j * 128 + 256])))
                    cw = rv(cu.off, 8, 256)
                wga = rv(uga.off, 16, 128)
                wgb = rv(ugb.off, 16, 128)
                for p in range(NPC):
                    bGA, w1 = PS.get()
                    bGB, w2 = PS.get()
                    bA, w3 = PS.get()
                    bB, w4 = PS.get()
                    eGA = mm_group(PS.t[bGA][:, 0:PW], [(wga[:, k, :], uT[:, k, pc(p)]) for k in range(16)], [uga.ev] + w1)
                    eGB = mm_group(PS.t[bGB][:, 0:PW], [(wgb[:, k, :], uT[:, k, pc(p)]) for k in range(16)], [ugb.ev] + w2)
                    eA = mm_group(PS.t[bA][:, 0:PW], [(cw[:, k, (jl % 2) * 128:(jl % 2) * 128 + 128], actA[:, k, 30 + p * PW:30 + (p + 1) * PW]) for k in range(8)],
                                  [cu.ev] + [c_ev[k][p] for k in range(8)] + w3)
                    eB = mm_group(PS.t[bB][:, 0:PW], [(pw_[:, k, jl * 128:jl * 128 + 128], actB[:, 2 * kg + k, 16 + p * PW:16 + (p + 1) * PW]) for k in range(2)],
                                  [pu.ev, pl_ev[2 * kg], pl_ev[2 * kg + 1]] + w4)
                    last_pe = eB
                    s1i, sga, sw1 = TB.get()
                    s2i, sgb, sw2 = TB.get()
                    e_sa = P.op("act", lambda g, sga=sga, bGA=bGA: g.activation(out=sga[:], in_=PS.t[bGA][:, 0:PW], func=AF.Sigmoid), waits=[eGA] + sw1)
                    PS.free(bGA, [e_sa])
                    e_sb = P.op("act", lambda g, sgb=sgb, bGB=bGB: g.activation(out=sgb[:], in_=PS.t[bGB][:, 0:PW], func=AF.Sigmoid), waits=[eGB] + sw2)
                    PS.free(bGB, [e_sb])
                    m1i, m1, mw1 = T32.get()
                    e_m1 = P.op("dve", lambda g, m1=m1, bA=bA, sga=sga: g.tensor_tensor(out=m1[:], in0=PS.t[bA][:, 0:PW], in1=sga[:], op=ALU.mult),
                                waits=[eA, e_sa] + mw1)
                    PS.free(bA, [e_m1])
                    TB.free(s1i, [e_m1])
                    e_m2 = P.op("dve", lambda g, sgb=sgb, bB=bB, j=j: g.scalar_tensor_tensor(out=sgb[:], in0=PS.t[bB][:, 0:PW], scalar=smc(sb + O_PSC + j),
                                                                                             in1=sgb[:], op0=ALU.mult, op1=ALU.mult), waits=[eB, e_sb])
                    PS.free(bB, [e_m2])
                    e_m = P.op("dve", lambda g, m1=m1, sgb=sgb, jl=jl, p=p: g.tensor_tensor(out=mrg[:, jl, pc(p)], in0=m1[:], in1=sgb[:], op=ALU.add),
                               waits=[e_m2, e_m1] + mrg_rel)
                    T32.free(m1i, [e_m])
                    TB.free(s2i, [e_m])
                    m_ev[jl][p] = e_m
                Wm.release(uga, [last_pe])
                Wm.release(ugb, [last_pe])
                if jl % 2 == 1:
                    Wm.release(cu, [last_pe])
            Wm.release(pu, [last_pe])
            if kg == 3:
                st["u_rd"] = [last_pe]
            for qd in range(4):
                wu_ = Wm.get(("w_out", ly, kg, qd), 4 * 512,
                             lambda off, kg=kg, qd=qd, wov=wov: (lambda g: g.dma_start(out=rv(off, 4, 512), in_=wov[:, kg * 4:kg * 4 + 4, qd * 512:qd * 512 + 512])))
                ww = rv(wu_.off, 4, 512)
                evl = None
                for jo4 in range(4):
                    jo = qd * 4 + jo4
                    for p in range(NPC):
                        b, bw = PS.get()
                        e_mm = mm_group(PS.t[b][:, 0:PW], [(ww[:, k, jo4 * 128:jo4 * 128 + 128], mrg[:, k, pc(p)]) for k in range(4)],
                                        [wu_.ev] + [m_ev[k][p] for k in range(4)] + bw)
                        evl = e_mm
                        e_x = P.op("dve", lambda g, jo=jo, p=p, b=b: g.tensor_tensor(out=xT[:, jo, pc(p)], in0=PS.t[b][:, 0:PW], in1=xT[:, jo, pc(p)], op=ALU.add),
                                   waits=[e_mm, x_ev[jo][p]])
                        PS.free(b, [e_x])
                        x_ev[jo][p] = e_x
                Wm.release(wu_, [evl])
                mrg_rel = [evl]
        st["arena_rd"] = list(mrg_rel)

        v_ev = rmsnorm(sb + O_GFFN, st["u_rd"])
        st["u_rd"] = []
        moe_w = list(st["arena_rd"])
        e_rw = P.dma("sp", lambda g, ly=ly: g.dma_start(out=rwf[:], in_=dr["rw"][ly].rearrange("p (a b) -> p a b", b=36)), "rw", waits=moe_w)
        e_pl_ld = P.dma("pool", lambda g, ly=ly: g.dma_start(out=pTl[:], in_=dr["pT"][ly].rearrange("(c p) w -> p c w", p=128)), "pTl", waits=moe_w)
        e_rws = None
        for c in range(16):
            e_rws = P.op("dve", lambda g, c=c: g.tensor_scalar(out=rwf[:, c, :], in0=rwf[:, c, :], scalar1=smc(sb + O_GFFN + c), scalar2=None, op0=ALU.mult),
                         waits=[e_rw, e_sm])
        e_z1 = P.op("dve", lambda g: g.memset(LG[:], 0.0), waits=moe_w)
        e_z2 = P.op("dve", lambda g: g.memset(CT[:], 0.0), waits=moe_w)
        e_zsl0 = P.op("dve", lambda g: g.memset(SLf[:, :, 0:1], float(NSLOT)), waits=moe_w)
        e_zsl = P.op("dve", lambda g: g.memset(SLf[:, :, 1:2], float(NSLOT + 128)), waits=moe_w + [e_zsl0])
        e_zsl = P.op("dve", lambda g: g.tensor_scalar(out=SLf[:], in0=SLf[:], scalar1=smc(O_IOTA), scalar2=None, op0=ALU.add), waits=[e_zsl, e_sm])
        e_on = P.op("dve", lambda g: g.memset(NB[:, 128:160], 1.0), waits=moe_w)
        e_z3 = P.op("dve", lambda g: g.memset(RS[:], 0.0), waits=moe_w)
        ct_ev = []
        for tt in range(NTT):
            t0 = tt * 128
            ts_ = min(128, W - t0)
            pidx = [q for q in range(NPC) if q * PW < t0 + ts_ and (q + 1) * PW > t0]
            b, bw = PS.get()
            bank = PS.t[b]
            e_lg = mm_group(bank[0:ts_, 0:36], [(xT[:, c, t0:t0 + ts_], rwf[:, c, :]) for c in range(16)],
                            [e_rws] + [x_ev[c][q] for c in range(16) for q in pidx] + bw)
            e_rt = P.op("pe", lambda g, bank=bank, t0=t0, ts_=ts_: g.matmul(bank[0:ts_, 64:66], lhsT=rstdW[:, t0:t0 + ts_], rhs=identf[:, 0:2], start=True, stop=True),
                        waits=st["rstd_rd"] + [e_idf], sig=True)
            R = RS[0:ts_, tt, :]
            lg = LG[0:ts_, tt, :]

            def rcol(i, n=1, R=R):
                return R[:, i:i + n]
            e0 = P.op("dve", lambda g, bank=bank, ts_=ts_, R=R: g.tensor_copy(out=R[:, 0:1], in_=bank[0:ts_, 64:65]), waits=[e_rt, e_z3])
            e1 = P.op("dve", lambda g, bank=bank, ts_=ts_, lg=lg, R=R, ly=ly: g.scalar_tensor_tensor(
                out=lg, in0=bank[0:ts_, 0:36], scalar=R[:, 0:1], in1=sm[0:ts_, O_RB + ly * 36:O_RB + ly * 36 + 36], op0=ALU.mult, op1=ALU.add),
                waits=[e_lg, e0, e_z1, e_sm])
            PS.free(b, [e1])
            e2 = P.op("dve", lambda g, lg=lg, R=R: g.tensor_reduce(out=R[:, 1:2], in_=lg[:, 0:4], axis=AX.X, op=ALU.max, negate=True), waits=[e1])
            e3 = P.op("act", lambda g, lg=lg, R=R: g.activation(out=R[:, 4:8], in_=lg[:, 0:4], func=AF.Exp, bias=R[:, 1:2], accum_out=R[:, 2:3]), waits=[e2])
            e4 = P.op("dve", lambda g, lg=lg, R=R: g.tensor_scalar(out=R[:, 8:12], in0=lg[:, 0:4], scalar1=R[:, 1:2], scalar2=0.0, op0=ALU.add, op1=ALU.is_equal), waits=[e2])
            e5 = P.op("dve", lambda g, R=R: g.reciprocal(out=R[:, 3:4], in_=R[:, 2:3]), waits=[e3])
            e6 = P.op("dve", lambda g, lg=lg, R=R: g.tensor_scalar(out=R[:, 16:24], in0=lg[:, 4:12], scalar1=R[:, 8:9], scalar2=None, op0=ALU.mult), waits=[e4])
            for gi in range(1, 4):
                e6 = P.op("dve", lambda g, lg=lg, R=R, gi=gi: g.scalar_tensor_tensor(out=R[:, 16:24], in0=lg[:, 4 + 8 * gi:12 + 8 * gi], scalar=R[:, 8 + gi:9 + gi],
                                                                                  in1=R[:, 16:24], op0=ALU.mult, op1=ALU.add), waits=[e6])
            e7 = P.op("dve", lambda g, R=R: g.tensor_reduce(out=R[:, 12:13], in_=R[:, 16:24], axis=AX.X, op=ALU.max), waits=[e6])
            e8 = P.op("dve", lambda g, R=R: g.tensor_scalar(out=R[:, 24:32], in0=R[:, 16:24], scalar1=R[:, 12:13], scalar2=None, op0=ALU.is_equal), waits=[e7])
            e9 = P.op("dve", lambda g, R=R: g.scalar_tensor_tensor(out=R[:, 32:40], in0=R[:, 24:32], scalar=-1e30, in1=R[:, 16:24], op0=ALU.mult, op1=ALU.add), waits=[e8])
            e10 = P.op("dve", lambda g, R=R: g.tensor_reduce(out=R[:, 13:14], in_=R[:, 32:40], axis=AX.X, op=ALU.max), waits=[e9])
            e11 = P.op("dve", lambda g, R=R: g.tensor_scalar(out=R[:, 40:48], in0=R[:, 32:40], scalar1=R[:, 13:14], scalar2=None, op0=ALU.is_equal), waits=[e10])
            e12 = P.op("dve", lambda g, R=R: g.tensor_tensor(out=R[:, 14:15], in0=R[:, 13:14], in1=R[:, 12:13], op=ALU.subtract), waits=[e10])
            e13 = P.op("act", lambda g, R=R: g.activation(out=R[:, 15:16], in_=R[:, 14:15], func=AF.Exp), waits=[e12])
            e14 = P.op("dve", lambda g, R=R: g.tensor_scalar(out=R[:, 48:49], in0=R[:, 15:16], scalar1=1.0, scalar2=None, op0=ALU.add), waits=[e13])
            e15 = P.op("dve", lambda g, R=R: g.reciprocal(out=R[:, 49:50], in_=R[:, 48:49]), waits=[e14])
            e16 = P.op("dve", lambda g, R=R: g.tensor_tensor(out=R[:, 50:51], in0=R[:, 49:50], in1=R[:, 3:4], op=ALU.mult), waits=[e15, e5])
            e17 = P.op("dve", lambda g, R=R: g.tensor_tensor(out=R[:, 51:52], in0=R[:, 50:51], in1=R[:, 15:16], op=ALU.mult), waits=[e16])
            e18 = P.op("dve", lambda g, R=R: g.tensor_tensor(out=R[:, 52:60], in0=R[:, 24:32], in1=R[:, 40:48], op=ALU.add), waits=[e8, e11, e17])
            e20 = None
            for gi in range(4):
                e20 = P.op("dve", lambda g, R=R, gi=gi, ts_=ts_, tt=tt: g.tensor_scalar(out=CT[0:ts_, tt, gi * 8:gi * 8 + 8], in0=R[:, 52:60], scalar1=R[:, 8 + gi:9 + gi],
                                                                                   scalar2=None, op0=ALU.mult), waits=[e18, e4, e_z2])
            ct_ev.append(e20)
            st["rstd_rd"] = st["rstd_rd"] + [e_rt]

        Nrep, TL, INC, BASE, ONF = NB[:, 0:32], NB[:, 32:64], NB[:, 64:96], NB[:, 96:128], NB[:, 128:160]
        bN, wN = PS.get()
        eN = None
        for tt in range(NTT):
            eN = P.op("pe", lambda g, tt=tt, bN=bN: g.matmul(PS.t[bN][:, 0:32], lhsT=onesb[:], rhs=CT[:, tt, :], start=(tt == 0), stop=(tt == NTT - 1)),
                      waits=[ct_ev[tt], e_ones] + (wN if tt == 0 else []), sig=(tt == NTT - 1))
        e_n = P.op("dve", lambda g, bN=bN: g.tensor_copy(out=Nrep, in_=PS.t[bN][:, 0:32]), waits=[eN] + moe_w)
        PS.free(bN, [e_n])
        e_tl = P.op("dve", lambda g: g.tensor_scalar(out=TL, in0=Nrep, scalar1=0.0, scalar2=None, op0=ALU.is_gt), waits=[e_n])
        for jj in range(1, 9):
            e_tl = P.op("dve", lambda g, jj=jj: g.scalar_tensor_tensor(out=TL, in0=Nrep, scalar=128.0 * jj, in1=TL, op0=ALU.is_gt, op1=ALU.add), waits=[e_tl])
        e_inc = P.op("dve", lambda g: g.tensor_tensor_scan(out=INC, data0=ONF, data1=TL, initial=0.0, op0=ALU.mult, op1=ALU.add), waits=[e_tl, e_on])
        e_b1 = P.op("dve", lambda g: g.tensor_tensor(out=BASE, in0=INC, in1=TL, op=ALU.subtract), waits=[e_inc])
        e_base = P.op("dve", lambda g: g.tensor_scalar(out=BASE, in0=BASE, scalar1=128.0, scalar2=None, op0=ALU.mult), waits=[e_b1])
        e_c1 = P.op("dve", lambda g: g.tensor_scalar(out=CMP[:, 0:32], in0=INC, scalar1=smc(O_IOTA), scalar2=None, op0=ALU.is_le), waits=[e_inc, e_sm] + moe_w)
        e_et = P.op("dve", lambda g: g.tensor_reduce(out=CMP[:, 32:33], in_=CMP[:, 0:32], axis=AX.X, op=ALU.add), waits=[e_c1])
        e_et2 = P.op("dve", lambda g: g.tensor_scalar(out=CMP[:, 32:33], in0=CMP[:, 32:33], scalar1=32.0, scalar2=None, op0=ALU.min), waits=[e_et])
        e_de = P.op("dve", lambda g: g.tensor_scalar(out=DE, in0=identf[:, 0:48], scalar1=CMP[:, 32:33], scalar2=None, op0=ALU.mult), waits=[e_et2, e_idf])
        bE, wE = PS.get()
        eE = P.op("pe", lambda g, bE=bE: g.matmul(PS.t[bE][:, 0:48], lhsT=onesb[:], rhs=DE, start=True, stop=True), waits=[e_de] + wE, sig=True)
        e_iwf = P.op("dve", lambda g, bE=bE: g.tensor_scalar(out=IWf, in0=PS.t[bE][:, 0:48], scalar1=128.0, scalar2=smc(O_IOTA), op0=ALU.mult, op1=ALU.add), waits=[eE])
        PS.free(bE, [e_iwf])
        e_iw = P.op("dve", lambda g: g.tensor_copy(out=IW, in_=IWf), waits=[e_iwf])
        Wm.set_gate(("iw", ly), e_iw)

        sl_ev = []
        for tt in range(NTT):
            t0 = tt * 128
            ts_ = min(128, W - t0)
            R = RS[0:ts_, tt, :]
            bP, wP = PS.get()
            eP = None
            for t2 in range(tt + 1):
                lhs = onesb if t2 < tt else trib
                eP = P.op("pe", lambda g, t2=t2, tt=tt, bP=bP, lhs=lhs: g.matmul(PS.t[bP][:, 0:32], lhsT=lhs[:], rhs=CT[:, t2, :], start=(t2 == 0), stop=(t2 == tt)),
                          waits=[e_tri] + (wP if t2 == 0 else []), sig=(t2 == tt))
            S = S2[0:ts_, tt, 0:32]
            Sg = S2[0:ts_, tt, 32:40]
            tm = S2[0:ts_, tt, 40:48]
            e_s = P.op("dve", lambda g, S=S, bP=bP, ts_=ts_: g.tensor_tensor(out=S, in0=PS.t[bP][0:ts_, 0:32], in1=BASE[0:ts_, :], op=ALU.add), waits=[eP, e_base] + moe_w)
            PS.free(bP, [e_s])
            e_g = P.op("dve", lambda g, S=S, Sg=Sg, R=R: g.tensor_scalar(out=Sg, in0=S[:, 0:8], scalar1=R[:, 8:9], scalar2=None, op0=ALU.mult), waits=[e_s])
            for gi in range(1, 4):
                e_g = P.op("dve", lambda g, S=S, Sg=Sg, R=R, gi=gi: g.scalar_tensor_tensor(out=Sg, in0=S[:, 8 * gi:8 * gi + 8], scalar=R[:, 8 + gi:9 + gi], in1=Sg,
                                                                                         op0=ALU.mult, op1=ALU.add), waits=[e_g])
            e_m1 = P.op("dve", lambda g, Sg=Sg, tm=tm, R=R: g.tensor_tensor(out=tm, in0=Sg, in1=R[:, 24:32], op=ALU.mult), waits=[e_g])
            e_r1 = P.op("dve", lambda g, tm=tm, ts_=ts_, tt=tt: g.tensor_reduce(out=SLf[0:ts_, tt, 0:1], in_=tm, axis=AX.X, op=ALU.add), waits=[e_m1])
            e_m2 = P.op("dve", lambda g, Sg=Sg, tm=tm, R=R: g.tensor_tensor(out=tm, in0=Sg, in1=R[:, 40:48], op=ALU.mult), waits=[e_r1])
            e_r2 = P.op("dve", lambda g, tm=tm, ts_=ts_, tt=tt: g.tensor_reduce(out=SLf[0:ts_, tt, 1:2], in_=tm, axis=AX.X, op=ALU.add), waits=[e_m2])
            e_sl = P.op("dve", lambda g, tt=tt: g.tensor_copy(out=SL[:, tt, :], in_=SLf[:, tt, :]), waits=[e_r2, e_zsl])
            sl_ev.append(e_sl)

        XS = dr["XS"]
        YS = dr["YS"]
        sc_ev = [None, None]
        vt_rel = [list(moe_w) + st_zi, list(moe_w) + st_zi]
        last_vtr = None
        for tt in range(NTT):
            t0 = tt * 128
            ts_ = min(128, W - t0)
            pidx = [q for q in range(NPC) if q * PW < t0 + ts_ and (q + 1) * PW > t0]
            s = tt % 2
            vt = vtok[s]
            bA, wA = PS.get()
            bB, wB = PS.get()
            bfA = PS.t[bA][:, :].bitcast(BF16)
            bfB = PS.t[bB][:, :].bitcast(BF16)
            evA = evB = None
            for c in range(16):
                bk = bfA if c < 8 else bfB
                w = [e_idb] + [v_ev[c][q] for q in pidx]
                if c == 0:
                    w += wA
                if c == 8:
                    w += wB
                ev = P.op("pe", lambda g, bk=bk, c=c, t0=t0, ts_=ts_: g.transpose(bk[0:ts_, (c % 8) * 128:(c % 8) * 128 + 128], uT[:, c, t0:t0 + ts_], identb[:, :]),
                          waits=w, sig=(c in (7, 15)))
                if c == 7:
                    evA = ev
                if c == 15:
                    evB = ev
            last_vtr = evB
            e_c1 = P.op("act", lambda g, vt=vt, bfA=bfA, ts_=ts_: g.activation(out=vt[0:ts_, 0:1024], in_=bfA[0:ts_, :], func=AF.Copy), waits=[evA] + vt_rel[s])
            e_c2 = P.op("dve", lambda g, vt=vt, bfB=bfB, ts_=ts_: g.tensor_copy(out=vt[0:ts_, 1024:2048], in_=bfB[0:ts_, :]), waits=[evB] + vt_rel[s])
            PS.free(bA, [e_c1])
            PS.free(bB, [e_c2])
            e_sc = None
            for k in range(2):
                e_sc = P.dma("pool", lambda g, vt=vt, tt=tt, k=k: g.indirect_dma_start(
                    out=XS[:, :], out_offset=bass.IndirectOffsetOnAxis(ap=SL[:, tt, k:k + 1], axis=0), in_=vt[:, :], in_offset=None), f"sc{s}", waits=[e_c1, e_c2, sl_ev[tt]])
            vt_rel[s] = [e_sc]
            sc_ev[s] = e_sc
        st["u_rd"] = [last_vtr]

        wgh = dr[f"wg{ly}"][:, :]
        wuh = dr[f"wu{ly}"][:, :]
        wdh = dr[f"wd{ly}"][:, :]
        sc_all = [e for e in sc_ev if e is not None]
        xs_rel = [[], []]
        xj_rel = [list(moe_w), list(moe_w)]
        hb_rel = [list(moe_w), list(moe_w)]
        y_rel = [list(moe_w), list(moe_w)]
        ys_ev = [None, None]
        gate = ("iw", ly)
        tile_order = []
        for i_ in range(NTILE // 2):
            tile_order += [i_, NTILE - 1 - i_]
        for idx_, j in enumerate(tile_order):
            s = idx_ % 2
            xs = vtok[s]
            e_xs = P.dma("sp", lambda g, j=j, xs=xs: g.dma_start(out=xs[:, :], in_=XS[j * 128:(j + 1) * 128, :]), f"xs{s}",
                         waits=sc_all + xs_rel[s])
            bA, wA = PS.get()
            bB, wB = PS.get()
            bfA = PS.t[bA][:, :].bitcast(BF16)
            bfB = PS.t[bB][:, :].bitcast(BF16)
            evA = evB = None
            for c in range(16):
                bk = bfA if c < 8 else bfB
                w = [e_xs, e_idb]
                if c == 0:
                    w += wA
                if c == 8:
                    w += wB
                ev = P.op("pe", lambda g, bk=bk, c=c, xs=xs: g.transpose(bk[:, (c % 8) * 128:(c % 8) * 128 + 128], xs[:, c * 128:(c + 1) * 128], identb[:, :]),
                          waits=w, sig=(c in (7, 15)))
                if c == 7:
                    evA = ev
                if c == 15:
                    evB = ev
            xs_rel[s] = [evB]
            xj2 = xjT2[s]
            xj3 = xjT3[s]
            e_x1 = P.op("act", lambda g, xj2=xj2, bfA=bfA: g.activation(out=xj2[:, 0:1024], in_=bfA[:, :], func=AF.Copy), waits=[evA] + xj_rel[s])
            e_x2 = P.op("dve", lambda g, xj2=xj2, bfB=bfB: g.tensor_copy(out=xj2[:, 1024:2048], in_=bfB[:, :]), waits=[evB] + xj_rel[s])
            PS.free(bA, [e_x1])
            PS.free(bB, [e_x2])
            ug = Wm.get(("mg", ly, j), 4096, lambda off, j=j, wgh=wgh: (lambda g: g.indirect_dma_start(
                out=ring[:, off:off + 4096], out_offset=None, in_=wgh, in_offset=bass.IndirectOffsetOnAxis(ap=IW[:, j:j + 1], axis=0), bounds_check=REGS["bc"], oob_is_err=False)), gate)
            uu = Wm.get(("mu", ly, j), 4096, lambda off, j=j, wuh=wuh: (lambda g: g.indirect_dma_start(
                out=ring[:, off:off + 4096], out_offset=None, in_=wuh, in_offset=bass.IndirectOffsetOnAxis(ap=IW[:, j:j + 1], axis=0), bounds_check=REGS["bc"], oob_is_err=False)), gate)
            wg3 = rv(ug.off, 16, 256)
            wu3 = rv(uu.off, 16, 256)
            bG, wG = PS.get()
            bU, wU = PS.get()
            eG = mm_group(PS.t[bG][:, 0:256], [(xj3[:, c, :], wg3[:, c, :]) for c in range(16)], [ug.ev, e_x1, e_x2] + wG)
            eU = mm_group(PS.t[bU][:, 0:256], [(xj3[:, c, :], wu3[:, c, :]) for c in range(16)], [uu.ev] + wU)
            xj_rel[s] = [eU]
            Wm.release(ug, [eG])
            Wm.release(uu, [eU])
            hb = hbuf[s]
            sgt, ht, hTt = hb[:, 0:256], hb[:, 256:512], hb[:, 512:768]
            hT3 = hTt.rearrange("p (a b) -> p a b", b=128)
            e_sg = P.op("act", lambda g, sgt=sgt, bG=bG: g.activation(out=sgt, in_=PS.t[bG][:, 0:256], func=AF.Silu), waits=[eG] + hb_rel[s])
            PS.free(bG, [e_sg])
            e_h = P.op("dve", lambda g, sgt=sgt, ht=ht, bU=bU: g.tensor_tensor(out=ht, in0=PS.t[bU][:, 0:256], in1=sgt, op=ALU.mult), waits=[eU, e_sg] + hb_rel[s])
            PS.free(bU, [e_h])
            bH, wH = PS.get()
            bfH = PS.t[bH][:, :].bitcast(BF16)
            evH = None
            for fc in range(2):
                evH = P.op("pe", lambda g, bfH=bfH, ht=ht, fc=fc: g.transpose(bfH[:, fc * 128:fc * 128 + 128], ht[:, fc * 128:fc * 128 + 128], identb[:, :]),
                           waits=[e_h] + (wH if fc == 0 else []), sig=(fc == 1))
            e_ht = P.op("act", lambda g, hTt=hTt, bfH=bfH: g.activation(out=hTt, in_=bfH[:, 0:256], func=AF.Copy), waits=[evH] + hb_rel[s])
            PS.free(bH, [e_ht])
            ud = Wm.get(("md", ly, j), 4096, lambda off, j=j, wdh=wdh: (lambda g: g.indirect_dma_start(
                out=ring[:, off:off + 4096], out_offset=None, in_=wdh, in_offset=bass.IndirectOffsetOnAxis(ap=IW[:, j:j + 1], axis=0), bounds_check=REGS["bc"], oob_is_err=False)), gate)
            wd3 = rv(ud.off, 2, 2048)
            yt = ytile[s]
            evD = None
            evl = []
            for n in range(4):
                bD, wD = PS.get()
                evD = mm_group(PS.t[bD][:, 0:512], [(hT3[:, fc, :], wd3[:, fc, n * 512:(n + 1) * 512]) for fc in range(2)], [ud.ev, e_ht] + wD)
                if n < 2:
                    e_y = P.op("act", lambda g, yt=yt, bD=bD, n=n: g.activation(out=yt[:, n * 512:(n + 1) * 512], in_=PS.t[bD][:, 0:512], func=AF.Copy), waits=[evD] + y_rel[s])
                else:
                    e_y = P.op("dve", lambda g, yt=yt, bD=bD, n=n: g.tensor_copy(out=yt[:, n * 512:(n + 1) * 512], in_=PS.t[bD][:, 0:512]), waits=[evD] + y_rel[s])
                PS.free(bD, [e_y])
                evl.append(e_y)
            hb_rel[s] = [evD]
            Wm.release(ud, [evD])
            e_ys = P.dma("act", lambda g, j=j, yt=yt: g.dma_start(out=YS[j * 128:(j + 1) * 128, :], in_=yt[:, :]), f"ys{s}", waits=evl)
            y_rel[s] = [e_ys]
            ys_ev[s] = e_ys

        ys_all = [e for e in ys_ev if e is not None]
        pairs = [(gbA[0], gbA[1]), (gbB[0], gbB[1]), (ytile[0], ytile[1])]
        gb_rel = [[], [], []]
        tail_w = xs_rel[0] + xs_rel[1] + xj_rel[0] + xj_rel[1] + hb_rel[0] + hb_rel[1]
        last_add = None
        for tt in range(NTT):
            t0 = tt * 128
            ts_ = min(128, W - t0)
            R = RS[0:ts_, tt, :]
            pi = tt % 3
            Y1, Y2 = pairs[pi]
            Dg = DG2[pi]
            extra = tail_w if pi == 1 else []
            e_g1 = P.dma("pool", lambda g, Y1=Y1, tt=tt: g.indirect_dma_start(
                out=Y1[:, :], out_offset=None, in_=YS[:, :], in_offset=bass.IndirectOffsetOnAxis(ap=SL[:, tt, 0:1], axis=0)),
                f"gb{pi}a", waits=ys_all + gb_rel[pi] + extra)
            e_g2 = P.dma("pool", lambda g, Y2=Y2, tt=tt: g.indirect_dma_start(
                out=Y2[:, :], out_offset=None, in_=YS[:, :], in_offset=bass.IndirectOffsetOnAxis(ap=SL[:, tt, 1:2], axis=0)),
                f"gb{pi}b", waits=ys_all + gb_rel[pi] + extra)
            hb_ = DE[0:ts_, 2 * tt:2 * tt + 2]
            e_h1 = P.op("dve", lambda g, hb_=hb_, R=R: g.tensor_copy(out=hb_, in_=R[:, 50:52]), waits=[e_iw])
            e_lo = P.op("dve", lambda g, hb_=hb_, R=R: g.tensor_tensor(out=R[:, 60:62], in0=R[:, 50:52], in1=hb_, op=ALU.subtract), waits=[e_h1])
            dws = []
            for k in range(2):
                dws.append(P.op("dve", lambda g, Dg=Dg, k=k, ts_=ts_, tt=tt: g.tensor_scalar(
                    out=Dg[0:ts_, 2 * k, 0:ts_], in0=identf[0:ts_, 0:ts_], scalar1=DE[0:ts_, 2 * tt + k:2 * tt + k + 1], scalar2=None, op0=ALU.mult),
                    waits=[e_h1, e_idf] + gb_rel[pi] + tail_w))
                dws.append(P.op("dve", lambda g, Dg=Dg, k=k, ts_=ts_, R=R: g.tensor_scalar(
                    out=Dg[0:ts_, 2 * k + 1, 0:ts_], in0=identf[0:ts_, 0:ts_], scalar1=R[:, 60 + k:61 + k], scalar2=None, op0=ALU.mult),
                    waits=[e_lo, e_idf] + gb_rel[pi] + tail_w))
            bq = [PS.get() for _ in range(4)]
            evq = [None] * 4
            for c in range(16):
                q = c // 4
                cc = c % 4
                b_, w_ = bq[q]
                evq[q] = mm_group(PS.t[b_][:, cc * 128:cc * 128 + ts_],
                                  [(Y1[0:ts_, c * 128:(c + 1) * 128], Dg[0:ts_, 0, 0:ts_]), (Y1[0:ts_, c * 128:(c + 1) * 128], Dg[0:ts_, 1, 0:ts_]),
                                   (Y2[0:ts_, c * 128:(c + 1) * 128], Dg[0:ts_, 2, 0:ts_]), (Y2[0:ts_, c * 128:(c + 1) * 128], Dg[0:ts_, 3, 0:ts_])],
                                  [e_g1, e_g2] + dws + (w_ if cc == 0 else []))
            gb_rel[pi] = [evq[3]]
            for q in range(4):
                b_, w_ = bq[q]
                last_add = P.op("dve", lambda g, b_=b_, q=q, t0=t0, ts_=ts_: g.tensor_tensor(
                    out=xT[:, 4 * q:4 * q + 4, t0:t0 + ts_], in0=PS.t[b_][:, :].rearrange("p (a b) -> p a b", b=128)[:, :, 0:ts_],
                    in1=xT[:, 4 * q:4 * q + 4, t0:t0 + ts_], op=ALU.add), waits=[evq[q]])
                PS.free(b_, [last_add])
        for c in range(16):
            for p in range(NPC):
                x_ev[c][p] = last_add

        q_ev = rmsnorm(sb + O_GPLE, st["u_rd"])
        st["u_rd"] = []
        pgv = dr["ple_gate_w"][ly].rearrange("(c p) n -> p c n", p=128)
        ppv = dr["ple_proj_w"][ly]
        pps = []
        for k in range(2):
            pps.append(Wm.get(("ple_proj", ly, k), 2048,
                              lambda off, k=k, ppv=ppv: (lambda g: g.dma_start(out=ring[:, off:off + 2048], in_=ppv[k * 128:k * 128 + 128, :]))))
        evl = None
        for j in range(16):
            ug = Wm.get(("ple_gate", ly, j), 16 * 128,
                        lambda off, j=j, pgv=pgv: (lambda g: g.dma_start(out=rv(off, 16, 128), in_=pgv[:, :, j * 128:j * 128 + 128])))
            wg_ = rv(ug.off, 16, 128)
            for p in range(NPC):
                bG, w1 = PS.get()
                bP, w2 = PS.get()
                eG = mm_group(PS.t[bG][:, 0:PW], [(wg_[:, k, :], uT[:, k, pc(p)]) for k in range(16)], [ug.ev] + [q_ev[k][p] for k in range(16)] + w1)
                eP = mm_group(PS.t[bP][:, 0:PW], [(ring[:, pps[k].off + j * 128:pps[k].off + j * 128 + 128], pTl[:, k, pc(p)]) for k in range(2)],
                              [pps[0].ev, pps[1].ev, e_pl_ld] + w2)
                evl = eP
                ti, tt_, tw = T32.get()
                e_sg = P.op("act", lambda g, tt_=tt_, bG=bG: g.activation(out=tt_[:], in_=PS.t[bG][:, 0:PW], func=AF.Sigmoid), waits=[eG] + tw)
                PS.free(bG, [e_sg])
                e_t = P.op("dve", lambda g, tt_=tt_, bP=bP: g.tensor_tensor(out=tt_[:], in0=PS.t[bP][:, 0:PW], in1=tt_[:], op=ALU.mult), waits=[eP, e_sg])
                PS.free(bP, [e_t])
                e_x = P.op("dve", lambda g, tt_=tt_, j=j, p=p: g.tensor_tensor(out=xT[:, j, pc(p)], in0=tt_[:], in1=xT[:, j, pc(p)], op=ALU.add),
                           waits=[e_t, x_ev[j][p]])
                T32.free(ti, [e_x])
                x_ev[j][p] = e_x
            Wm.release(ug, [evl])
        for k in range(2):
            Wm.release(pps[k], [evl])
        st["u_rd"] = [evl]
        st["arena_rd"] = [evl]

    for _ly in range(n_layers):
        layer_body(_ly)

    fin_w = list(st["arena_rd"])
    ov = dr["outT"].rearrange("(c p) t -> p c t", p=128)
    out_evs = []
    rs_ev = [None] * NPC
    for p in range(NPC):
        b, bw = PS.get()
        bank = PS.t[b]
        last = None
        for c in range(16):
            si, sq, sw = SQ.get()
            e_sq = P.op("act", lambda g, c=c, p=p, sq=sq: g.activation(out=sq[:], in_=xT[:, c, pc(p)], func=AF.Square), waits=[x_ev[c][p]] + sw)
            last = P.op("pe", lambda g, c=c, sq=sq, bank=bank: g.matmul(bank[:, 0:PW], lhsT=onesb[:], rhs=sq[:], start=(c == 0), stop=(c == 15)),
                        waits=[e_sq] + (bw if c == 0 else []), sig=True)
            SQ.free(si, [last])
        ti, tt, tw = T32.get()
        e_rt = P.op("act", lambda g, tt=tt, bank=bank: g.activation(out=tt[:], in_=bank[:, 0:PW], func=AF.Sqrt, bias=smc(O_EPS), scale=1.0 / D),
                    waits=[last] + tw)
        PS.free(b, [e_rt])
        rs_ev[p] = P.op("dve", lambda g, tt=tt, p=p: g.reciprocal(out=rstdW[:, pc(p)], in_=tt[:]), waits=[e_rt] + st["rstd_rd"])
        T32.free(ti, [rs_ev[p]])
    ot_rel = [[], []]
    for c in range(16):
        s = c % 2
        e_o = None
        for p in range(NPC):
            e_o = P.op("dve", lambda g, c=c, p=p, s=s: g.scalar_tensor_tensor(out=outtmp[:, s, pc(p)], in0=xT[:, c, pc(p)], scalar=smc(O_GFIN + c), in1=rstdW[:, pc(p)],
                                                                             op0=ALU.mult, op1=ALU.mult), waits=[rs_ev[p], x_ev[c][p]] + fin_w + ot_rel[s])
        q = "sp" if c % 2 == 0 else "act"
        e_d = P.dma(q, lambda g, c=c, s=s: g.dma_start(out=ov[:, c, :], in_=outtmp[:, s, H:W]), f"o{c}", waits=[e_o])
        ot_rel[s] = [e_d]
        out_evs.append(e_d)
    P.op("sp", lambda g: g.nop(), waits=out_evs, sig=False)


O_IOTA = None
O_EPS = None


def build(n_layers=NL):
    global O_EPS, O_IOTA, NS
    O_EPS = O_RB + NL * 36
    O_IOTA = O_EPS + 1
    nst = O_IOTA + 1
    nc = bass.Bass("TRN2", target_bir_lowering=False)
    dr = {}

    def din(name, shape):
        dr[name] = nc.dram_tensor(name, list(shape), F32, kind="ExternalInput").ap()

    din("xT", (D, W))
    din("pT", (NL, 256, W))
    din("sm", (128, nst))
    din("rw", (NL, 128, 16 * 36))
    din("ident", (128, 128))
    din("tri", (128, 128))
    dr["zsrc"] = nc.dram_tensor("zsrc", [128, 1024], F32, kind="ExternalInput").bitcast(BF16)
    din("w_in", (NL, D, 7168))
    din("w_conv_out", (NL, 1024, D))
    din("pool_w", (NL, 4, 256, 512))
    din("w_out", (NL, D, D))
    for l_ in range(NL):
        for nm_ in ("wg", "wu", "wd"):
            dr[f"{nm_}{l_}"] = nc.dram_tensor(f"{nm_}{l_}", [4096, 4096], F32, kind="ExternalInput")
    din("ple_gate_w", (NL, D, D))
    din("ple_proj_w", (NL, 256, D))
    dr["outT"] = nc.dram_tensor("outT", [D, T], F32, kind="ExternalOutput").ap()
    dr["XS"] = nc.dram_tensor("XS", [NSLOT + 256, D], BF16, kind="Internal")
    dr["YS"] = nc.dram_tensor("YS", [NSLOT + 256, D], BF16, kind="Internal")

    tn = {"dram": dr}
    tn["xT"] = nc.alloc_sbuf_tensor("xT_sb", [128, 16, W], F32)
    tn["uT"] = nc.alloc_sbuf_tensor("uT_sb", [128, 16, W], BF16)
    tn["arena"] = nc.alloc_sbuf_tensor("arena", [128, ARENA], BF16)
    tn["ring"] = nc.alloc_sbuf_tensor("ring", [128, RING], BF16)
    tn["sm"] = nc.alloc_sbuf_tensor("sm_sb", [128, nst], F32)
    tn["identf"] = nc.alloc_sbuf_tensor("identf", [128, 128], F32)
    tn["identb"] = nc.alloc_sbuf_tensor("identb", [128, 128], BF16)
    tn["onesb"] = nc.alloc_sbuf_tensor("onesb", [128, 128], BF16)
    tn["rstdW"] = nc.alloc_sbuf_tensor("rstdW", [128, W], F32)
    tn["sqp"] = [nc.alloc_sbuf_tensor(f"sqp{i}", [128, PW], BF16) for i in range(3)]
    tn["t32"] = [nc.alloc_sbuf_tensor(f"t32_{i}", [128, PW], F32) for i in range(3)]
    tn["tb16"] = [nc.alloc_sbuf_tensor(f"tb16_{i}", [128, PW], BF16) for i in range(6)]
    tn["trib"] = nc.alloc_sbuf_tensor("trib", [128, 128], BF16)
    tn["psum"] = [nc.alloc_psum_tensor(f"ps{i}", [128, 512], F32) for i in range(8)]

    Pd = Prog(nc, dry=True)
    Wd = WMgr(Pd, None)
    emit(nc, Pd, Wd, tn, n_layers)
    P = Prog(nc, dry=False)
    Wm = WMgr(P, Wd.req)
    emit(nc, P, Wm, tn, n_layers)
    assert Wm.cur == len(Wd.req)
    P.run()
    return nc


def _prep_inputs(inp):
    f = lambda a: np.ascontiguousarray(np.asarray(a, dtype=np.float32))
    x = f(inp["x"])
    p = f(inp["p"])
    nst = O_RB + NL * 36 + 2

    def pm(v, nch):
        return v.reshape(nch, 128).T

    sm = np.zeros((128, nst), np.float32)
    for l in range(NL):
        b = l * LS
        sm[:, b + O_GMIX:b + O_GMIX + 16] = pm(f(inp["norm_mix_g"])[l], 16)
        sm[:, b + O_BGLU:b + O_BGLU + 16] = pm(f(inp["b_glu"])[l], 16)
        cw = f(inp["conv_w"])[l]
        sm[:, b + O_CONVW:b + O_CONVW + 248] = cw.reshape(31, 8, 128).transpose(2, 0, 1).reshape(128, 248)
        sm[:, b + O_CONVB:b + O_CONVB + 8] = pm(f(inp["conv_b"])[l], 8)
        sm[:, b + O_LNG:b + O_LNG + 8] = pm(f(inp["conv_ln_g"])[l], 8)
        sm[:, b + O_LNB:b + O_LNB + 8] = pm(f(inp["conv_ln_b"])[l], 8)
        sm[:, b + O_PSC:b + O_PSC + 16] = pm(f(inp["pool_scale"])[l], 16)
        sm[:, b + O_GFFN:b + O_GFFN + 16] = pm(f(inp["norm_ffn_g"])[l], 16)
        sm[:, b + O_GPLE:b + O_GPLE + 16] = pm(f(inp["norm_ple_g"])[l], 16)
        rb = np.concatenate([f(inp["router_group_b"])[l], f(inp["router_expert_b"])[l]])
        sm[:, O_RB + l * 36:O_RB + l * 36 + 36] = rb[None, :]
    sm[:, O_GFIN:O_GFIN + 16] = pm(f(inp["final_norm_g"]), 16)
    sm[:, nst - 2] = EPS
    sm[:, nst - 1] = np.arange(128, dtype=np.float32)
    rw = np.stack([np.concatenate([f(inp["router_group_w"])[l], f(inp["router_expert_w"])[l]], axis=1)
                   .reshape(16, 128, 36).transpose(1, 0, 2).reshape(128, 16 * 36) for l in range(NL)])
    shared = {
        "rw": np.ascontiguousarray(rw),
        "ident": np.eye(128, dtype=np.float32),
        "w_in": f(inp["w_in"]),
        "w_conv_out": f(inp["w_conv_out"]),
        "pool_w": f(inp["pool_w"]),
        "w_out": f(inp["w_out"]),
        "tri": np.triu(np.ones((128, 128), np.float32), 1),
        "zsrc": np.zeros((128, 1024), np.float32),
        "ple_gate_w": f(inp["ple_gate_w"]),
        "ple_proj_w": f(inp["ple_proj_w"]),
    }
    wgh = f(inp["expert_w_gate"]).reshape(NL, 32, 16, 128, 256).transpose(0, 1, 3, 2, 4).reshape(NL, 4096, 4096)
    wuh = f(inp["expert_w_up"]).reshape(NL, 32, 16, 128, 256).transpose(0, 1, 3, 2, 4).reshape(NL, 4096, 4096)
    wdh = f(inp["expert_w_down"]).reshape(NL, 32, 2, 128, D).transpose(0, 1, 3, 2, 4).reshape(NL, 4096, 4096)
    for l in range(NL):
        shared[f"wg{l}"] = np.ascontiguousarray(wgh[l])
        shared[f"wu{l}"] = np.ascontiguousarray(wuh[l])
        shared[f"wd{l}"] = np.ascontiguousarray(wdh[l])
    in_maps = []
    for core in range(8):
        b, half = core // 2, core % 2
        t0 = half * T - H
        xs = np.zeros((W, D), np.float32)
        ps = np.zeros((NL, W, 256), np.float32)
        lo = max(t0, 0)
        xs[lo - t0:] = x[b, lo:t0 + W]
        ps[:, lo - t0:] = p[:, b, lo:t0 + W]
        smc = sm.copy()
        smc[:, O_HM] = float(half)
        for gi, wz in enumerate(POOL_WIN):
            pos = half * T + np.arange(16)
            smc[:, O_INVC + gi * 16:O_INVC + gi * 16 + 16] = (1.0 / np.minimum(pos + 1, wz)).astype(np.float32)[None, :]
        m = dict(shared)
        m["xT"] = np.ascontiguousarray(xs.T)
        m["pT"] = np.ascontiguousarray(ps.transpose(0, 2, 1))
        m["sm"] = smc
        in_maps.append(m)
    return in_maps


_NC_CACHE = {}


def kernel(**inputs):
    if "nc" not in _NC_CACHE:
        _NC_CACHE["nc"] = build(NL)
    nc = _NC_CACHE["nc"]
    in_maps = _prep_inputs(inputs)
    res = run_bass_kernel_spmd(nc, in_maps, core_ids=list(range(8)))
    out = np.zeros((4, 2 * T, D), np.float32)
    for core in range(8):
        b, half = core // 2, core % 2
        out[b, half * T:(half + 1) * T, :] = res.results[core]["outT"].T
    return out
```
